# Optimizing a Trainium2 kernel written in Bass

```python
import math
import jax
import jax.numpy as jnp
from jax import lax
import numpy as np

D_MODEL = 1024
BATCH = 2
SEQ = 16384
DEPTH = 2

DN_ALPHA = (2.0 * DEPTH) ** 0.25
DN_BETA = (8.0 * DEPTH) ** -0.25
LN_EPS = 1e-5
NORM_EPS = 1e-6
CONV_W = 4
N_EVEN = (DEPTH + 1) // 2
N_ODD = DEPTH // 2

SSD_HEAD_DIM = 64
SSD_D_INNER = D_MODEL
SSD_HEADS = SSD_D_INNER // SSD_HEAD_DIM
SSD_GROUPS = 4
SSD_HPG = SSD_HEADS // SSD_GROUPS
SSD_STATE = 128
SSD_CHUNK = 128
SSD_CONV_DIM = SSD_D_INNER + 2 * SSD_GROUPS * SSD_STATE

GDN_HEAD_DIM = 128
GDN_HEADS = D_MODEL // GDN_HEAD_DIM
GDN_DIM = GDN_HEADS * GDN_HEAD_DIM
GDN_CHUNK = 64

RW_HEAD_DIM = 64
RW_DIM = D_MODEL
RW_HEADS = RW_DIM // RW_HEAD_DIM
RW_DECAY_LORA = 64
RW_AAA_LORA = 64
RW_GATE_LORA = 160
RW_GN_EPS = 64e-5
RW_SPLITS = (RW_DIM, RW_DIM, RW_DIM, RW_DECAY_LORA, RW_AAA_LORA, RW_GATE_LORA)
RW_SHIFT_COLS = sum(RW_SPLITS)

MB_HEAD_DIM = 128
MB_HEADS = D_MODEL // MB_HEAD_DIM
MB_DIM = MB_HEADS * MB_HEAD_DIM
MB_BLOCK = 256
MB_TOPK = 3
MB_QCHUNK = 32

EVEN_SPLITS = (SSD_D_INNER, SSD_CONV_DIM, SSD_HEADS, 3 * GDN_DIM, GDN_DIM, GDN_HEADS, GDN_HEADS)
EVEN_COLS = sum(EVEN_SPLITS)
EVEN_OUT = SSD_D_INNER + GDN_DIM
ODD_SPLITS = (RW_SHIFT_COLS, MB_DIM, MB_DIM, MB_DIM)
ODD_COLS = sum(ODD_SPLITS)
ODD_OUT = RW_DIM + MB_DIM

N_EXPERTS = 32
TOP_K = 4
D_FF = D_MODEL
SWIGLU_LIMIT = 7.0
SWIGLU_ALPHA = 1.702
MOE_ROWS = 256

kernel_name = 'hybrid_ssd_gdn_rwkv7_moba_moe'


def _split(t, sizes):
    return jnp.split(t, [int(s) for s in np.cumsum(sizes)[:-1]], axis=-1)


def layer_norm(x, g, b):
    xf = x.astype(jnp.float32)
    mu = jnp.mean(xf, axis=-1, keepdims=True)
    var = jnp.mean(jnp.square(xf - mu), axis=-1, keepdims=True)
    return ((xf - mu) * lax.rsqrt(var + LN_EPS) * g + b).astype(x.dtype)


def rms_norm(x, w):
    return x * lax.rsqrt(jnp.mean(jnp.square(x), axis=-1, keepdims=True) + NORM_EPS) * w


def l2_normalize(x):
    return x * lax.rsqrt(jnp.sum(jnp.square(x), axis=-1, keepdims=True) + NORM_EPS)


def causal_dwconv(x, w):
    return lax.conv_general_dilated(
        x, w.astype(x.dtype)[:, None, :], window_strides=(1,),
        padding=((CONV_W - 1, 0),), dimension_numbers=('NWC', 'WIO', 'NWC'),
        feature_group_count=x.shape[-1])


def token_shift(x):
    return jnp.pad(x, ((0, 0), (1, 0), (0, 0)))[:, :-1]


def ssd_chunked(x, dt, a_head, bm, cm):
    b, L, G, J, P = x.shape
    N = bm.shape[-1]
    Q = SSD_CHUNK
    nc = L // Q
    la = (dt * a_head).reshape(b, nc, Q, G, J)
    xd = (x * dt[..., None]).reshape(b, nc, Q, G, J, P)
    bc = bm.reshape(b, nc, Q, G, N)
    cc = cm.reshape(b, nc, Q, G, N)
    acum = jnp.cumsum(la, axis=2)
    causal = jnp.tril(jnp.ones((Q, Q), bool))[:, :, None, None]
    seg = jnp.exp(jnp.where(causal, acum[:, :, :, None] - acum[:, :, None, :], -jnp.inf))
    cb = jnp.einsum('bclgn,bcsgn->bclsg', cc, bc)
    y_diag = jnp.einsum('bclsgj,bcsgjp->bclgjp', cb[..., None] * seg, xd)
    decay_to_end = jnp.exp(acum[:, :, -1:] - acum)
    states = jnp.einsum('bclgn,bclgjp->bcgjpn', bc, xd * decay_to_end[..., None])
    chunk_decay = jnp.exp(acum[:, :, -1])

    def step(h, inp):
        s_c, d_c = inp
        return h * d_c[..., None, None] + s_c, h

    h0 = jnp.zeros((b, G, J, P, N), jnp.float32)
    _, prev = lax.scan(step, h0, (jnp.moveaxis(states, 1, 0), jnp.moveaxis(chunk_decay, 1, 0)))
    prev = jnp.moveaxis(prev, 0, 1)
    y_off = jnp.einsum('bclgn,bcgjpn->bclgjp', cc, prev) * jnp.exp(acum)[..., None]
    return (y_diag + y_off).reshape(b, L, G, J, P)


def gated_delta_rule_chunked(q, k, v, g, beta):
    b, L, h, dk = q.shape
    dv = v.shape[-1]
    C = GDN_CHUNK
    nc = L // C

    def chunks(t):
        t = jnp.moveaxis(t, 2, 1)
        return t.reshape((b, h, nc, C) + t.shape[3:])

    q, k, v, g, beta = (chunks(t) for t in (q, k, v, g, beta))
    gc = jnp.cumsum(g, axis=-1)
    incl = jnp.tril(jnp.ones((C, C), bool))
    strict = jnp.tril(jnp.ones((C, C), bool), -1)
    decay = jnp.exp(jnp.where(incl, gc[..., :, None] - gc[..., None, :], -jnp.inf))
    kb = k * beta[..., None]
    m = jnp.where(strict, jnp.einsum('bhcid,bhcjd->bhcij', kb, k) * decay, 0.0)
    rhs = jnp.concatenate([v * beta[..., None], kb * jnp.exp(gc)[..., None]], axis=-1)
    sol = lax.linalg.triangular_solve(m + jnp.eye(C, dtype=m.dtype), rhs, left_side=True,
                                      lower=True, unit_diagonal=True)
    u, w = sol[..., :dv], sol[..., dv:]
    attn = jnp.einsum('bhcid,bhcjd->bhcij', q, k) * decay
    g_last = gc[..., -1]
    k_tail = k * jnp.exp(g_last[..., None] - gc)[..., None]
    q_head = q * jnp.exp(gc)[..., None]

    def step(S, inp):
        qh, kt, u_c, w_c, a_c, gl = inp
        v_new = u_c - jnp.einsum('bhck,bhkv->bhcv', w_c, S)
        o = jnp.einsum('bhck,bhkv->bhcv', qh, S) + jnp.einsum('bhij,bhjv->bhiv', a_c, v_new)
        S = S * jnp.exp(gl)[..., None, None] + jnp.einsum('bhck,bhcv->bhkv', kt, v_new)
        return S, o

    xs = tuple(jnp.moveaxis(t, 2, 0) for t in (q_head, k_tail, u, w, attn, g_last))
    _, o = lax.scan(step, jnp.zeros((b, h, dk, dv), jnp.float32), xs)
    return jnp.transpose(o, (1, 0, 3, 2, 4)).reshape(b, L, h, dv)


def rwkv7_time_mix(p, mu, w0, w2, a0, a2, g2, k_k, k_a, r_k, gn_g, gn_b):
    b, L, _ = p.shape
    H, N = RW_HEADS, RW_HEAD_DIM
    heads = lambda t: t.reshape(b, L, H, N)
    p = p + mu * (token_shift(p) - p)
    r, k, v, pw, pa, pg = _split(p, RW_SPLITS)
    w = w0 + jnp.tanh(pw) @ w2
    decay = jnp.exp(-jnp.exp(-jax.nn.softplus(-w) - 0.5))
    a = jax.nn.sigmoid(a0 + pa @ a2)
    g = jax.nn.sigmoid(pg) @ g2
    kk = l2_normalize(heads(k * k_k))
    k = k * (1.0 + (a - 1.0) * k_a)
    r, decay, k, v, a = (heads(t) for t in (r, decay, k, v, a))

    def step(S, inp):
        r_t, w_t, k_t, v_t, kk_t, a_t = inp
        sa = jnp.einsum('bhvk,bhk->bhv', S, -kk_t)
        S = (S * w_t[:, :, None, :] + sa[..., None] * (kk_t * a_t)[:, :, None, :]
             + v_t[..., None] * k_t[:, :, None, :])
        return S, jnp.einsum('bhvk,bhk->bhv', S, r_t)

    xs = tuple(jnp.moveaxis(t, 1, 0) for t in (r, decay, k, v, kk, a))
    _, y = lax.scan(step, jnp.zeros((b, H, N, N), jnp.float32), xs)
    y = jnp.moveaxis(y, 0, 1)
    m = jnp.mean(y, axis=-1, keepdims=True)
    var = jnp.mean(jnp.square(y - m), axis=-1, keepdims=True)
    y = (y - m) * lax.rsqrt(var + RW_GN_EPS) * gn_g.reshape(H, N) + gn_b.reshape(H, N)
    y = y + jnp.sum(r * k * r_k, axis=-1, keepdims=True) * v
    return y.reshape(b, L, RW_DIM) * g


def moba_attention(q, k, v):
    b, L, h, dh = q.shape
    Lp = -(-L // MB_BLOCK) * MB_BLOCK
    nblk = Lp // MB_BLOCK
    nq = Lp // MB_QCHUNK
    topk = min(MB_TOPK, nblk)
    pad = ((0, 0), (0, Lp - L), (0, 0), (0, 0))
    q, k, v = (jnp.moveaxis(jnp.pad(t, pad), 2, 1) for t in (q, k, v))
    kblk = k.reshape(b, h, nblk, MB_BLOCK, dh)
    vblk = v.reshape(b, h, nblk, MB_BLOCK, dh)
    kmean = jnp.mean(kblk, axis=3)
    qch = jnp.moveaxis(q.reshape(b, h, nq, MB_QCHUNK, dh), 2, 0)
    bi = jnp.arange(b)[:, None, None, None]
    hi = jnp.arange(h)[None, :, None, None]
    scale = dh ** -0.5

    def attend(args):
        qi, ci = args
        start = ci * MB_QCHUNK
        qb = start // MB_BLOCK
        qpos = start + jnp.arange(MB_QCHUNK)
        gate = jnp.einsum('bhqd,bhnd->bhqn', qi, kmean)
        gate = jnp.where(jnp.arange(nblk) < qb, gate, -jnp.inf)
        _, idx = lax.top_k(gate, topk)
        valid = idx < qb
        ks = kblk[bi, hi, idx]
        vs = vblk[bi, hi, idx]
        s_sel = jnp.einsum('bhqd,bhqtkd->bhqtk', qi, ks) * scale
        s_sel = jnp.where(valid[..., None], s_sel, -jnp.inf).reshape(b, h, MB_QCHUNK, topk * MB_BLOCK)
        k_own = lax.dynamic_index_in_dim(kblk, qb, axis=2, keepdims=False)
        v_own = lax.dynamic_index_in_dim(vblk, qb, axis=2, keepdims=False)
        kpos = qb * MB_BLOCK + jnp.arange(MB_BLOCK)
        s_own = jnp.where(kpos[None, :] <= qpos[:, None],
                          jnp.einsum('bhqd,bhkd->bhqk', qi, k_own) * scale, -jnp.inf)
        p = jax.nn.softmax(jnp.concatenate([s_sel, s_own], axis=-1).astype(jnp.float32), axis=-1)
        p_sel = p[..., :topk * MB_BLOCK].reshape(b, h, MB_QCHUNK, topk, MB_BLOCK)
        p_own = p[..., topk * MB_BLOCK:]
        return (jnp.einsum('bhqtk,bhqtkd->bhqd', p_sel, vs)
                + jnp.einsum('bhqk,bhkd->bhqd', p_own, v_own))

    out = lax.map(attend, (qch, jnp.arange(nq)))
    return jnp.transpose(out, (1, 0, 3, 2, 4)).reshape(b, Lp, h, dh)[:, :L]


def moe_ffn(x, router_w, router_b, w_up, b_up, w_down, b_down):
    b, L, d = x.shape
    n_tok = b * L
    n_asg = n_tok * TOP_K
    xt = x.reshape(n_tok, d)
    logits = (xt @ router_w + router_b).astype(jnp.float32)
    top_val, top_idx = lax.top_k(logits, TOP_K)
    gates = jax.nn.softmax(top_val, axis=-1).reshape(n_asg)
    e_flat = top_idx.reshape(n_asg)
    order = jnp.argsort(e_flat)
    e_sorted = e_flat[order]
    tok_sorted = order // TOP_K
    gate_sorted = gates[order]
    counts = jnp.bincount(e_flat, length=N_EXPERTS)
    padded = (counts + MOE_ROWS - 1) // MOE_ROWS * MOE_ROWS
    start = jnp.cumsum(counts) - counts
    pend = jnp.cumsum(padded)
    pstart = pend - padded
    dest = pstart[e_sorted] + jnp.arange(n_asg) - start[e_sorted]
    n_groups = -(-(n_asg + N_EXPERTS * (MOE_ROWS - 1)) // MOE_ROWS)
    group_expert = jnp.minimum(
        jnp.searchsorted(pend, jnp.arange(n_groups) * MOE_ROWS, side='right'), N_EXPERTS - 1)
    buf = jnp.zeros((n_groups * MOE_ROWS, d), x.dtype).at[dest].set(xt[tok_sorted])

    def expert_rows(args):
        rows, e = args
        hid = (rows @ w_up[e] + b_up[e]).astype(jnp.float32)
        glu = jnp.minimum(hid[:, 0::2], SWIGLU_LIMIT)
        lin = jnp.clip(hid[:, 1::2], -SWIGLU_LIMIT, SWIGLU_LIMIT)
        act = glu * jax.nn.sigmoid(SWIGLU_ALPHA * glu) * (lin + 1.0)
        return (act.astype(rows.dtype) @ w_down[e] + b_down[e]).astype(jnp.float32)

    out = lax.map(expert_rows, (buf.reshape(n_groups, MOE_ROWS, d), group_expert))
    y = out.reshape(-1, d)[dest] * gate_sorted[:, None]
    y = jax.ops.segment_sum(y, tok_sorted, num_segments=n_tok)
    return y.reshape(b, L, d).astype(x.dtype)


def even_mixer(x, w_in, ssd_conv_w, ssd_conv_b, ssd_dt_bias, ssd_a_log, ssd_d, ssd_norm_w,
               gdn_conv_w, gdn_a_log, gdn_dt_bias, gdn_norm_w, w_out):
    b, L, _ = x.shape
    G, J, P, N = SSD_GROUPS, SSD_HPG, SSD_HEAD_DIM, SSD_STATE
    H, Dh = GDN_HEADS, GDN_HEAD_DIM
    proj = jnp.einsum('bld,dc->blc', x, w_in).astype(jnp.float32)
    z_s, xbc, dt_raw, qkv, z_g, b_raw, a_raw = _split(proj, EVEN_SPLITS)
    xbc = jax.nn.silu(causal_dwconv(xbc, ssd_conv_w) + ssd_conv_b)
    xs, bm, cm = _split(xbc, (SSD_D_INNER, G * N, G * N))
    xs = xs.reshape(b, L, G, J, P)
    dt = jax.nn.softplus(dt_raw + ssd_dt_bias).reshape(b, L, G, J)
    a_head = -jnp.exp(ssd_a_log.astype(jnp.float32)).reshape(G, J)
    y = ssd_chunked(xs, dt, a_head, bm.reshape(b, L, G, N), cm.reshape(b, L, G, N))
    y = y + xs * ssd_d.reshape(G, J)[..., None]
    y = y.reshape(b, L, SSD_D_INNER) * jax.nn.silu(z_s)
    y_ssd = rms_norm(y.reshape(b, L, G, SSD_D_INNER // G),
                     ssd_norm_w.reshape(G, SSD_D_INNER // G)).reshape(b, L, SSD_D_INNER)
    qkv = jax.nn.silu(causal_dwconv(qkv, gdn_conv_w))
    q, k, v = (t.reshape(b, L, H, Dh) for t in _split(qkv, (GDN_DIM, GDN_DIM, GDN_DIM)))
    q = l2_normalize(q) * Dh ** -0.5
    k = l2_normalize(k)
    beta = jax.nn.sigmoid(b_raw)
    g = -jnp.exp(gdn_a_log) * jax.nn.softplus(a_raw + gdn_dt_bias)
    o = gated_delta_rule_chunked(q, k, v, g, beta)
    o = rms_norm(o, gdn_norm_w) * jax.nn.silu(z_g.reshape(b, L, H, Dh))
    mixed = jnp.concatenate([y_ssd, o.reshape(b, L, GDN_DIM)], axis=-1)
    return jnp.einsum('blc,cd->bld', mixed.astype(x.dtype), w_out).astype(x.dtype)


def odd_mixer(x, w_in, rw_mu, rw_w0, rw_w2, rw_a0, rw_a2, rw_g2, rw_k_k, rw_k_a, rw_r_k,
              rw_gn_g, rw_gn_b, w_out):
    b, L, _ = x.shape
    proj = jnp.einsum('bld,dc->blc', x, w_in).astype(jnp.float32)
    p_rw, q, k, v = _split(proj, ODD_SPLITS)
    y_rw = rwkv7_time_mix(p_rw, rw_mu, rw_w0, rw_w2, rw_a0, rw_a2, rw_g2, rw_k_k, rw_k_a,
                          rw_r_k, rw_gn_g, rw_gn_b)
    heads = lambda t: t.reshape(b, L, MB_HEADS, MB_HEAD_DIM)
    o = moba_attention(heads(q), heads(k), heads(v)).reshape(b, L, MB_DIM)
    mixed = jnp.concatenate([y_rw, o], axis=-1)
    return jnp.einsum('blc,cd->bld', mixed.astype(x.dtype), w_out).astype(x.dtype)


def _dt_bias(key, shape):
    dt = jnp.exp(jax.random.uniform(key, shape, jnp.float32, math.log(1e-3), math.log(1e-1)))
    return dt + jnp.log(-jnp.expm1(-dt))


def _a_log(key, shape):
    return jnp.log(jax.random.uniform(key, shape, jnp.float32, 1.0, 16.0))


def setup_inputs(seed: int = 0) -> dict:
    key = jax.random.key(seed)
    ks = iter(jax.random.split(key, 64))
    nrm = lambda shape, scale: jax.random.normal(next(ks), shape, jnp.float32) * scale
    gain = lambda shape: 1.0 + nrm(shape, 0.02)
    E, O, D = N_EVEN, N_ODD, D_MODEL
    ratio = jnp.linspace(0.0, 1.0, RW_DIM)
    inp = {}
    inp['x'] = nrm((BATCH, SEQ, D), 1.0)
    inp['ev_w_in'] = nrm((E, D, EVEN_COLS), D ** -0.5)
    inp['ev_ssd_conv_w'] = nrm((E, CONV_W, SSD_CONV_DIM), CONV_W ** -0.5)
    inp['ev_ssd_conv_b'] = nrm((E, SSD_CONV_DIM), 0.01)
    inp['ev_ssd_dt_bias'] = _dt_bias(next(ks), (E, SSD_HEADS))
    inp['ev_ssd_a_log'] = _a_log(next(ks), (E, SSD_HEADS))
    inp['ev_ssd_d'] = gain((E, SSD_HEADS))
    inp['ev_ssd_norm_w'] = gain((E, SSD_D_INNER))
    inp['ev_gdn_conv_w'] = nrm((E, CONV_W, 3 * GDN_DIM), CONV_W ** -0.5)
    inp['ev_gdn_a_log'] = _a_log(next(ks), (E, GDN_HEADS))
    inp['ev_gdn_dt_bias'] = _dt_bias(next(ks), (E, GDN_HEADS))
    inp['ev_gdn_norm_w'] = gain((E, GDN_HEAD_DIM))
    inp['ev_w_out'] = nrm((E, EVEN_OUT, D), EVEN_OUT ** -0.5 * DN_BETA)
    inp['od_w_in'] = nrm((O, D, ODD_COLS), D ** -0.5)
    inp['od_rw_mu'] = jax.random.uniform(next(ks), (O, RW_SHIFT_COLS), jnp.float32)
    inp['od_rw_w0'] = -6.5 + 5.0 * ratio ** 0.85 + nrm((O, RW_DIM), 0.1)
    inp['od_rw_w2'] = nrm((O, RW_DECAY_LORA, RW_DIM), 0.1)
    inp['od_rw_a0'] = nrm((O, RW_DIM), 0.1)
    inp['od_rw_a2'] = nrm((O, RW_AAA_LORA, RW_DIM), RW_AAA_LORA ** -0.5)
    inp['od_rw_g2'] = nrm((O, RW_GATE_LORA, RW_DIM), RW_GATE_LORA ** -0.5)
    inp['od_rw_k_k'] = 0.85 + nrm((O, RW_DIM), 0.02)
    inp['od_rw_k_a'] = gain((O, RW_DIM))
    inp['od_rw_r_k'] = -0.04 + nrm((O, RW_HEADS, RW_HEAD_DIM), 0.1)
    inp['od_rw_gn_g'] = gain((O, RW_DIM))
    inp['od_rw_gn_b'] = nrm((O, RW_DIM), 0.01)
    inp['od_w_out'] = nrm((O, ODD_OUT, D), ODD_OUT ** -0.5 * DN_BETA)
    inp['mix_ln_g'] = gain((DEPTH, D))
    inp['mix_ln_b'] = nrm((DEPTH, D), 0.01)
    inp['moe_router_w'] = nrm((DEPTH, D, N_EXPERTS), D ** -0.5)
    inp['moe_router_b'] = nrm((DEPTH, N_EXPERTS), 0.01)
    inp['moe_w_up'] = nrm((DEPTH, N_EXPERTS, D, 2 * D_FF), D ** -0.5)
    inp['moe_b_up'] = nrm((DEPTH, N_EXPERTS, 2 * D_FF), 0.01)
    inp['moe_w_down'] = nrm((DEPTH, N_EXPERTS, D_FF, D), D_FF ** -0.5 * DN_BETA)
    inp['moe_b_down'] = nrm((DEPTH, N_EXPERTS, D), 0.01)
    inp['ffn_ln_g'] = gain((DEPTH, D))
    inp['ffn_ln_b'] = nrm((DEPTH, D), 0.01)
    return inp


def reference(x, ev_w_in, ev_ssd_conv_w, ev_ssd_conv_b, ev_ssd_dt_bias, ev_ssd_a_log, ev_ssd_d,
              ev_ssd_norm_w, ev_gdn_conv_w, ev_gdn_a_log, ev_gdn_dt_bias, ev_gdn_norm_w, ev_w_out,
              od_w_in, od_rw_mu, od_rw_w0, od_rw_w2, od_rw_a0, od_rw_a2, od_rw_g2, od_rw_k_k,
              od_rw_k_a, od_rw_r_k, od_rw_gn_g, od_rw_gn_b, od_w_out, mix_ln_g, mix_ln_b,
              moe_router_w, moe_router_b, moe_w_up, moe_b_up, moe_w_down, moe_b_down,
              ffn_ln_g, ffn_ln_b):
    for i in range(DEPTH):
        j = i // 2
        if i % 2 == 0:
            h = even_mixer(x, ev_w_in[j], ev_ssd_conv_w[j], ev_ssd_conv_b[j], ev_ssd_dt_bias[j],
                           ev_ssd_a_log[j], ev_ssd_d[j], ev_ssd_norm_w[j], ev_gdn_conv_w[j],
                           ev_gdn_a_log[j], ev_gdn_dt_bias[j], ev_gdn_norm_w[j], ev_w_out[j])
        else:
            h = odd_mixer(x, od_w_in[j], od_rw_mu[j], od_rw_w0[j], od_rw_w2[j], od_rw_a0[j],
                          od_rw_a2[j], od_rw_g2[j], od_rw_k_k[j], od_rw_k_a[j], od_rw_r_k[j],
                          od_rw_gn_g[j], od_rw_gn_b[j], od_w_out[j])
        x = layer_norm(DN_ALPHA * x + h, mix_ln_g[i], mix_ln_b[i])
        h = moe_ffn(x, moe_router_w[i], moe_router_b[i], moe_w_up[i], moe_b_up[i],
                    moe_w_down[i], moe_b_down[i])
        x = layer_norm(DN_ALPHA * x + h, ffn_ln_g[i], ffn_ln_b[i])
    return x
```

```python
from contextlib import ExitStack
import numpy as np
import concourse.bass as bass
import concourse.mybir as mybir
from concourse.bass_utils import run_bass_kernel_spmd

F32, BF16 = mybir.dt.float32, mybir.dt.bfloat16
AF = mybir.ActivationFunctionType
ALU = mybir.AluOpType
AX = mybir.AxisListType


class Sched:
    EPOCH = 20000

    def __init__(self, nc, strict=True, G=None):
        self.nc = nc
        self.G = G
        self._q = {}
        self.ops = []
        self.strict = strict
        self.last_writer = {}
        self.readers = {}

    def qv(self, e):
        k = id(e)
        if k not in self._q:
            self._q[k] = e.partition_id() % 4
        return self._q[k]

    def op(self, eng, fn, reads=(), writes=(), dma=False, acc=False, dkey=None, cc=False, alts=None):
        i = len(self.ops)
        deps = set()
        for k in reads:
            w = self.last_writer.get(k)
            if w is not None:
                deps.add(w)
            if isinstance(k, tuple) and k[0] == 'ps':
                for r in self.readers.get(k, ()):
                    if self.ops[r]['eng'] != eng:
                        deps.add(r)
        for k in writes:
            w = self.last_writer.get(k)
            if w is not None:
                pw = self.ops[w]
                if not (pw['eng'] == eng and not pw['dma'] and not dma):
                    deps.add(w)
            for r in self.readers.get(k, ()):
                pr_ = self.ops[r]
                if not (pr_['eng'] == eng and not pr_['dma'] and not dma):
                    deps.add(r)
        for k in reads:
            self.readers.setdefault(k, []).append(i)
        for k in writes:
            self.last_writer[k] = i
            self.readers[k] = []
        if cc:
            dma = True
        if dma and dkey is None:
            dkey = (tuple(writes) + tuple(reads))[0]
        self.ops.append(dict(eng=eng, fn=fn, deps=deps, dma=dma, sig=False, dkey=dkey, tok=None, inc=(1 if cc else 16), alts=alts))
        return i

    def emit(self, final_eng='sync'):
        nc = self.nc
        ops = self.ops
        for op in ops:
            keep = set()
            for d in op['deps']:
                p = ops[d]
                if p['dma'] or op['dma'] or p['eng'] != op['eng'] or self.strict:
                    p['sig'] = True
                    keep.add(d)
            op['deps'] = keep
        last_of = {}
        for i, op in enumerate(ops):
            if not op['dma'] and op['fn'] is not None:
                last_of[op['eng']] = i
        for i in last_of.values():
            ops[i]['sig'] = True
        eng_cnt = {}
        dma_cnt = {}
        sem_names = []
        for op in ops:
            if op['dma']:
                op['sig'] = True
                n = dma_cnt.get(op['dkey'], 0) + 1
                dma_cnt[op['dkey']] = n
                op['tok'] = (('d', op['dkey']), op['inc'] * n)
            elif op['sig']:
                n = eng_cnt.get(op['eng'], 0)
                eng_cnt[op['eng']] = n + 1
                op['tok'] = (('e', op['eng'], n // self.EPOCH), n % self.EPOCH + 1)
        names = []
        for op in ops:
            if op['tok'] is not None and op['tok'][0] not in names:
                names.append(op['tok'][0])
        self.n_sems = len(names)
        G = self.G
        G.phase += 1
        phase = G.phase
        with ExitStack() as st:
            sems = {}
            for i, nm in enumerate(names):
                sems[nm] = G.sem_stack.enter_context(nc.semaphore("p%d_s%d" % (phase, i)))
            block = st.enter_context(nc.Block())
            engs = ['tensor', 'vector', 'scalar', 'gpsimd', 'sync']
            final = {}
            for op in ops:
                if op['dma']:
                    final[op['tok'][0]] = max(final.get(op['tok'][0], 0), op['tok'][1])

            def make_body(en):
                def body(e):
                    waited = {}
                    for op in ops:
                        if op['eng'] != en:
                            continue
                        for d in sorted(op['deps']):
                            nm, val = ops[d]['tok']
                            if waited.get(nm, 0) < val:
                                e.wait_ge(sems[nm], val)
                                waited[nm] = val
                        if op['fn'] is None:
                            continue
                        if op['alts'] is not None:
                            q = self.qv(e)
                            nm, val = op['tok']
                            for j, f in enumerate(op['alts']):
                                with e.If(q == j):
                                    f(e).then_inc(sems[nm], op['inc'])
                            continue
                        ins = op['fn'](e)
                        if op['sig']:
                            nm, val = op['tok']
                            ins.then_inc(sems[nm], op['inc'] if op['dma'] else 1)
                    if en == final_eng:
                        for nm, val in final.items():
                            if waited.get(nm, 0) < val:
                                e.wait_ge(sems[nm], val)
                    if en in last_of:
                        nm, val = ops[last_of[en]]['tok']
                        if waited.get(nm, 0) < val:
                            e.wait_ge(sems[nm], val)
                    e.nop().then_inc(G.bar, 1)
                    e.wait_ge(G.bar, 5 * phase)
                return body

            for en in engs:
                getattr(block, en)(make_body(en))


D = 1024
NE = 32
DN_ALPHA = 4.0 ** 0.25
LN_EPS = 1e-5


class Stop(Exception):
    pass


def phase_post(nc, G, L, T, PT, Wd, xres, y, mxo, mxg, NEXP=NE, strict=True):
    stage = 99
    NP = T // PT
    NT = PT // 128
    NH = PT // 512
    w_out, lnp, rw, rb, w_up, b_upT, w_down, b_down, ident_d, x1s = (Wd[k] for k in
        ['w_out', 'lnp', 'rw', 'rb', 'w_up', 'b_upT', 'w_down', 'b_down', 'ident', 'x1s'])
    TB = mxo.shape[0]

    S = Sched(nc, strict=strict, G=G)
    with ExitStack() as es:
        sb = lambda name, shape, dty=F32: es.enter_context(nc.sbuf_tensor("ph%d_%s" % (G.phase + 1, name), shape, dty))
        wbu = [sb("wbu%d" % i, [128, 8 * 2048], BF16) for i in range(2)]
        wbd = sb("wbd", [128, 8, D], BF16)
        xT = sb("xT", [128, 8, PT], BF16)
        yacc = sb("yacc", [128, NT, D])
        mt = sb("mt", [128, 16, 128], BF16)
        mtok = sb("mtok", [128, 4, 512], BF16)
        identb = sb("identb", [128, 128], BF16)
        xr = sb("xr", [128, D])
        z = sb("z", [128, D])
        x1 = [sb("x1_%d" % i, [128, D]) for i in range(2)]
        xT32 = sb("xT32", [128, 8, 128])
        actT = sb("actT", [128, 8, 512], BF16)
        gt = [sb("g%d" % i, [128, 512]) for i in range(2)]
        st_ = [sb("s%d" % i, [128, 512]) for i in range(2)]
        lt = [sb("l%d" % i, [128, 512]) for i in range(2)]
        gates = sb("gates", [128, T // 128, NE])
        gT = sb("gT", [NE, 128])
        bu = sb("bu", [128, NE * 16])
        bu1 = sb("bu1", [128, NE * 16])
        rw32 = sb("rw32", [128, 8, NE])
        rbb = sb("rbb", [128, NE])
        bd32 = sb("bd32", [NE, D])
        ident = sb("identsb", [128, 128])
        lnpb = sb("lnpb", [128, 4 * D])
        stats = sb("stats", [128, 2, 6])
        mv = sb("mv", [128, 2])
        sm = sb("sm", [128, 8])
        lg = sb("lg", [128, NE])
        m8 = sb("m8", [128, 8])
        ex = sb("ex", [128, NE])
        sel = sb("sel", [128, NE])
        ps = G.ps
        psb = [p_[:].bitcast(BF16) for p_ in ps]

        RCH = 1024
        NCH = TB // RCH
        mxg4 = mxg.rearrange("(i r t) c -> i r t c", i=NCH, r=4)
        for i in range(NCH):
            S.op('gpsimd', lambda e, i=i: e.collective_compute('AllGather', ALU.bypass, replica_groups=[[0, 1, 2, 3], [4, 5, 6, 7]],
                                                               ins=[mxo[i * RCH:(i + 1) * RCH, :]], outs=[mxg[i * 4 * RCH:(i + 1) * 4 * RCH, :]]),
                 writes=[('mxg', i)], cc=True, dkey='ccg')
        S.op('sync', lambda e: e.dma_start(out=lnpb[:], in_=lnp.partition_broadcast(128)), writes=['lnpb'], dma=True)
        S.op('sync', lambda e: e.dma_start(out=rbb[:], in_=rb.partition_broadcast(128)), writes=['rbb'], dma=True)
        S.op('sync', lambda e: e.dma_start(out=rw32[:], in_=rw.rearrange("(k p) e -> p k e", p=128)), writes=['rw32'], dma=True)
        S.op('sync', lambda e: e.dma_start(out=bu[:], in_=b_upT), writes=['bu'], dma=True)
        S.op('sync', lambda e: e.dma_start(out=bd32[:], in_=b_down), writes=['bd32'], dma=True)
        S.op('sync', lambda e: e.dma_start(out=ident[:], in_=ident_d), writes=['ident'], dma=True)
        S.op('vector', lambda e: e.tensor_copy(out=identb[:], in_=ident[:]), reads=['ident'], writes=['identb'])
        S.op('vector', lambda e: e.tensor_scalar(out=bu1[:], in0=bu[:], scalar1=1.0, scalar2=None, op0=ALU.add),
             reads=['bu'], writes=['bu1'])

        def layer_norm(zkey, zt, okey, ot, pidx):
            for h in range(2):
                S.op('vector', lambda e, h=h: e.bn_stats(out=stats[:, h, :], in_=zt[:, h * 512:(h + 1) * 512]),
                     reads=[zkey], writes=[('stats', h)])
            S.op('vector', lambda e: e.bn_aggr(out=mv[:], in_=stats[:].rearrange("p a b -> p (a b)")),
                 reads=[('stats', 0), ('stats', 1)], writes=['mv'])
            S.op('vector', lambda e: e.tensor_scalar(out=sm[:, 0:1], in0=mv[:, 1:2], scalar1=LN_EPS, scalar2=None, op0=ALU.add),
                 reads=['mv'], writes=['sm0'])
            S.op('scalar', lambda e: e.activation(out=sm[:, 1:2], in_=sm[:, 0:1], func=AF.Sqrt), reads=['sm0'], writes=['sm1'])
            S.op('vector', lambda e: e.reciprocal(out=sm[:, 2:3], in_=sm[:, 1:2]), reads=['sm1'], writes=['sm2'])
            S.op('vector', lambda e: e.tensor_scalar(out=zt[:], in0=zt[:], scalar1=mv[:, 0:1], scalar2=sm[:, 2:3],
                                                     op0=ALU.subtract, op1=ALU.mult),
                 reads=[zkey, 'mv', 'sm2'], writes=[zkey])
            S.op('vector', lambda e: e.tensor_tensor(out=zt[:], in0=zt[:], in1=lnpb[:, pidx * D:(pidx + 1) * D], op=ALU.mult),
                 reads=[zkey, 'lnpb'], writes=[zkey])
            S.op('vector', lambda e: e.tensor_tensor(out=ot[:], in0=zt[:], in1=lnpb[:, (pidx + 1) * D:(pidx + 2) * D], op=ALU.add),
                 reads=[zkey, 'lnpb'], writes=[okey])

        w_out_v = w_out.rearrange("(k p) d -> p k d", p=128)
        wo = wbu[1][:, :].rearrange("p (k d) -> p k d", k=16)
        wu = [wbu[i][:, :].rearrange("p (k f) -> p k f", k=8) for i in range(2)]

        try:
          if stage == 0: raise Stop
          for p in range(NP):
              for hh in range(2):
                  S.op('gpsimd', lambda e, hh=hh: e.dma_start(out=wo[:, hh * 8:(hh + 1) * 8, :], in_=w_out_v[:, hh * 8:(hh + 1) * 8, :]),
                       writes=[('wbu', 1)], dma=True, dkey=('wbu', 1))
              for i in range(NT):
                  gi = p * NT + i
                  t0 = gi * 128
                  S.op('sync', True, reads=[('mxg', i_) for i_ in range(NCH)], writes=['mtok'], dma=True, dkey='mtokld',
                       alts=[(lambda e, tt=jq * T + t0: e.dma_start(out=mtok[:], in_=mxg4[tt // RCH, :, tt % RCH:tt % RCH + 128, :].rearrange("r t c -> t r c")))
                             for jq in range(4)])
                  for kk in range(16):
                      S.op('tensor', lambda e, kk=kk: e.transpose(out=psb[2 + kk // 8][:, (kk % 8) * 128:(kk % 8 + 1) * 128],
                                                                    in_=mtok[:, kk // 4, (kk % 4) * 128:(kk % 4 + 1) * 128], identity=identb[:]),
                           reads=['mtok', 'identb'], writes=[('ps', 2 + kk // 8)], acc=True)
                  for hh in range(2):
                      S.op('vector' if hh == 0 else 'scalar', (lambda e, hh=hh: e.tensor_copy(out=mt[:, hh * 8:(hh + 1) * 8, :], in_=psb[2 + hh][:].rearrange("p (c t) -> p c t", c=8))) if hh == 0 else
                           (lambda e, hh=hh: e.activation(out=mt[:, hh * 8:(hh + 1) * 8, :], in_=psb[2 + hh][:].rearrange("p (c t) -> p c t", c=8), func=AF.Identity)),
                           reads=[('ps', 2 + hh)], writes=['mt'])
                  S.op('sync', lambda e, t0=t0: e.dma_start(out=xr[:], in_=xres[t0:t0 + 128, :]), writes=['xr'], dma=True)
                  if stage == 1: raise Stop
                  for h in range(2):
                      for k in range(16):
                          S.op('tensor', lambda e, h=h, k=k: e.matmul(ps[h][:], lhsT=mt[:, k, :], rhs=wo[:, k, h * 512:(h + 1) * 512],
                                                                      start=(k == 0), stop=(k == 15)),
                               reads=['mt', ('wbu', 1)], writes=[('ps', h)], acc=True)
                      S.op('vector', lambda e, h=h: e.scalar_tensor_tensor(out=z[:, h * 512:(h + 1) * 512], in0=xr[:, h * 512:(h + 1) * 512],
                                                                           scalar=DN_ALPHA, in1=ps[h][:], op0=ALU.mult, op1=ALU.add),
                           reads=['xr', ('ps', h)], writes=['z'])
                  if stage == 2: raise Stop
                  xs = x1[gi % 2]
                  xk = ('x1', gi % 2)
                  layer_norm('z', z, xk, xs, 0)
                  if stage == 3: raise Stop
                  S.op('sync', lambda e, t0=t0, xs=xs: e.dma_start(out=x1s[t0:t0 + 128, :], in_=xs[:]), reads=[xk], writes=[('x1s', gi)],
                       dma=True, dkey=('x1st', gi % 2))
                  if stage == 31: raise Stop
                  for c in range(8):
                      S.op('tensor', lambda e, c=c, xs=xs: e.transpose(out=ps[2 + c // 4][:, (c % 4) * 128:(c % 4 + 1) * 128],
                                                                       in_=xs[:, c * 128:(c + 1) * 128], identity=ident[:]),
                           reads=[xk, 'ident'], writes=[('ps', 2 + c // 4)], acc=True)
                  if stage == 32: raise Stop
                  for hh in range(2):
                      S.op('scalar', lambda e, hh=hh: e.activation(out=xT32[:, hh * 4:(hh + 1) * 4, :],
                                                                   in_=ps[2 + hh][:].rearrange("p (c t) -> p c t", c=4), func=AF.Identity),
                           reads=[('ps', 2 + hh)], writes=[('xT32', hh)])
                      if stage == 33: continue
                      S.op('vector', lambda e, hh=hh, i=i: e.tensor_copy(out=xT[:, hh * 4:(hh + 1) * 4, i * 128:(i + 1) * 128],
                                                                         in_=xT32[:, hh * 4:(hh + 1) * 4, :]),
                           reads=[('xT32', hh)], writes=[('xT', i)])
                  if stage == 4: raise Stop
                  for c in range(8):
                      S.op('tensor', lambda e, c=c: e.matmul(ps[6][:, 0:NE], lhsT=xT32[:, c, :], rhs=rw32[:, c, :], start=(c == 0), stop=(c == 7)),
                           reads=[('xT32', c // 4), 'rw32'], writes=[('ps', 6)], acc=True)
                  S.op('vector', lambda e: e.tensor_tensor(out=lg[:], in0=ps[6][:, 0:NE], in1=rbb[:], op=ALU.add),
                       reads=[('ps', 6), 'rbb'], writes=['lg'])
                  S.op('vector', lambda e: e.max(out=m8[:], in_=lg[:]), reads=['lg'], writes=['m8'])
                  S.op('vector', lambda e: e.tensor_scalar(out=sel[:], in0=lg[:], scalar1=m8[:, 3:4], scalar2=None, op0=ALU.is_ge),
                       reads=['lg', 'm8'], writes=['sel'])
                  S.op('vector', lambda e: e.tensor_scalar(out=sm[:, 3:4], in0=m8[:, 0:1], scalar1=-1.0, scalar2=None, op0=ALU.mult),
                       reads=['m8'], writes=['sm3'])
                  S.op('scalar', lambda e: e.activation(out=ex[:], in_=lg[:], func=AF.Exp, bias=sm[:, 3:4], scale=1.0),
                       reads=['lg', 'sm3'], writes=['ex'])
                  S.op('vector', lambda e: e.tensor_tensor(out=ex[:], in0=ex[:], in1=sel[:], op=ALU.mult), reads=['ex', 'sel'], writes=['ex'])
                  S.op('vector', lambda e: e.reduce_sum(out=sm[:, 4:5], in_=ex[:], axis=AX.X), reads=['ex'], writes=['sm4'])
                  S.op('vector', lambda e: e.reciprocal(out=sm[:, 5:6], in_=sm[:, 4:5]), reads=['sm4'], writes=['sm5'])
                  S.op('vector', lambda e, gi=gi: e.tensor_scalar(out=gates[:, gi, :], in0=ex[:], scalar1=sm[:, 5:6], scalar2=None, op0=ALU.mult),
                       reads=['ex', 'sm5'], writes=[('gates', gi)])
                  if stage == 5: raise Stop
                  S.op('tensor', lambda e, gi=gi: e.transpose(out=ps[7][0:NE, 0:128], in_=gates[:, gi, :], identity=ident[:]),
                       reads=[('gates', gi), 'ident'], writes=[('ps', 7)])
                  S.op('scalar', lambda e: e.activation(out=gT[:], in_=ps[7][0:NE, 0:128], func=AF.Identity), reads=[('ps', 7)], writes=['gT'])
                  for h in range(2):
                      S.op('tensor', lambda e, h=h: e.matmul(ps[4 + h][:], lhsT=gT[:], rhs=bd32[:, h * 512:(h + 1) * 512], start=True, stop=True),
                           reads=['gT', 'bd32'], writes=[('ps', 4 + h)])
                      S.op('scalar', lambda e, h=h, i=i: e.activation(out=yacc[:, i, h * 512:(h + 1) * 512], in_=ps[4 + h][:], func=AF.Identity),
                           reads=[('ps', 4 + h)], writes=[('yacc', i)])
              if stage == 6: raise Stop
              for ex_i in range(NEXP):
                  sl = ex_i % 2
                  for hh in range(2):
                      S.op('gpsimd', lambda e, ex_i=ex_i, sl=sl, hh=hh: e.dma_start(
                          out=wu[sl][:, hh * 4:(hh + 1) * 4, :],
                          in_=w_up[ex_i].rearrange("(k p) f -> p k f", p=128)[:, hh * 4:(hh + 1) * 4, :]),
                          writes=[('wbu', sl)], dma=True, dkey=('wbu', sl))
                  S.op('gpsimd', lambda e, ex_i=ex_i: e.dma_start(out=wbd[:], in_=w_down[ex_i].rearrange("(k p) d -> p k d", p=128)),
                       writes=['wbd'], dma=True)
                  for th in range(NH):
                      for fc in range(8):
                          q = fc % 2
                          for half in range(2):
                              pst = ps[q * 2 + half]
                              for k in range(8):
                                  S.op('tensor', lambda e, pst=pst, sl=sl, k=k, fc=fc, half=half, th=th: e.matmul(
                                      pst[:], lhsT=wu[sl][:, k, half * 1024 + fc * 128: half * 1024 + (fc + 1) * 128],
                                      rhs=xT[:, k, th * 512:(th + 1) * 512], start=(k == 0), stop=(k == 7)),
                                      reads=[('wbu', sl)] + [('xT', th * 4 + j) for j in range(4)], writes=[('ps', q * 2 + half)], acc=True)
                          col = ex_i * 16 + fc
                          S.op('vector', lambda e, q=q, col=col: e.tensor_scalar(out=gt[q][:], in0=ps[q * 2][:], scalar1=bu[:, col:col + 1], scalar2=7.0,
                                                                                 op0=ALU.add, op1=ALU.min),
                               reads=[('ps', q * 2), 'bu'], writes=[('g', q)])
                          S.op('vector', lambda e, q=q, col=col: e.tensor_scalar(out=lt[q][:], in0=ps[q * 2 + 1][:], scalar1=bu1[:, col + 8:col + 9], scalar2=8.0,
                                                                                 op0=ALU.add, op1=ALU.min),
                               reads=[('ps', q * 2 + 1), 'bu1'], writes=[('l', q)])
                          S.op('scalar', lambda e, q=q: e.activation(out=st_[q][:], in_=gt[q][:], func=AF.Sigmoid, scale=1.702),
                               reads=[('g', q)], writes=[('s', q)])
                          S.op('vector', lambda e, q=q: e.tensor_tensor(out=gt[q][:], in0=gt[q][:], in1=st_[q][:], op=ALU.mult),
                               reads=[('g', q), ('s', q)], writes=[('g', q)])
                          S.op('vector', lambda e, q=q, fc=fc: e.scalar_tensor_tensor(out=actT[:, fc, :], in0=lt[q][:], scalar=-6.0, in1=gt[q][:],
                                                                                      op0=ALU.max, op1=ALU.mult),
                               reads=[('l', q), ('g', q)], writes=[('actT', fc)])
                      for tt in range(4):
                          ti = th * 4 + tt
                          gi = p * NT + ti
                          for dh in range(2):
                              for fc in range(8):
                                  S.op('tensor', lambda e, dh=dh, fc=fc, tt=tt: e.matmul(
                                      ps[4 + dh][:], lhsT=actT[:, fc, tt * 128:(tt + 1) * 128], rhs=wbd[:, fc, dh * 512:(dh + 1) * 512],
                                      start=(fc == 0), stop=(fc == 7)),
                                      reads=[('actT', fc), 'wbd'], writes=[('ps', 4 + dh)], acc=True)
                              S.op('vector', lambda e, dh=dh, ti=ti, gi=gi, ex_i=ex_i: e.scalar_tensor_tensor(
                                  out=yacc[:, ti, dh * 512:(dh + 1) * 512], in0=ps[4 + dh][:], scalar=gates[:, gi, ex_i:ex_i + 1],
                                  in1=yacc[:, ti, dh * 512:(dh + 1) * 512], op0=ALU.mult, op1=ALU.add),
                                  reads=[('ps', 4 + dh), ('gates', gi), ('yacc', ti)], writes=[('yacc', ti)])
              for i in range(NT):
                  gi = p * NT + i
                  t0 = gi * 128
                  S.op('sync', lambda e, t0=t0: e.dma_start(out=xr[:], in_=x1s[t0:t0 + 128, :]), reads=[('x1s', gi)], writes=['xr'], dma=True)
                  S.op('vector', lambda e, i=i: e.scalar_tensor_tensor(out=z[:], in0=xr[:], scalar=DN_ALPHA, in1=yacc[:, i, :], op0=ALU.mult, op1=ALU.add),
                       reads=['xr', ('yacc', i)], writes=['z'])
                  os_ = x1[gi % 2]
                  ok = ('x1', gi % 2)
                  layer_norm('z', z, ok, os_, 2)
                  S.op('sync', lambda e, t0=t0, os_=os_: e.dma_start(out=y[t0:t0 + 128, :], in_=os_[:]), reads=[ok], writes=[('y', gi)],
                       dma=True, dkey=('x1st', gi % 2))
        except Stop:
            pass
        S.emit()
    return S


def post_inputs(mixed, xres, w_out, lng1, lnb1, rw, rb, w_up, b_up, w_down, b_down, lng2, lnb2):
    perm = np.concatenate([np.arange(0, 2 * D, 2), np.arange(1, 2 * D, 2)])
    b_up_p = b_up[:, perm]
    b_upT = np.ascontiguousarray(b_up_p.reshape(NE, 16, 128).transpose(2, 0, 1).reshape(128, NE * 16))
    return {
        "mixT": np.ascontiguousarray(mixed.T), "xres": np.ascontiguousarray(xres), "w_out": np.ascontiguousarray(w_out),
        "lnp": np.ascontiguousarray(np.concatenate([lng1, lnb1, lng2, lnb2])), "rw": np.ascontiguousarray(rw), "rb": np.ascontiguousarray(rb),
        "w_up": np.ascontiguousarray(w_up[:, :, perm]), "b_upT": b_upT, "w_down": np.ascontiguousarray(w_down),
        "b_down": np.ascontiguousarray(b_down), "ident": np.eye(128, dtype=np.float32),
    }


D = 1024
NORM_EPS = 1e-6
NPV = 912


def phase_mix0(nc, G, T, Wd, mxo, strict=True):
    NTL = T // 128
    xTp, Wc, cw, Wp, pv, consts = (Wd[k] for k in ['xTp', 'Wc', 'cw', 'Wp', 'pv0', 'consts'])
    out = mxo

    S = Sched(nc, strict=strict, G=G)
    with ExitStack() as es:
        sb = lambda name, shape, dty=F32: es.enter_context(nc.sbuf_tensor("ph%d_%s" % (G.phase + 1, name), shape, dty))
        wc = sb("wc", [128, 8, 4, 1280], BF16)
        cwb = sb("cwb", [128, 4, 1280])
        stage = sb("stage", [128, 1280])
        wp = sb("wp", [128, 8, 520], BF16)
        pvb = sb("pvb", [128, NPV])
        cst = sb("cst", [128, 4, 128])
        ident, tri, strictm, ones = (cst[:, i, :] for i in range(4))
        xt = sb("xt", [128, 8, 131], BF16)
        xbc = sb("xbc", [128, 512])
        qk = sb("qk", [128, 512])
        vv = sb("vv", [128, 256])
        zz = sb("zz", [128, 512])
        small = sb("small", [128, 8])
        ah6 = sb("ah6", [128, 6])
        t6 = sb("t6", [128, 6])
        g6 = sb("g6", [128, 6])
        sp6 = sb("sp6", [128, 6])
        beta = sb("beta", [128, 2])
        ss4 = sb("ss4", [128, 8])
        rn4 = sb("rn4", [128, 8])
        junk = sb("junk", [128, 256])
        kb = sb("kb", [128, 256])
        Rr = [sb("R%d" % h, [128, 256]) for h in range(2)]
        xd = sb("xd", [128, 256])
        gc6 = sb("gc6", [128, 6])
        gl6 = sb("gl6", [128, 6])
        egc6 = sb("egc6", [128, 6])
        egl6 = sb("egl6", [128, 6])
        kt6 = sb("kt6", [128, 6])
        triG = sb("triG", [128, 6, 128])
        decT = sb("decT", [128, 6, 128])
        T1 = sb("T1", [128, 4, 128])
        T2 = sb("T2", [128, 4, 128])
        NTm = [[sb("NT%d_%d" % (h, i), [128, 128]) for i in range(2)] for h in range(2)]
        Nm = [[sb("N%d_%d" % (h, i), [128, 128]) for i in range(2)] for h in range(2)]
        attnT = sb("attnT", [128, 6, 128])
        CBT = sb("CBT", [128, 128])
        tmpA = [sb("tmpA%d" % h, [128, 128]) for h in range(6)]
        wT = [sb("wT%d" % h, [128, 128]) for h in range(2)]
        vnew = [sb("vnew%d" % h, [128, 128]) for h in range(2)]
        ktail = [sb("ktail%d" % h, [128, 128]) for h in range(6)]
        Sg = [sb("Sg%d" % h, [128, 128]) for h in range(2)]
        Ss = [sb("Ss%d" % h, [128, 64]) for h in range(4)]
        oo = sb("oo", [128, 512])
        outt = sb("outt", [128, 512], BF16)
        ps = G.ps
        rr = [0]

        def newps():
            rr[0] = (rr[0] + 1) % 6
            return 2 + rr[0]

        V = lambda fn, r, w: S.op('vector', fn, r, w)
        A = lambda fn, r, w: S.op('scalar', fn, r, w)
        PE = lambda fn, r, w: S.op('tensor', fn, r, w, acc=True)

        S.op('sync', lambda e: e.dma_start(out=cst[:], in_=consts.rearrange("c p f -> p c f")), writes=['cst'], dma=True)
        S.op('sync', lambda e: e.dma_start(out=pvb[:], in_=pv.partition_broadcast(128)), writes=['pvb'], dma=True)
        S.op('sync', lambda e: e.dma_start(out=cwb[:].rearrange("p a c -> p (a c)"), in_=cw.partition_broadcast(128)), writes=['cwb'], dma=True)
        S.op('gpsimd', lambda e: e.dma_start(out=wp[:], in_=Wp.rearrange("(k p) c -> p k c", p=128)), writes=['wp'], dma=True)
        for k in range(8):
            S.op('sync', lambda e, k=k: e.dma_start(out=stage[:], in_=Wc[k * 128:(k + 1) * 128, :]), writes=['stage'], dma=True)
            for j in range(4):
                V(lambda e, k=k, j=j: e.tensor_tensor(out=wc[:, k, j, :], in0=stage[:], in1=cwb[:, j, :], op=ALU.mult),
                  ['stage', 'cwb'], [('wc', k, j)])
        wc_keys = [('wc', k, j) for k in range(8) for j in range(4)]
        A(lambda e: e.activation(out=ah6[:], in_=pvb[:, 518:524], func=AF.Exp), ['pvb'], ['ah6'])
        V(lambda e: e.tensor_scalar(out=ah6[:], in0=ah6[:], scalar1=-1.0, scalar2=None, op0=ALU.mult), ['ah6'], ['ah6'])
        for h in range(2):
            V(lambda e, h=h: e.memset(Sg[h][:], 0.0), [], [('Sg', h)])
        for h in range(4):
            V(lambda e, h=h: e.memset(Ss[h][:], 0.0), [], [('Ss', h)])

        for t in range(NTL):
            t0 = t * 128
            S.op('gpsimd', lambda e, t0=t0: e.dma_start(out=xt[:], in_=xTp.rearrange("(k p) t -> p k t", p=128)[:, :, t0:t0 + 131]),
                 writes=['xt'], dma=True)
            blocks = [(0, 512), (512, 1024), (1024, 1280)]
            for bi, (c0, c1) in enumerate(blocks):
                b = bi % 2
                n = 0
                for j in range(4):
                    for k in range(8):
                        PE(lambda e, b=b, c0=c0, c1=c1, j=j, k=k, n=n: e.matmul(ps[b][:, 0:c1 - c0], lhsT=xt[:, k, j:j + 128], rhs=wc[:, k, j, c0:c1],
                                                                                start=(n == 0), stop=(n == 31)),
                           ['xt', ('wc', k, j)], [('ps', b)])
                        n += 1
                if bi == 0:
                    V(lambda e, b=b: e.tensor_tensor(out=xbc[:], in0=ps[b][:], in1=pvb[:, 0:512], op=ALU.add), [('ps', b), 'pvb'], ['xbc'])
                    A(lambda e: e.activation(out=xbc[:], in_=xbc[:], func=AF.Silu), ['xbc'], ['xbc'])
                elif bi == 1:
                    A(lambda e, b=b: e.activation(out=qk[:], in_=ps[b][:], func=AF.Silu), [('ps', b)], ['qk'])
                else:
                    A(lambda e, b=b: e.activation(out=vv[:], in_=ps[b][:, 0:256], func=AF.Silu), [('ps', b)], ['vv'])
            for bi, (c0, c1) in enumerate([(0, 512), (512, 520)]):
                b = (bi + 1) % 2
                for k in range(8):
                    PE(lambda e, b=b, c0=c0, c1=c1, k=k: e.matmul(ps[b][:, 0:c1 - c0], lhsT=xt[:, k, 3:131], rhs=wp[:, k, c0:c1],
                                                                  start=(k == 0), stop=(k == 7)),
                       ['xt', 'wp'], [('ps', b)])
                if bi == 0:
                    A(lambda e, b=b: e.activation(out=zz[:], in_=ps[b][:], func=AF.Silu), [('ps', b)], ['zz'])
                else:
                    V(lambda e, b=b: e.tensor_copy(out=small[:], in_=ps[b][:, 0:8]), [('ps', b)], ['small'])
            V(lambda e: e.tensor_tensor(out=t6[:], in0=small[:, 0:6], in1=pvb[:, 512:518], op=ALU.add), ['small', 'pvb'], ['t6'])
            A(lambda e: e.activation(out=t6[:], in_=t6[:], func=AF.Exp), ['t6'], ['t6'])
            V(lambda e: e.tensor_scalar(out=t6[:], in0=t6[:], scalar1=1.0, scalar2=None, op0=ALU.add), ['t6'], ['t6'])
            A(lambda e: e.activation(out=sp6[:], in_=t6[:], func=AF.Ln), ['t6'], ['sp6'])
            V(lambda e: e.tensor_tensor(out=g6[:], in0=sp6[:], in1=ah6[:], op=ALU.mult), ['sp6', 'ah6'], ['g6'])
            A(lambda e: e.activation(out=beta[:], in_=small[:, 6:8], func=AF.Sigmoid), ['small'], ['beta'])
            for i in range(4):
                A(lambda e, i=i: e.activation(out=junk[:, 0:128], in_=qk[:, i * 128:(i + 1) * 128], func=AF.Square, accum_out=ss4[:, i:i + 1]),
                  ['qk'], ['junk', ('ss4', i)])
            V(lambda e: e.tensor_scalar(out=ss4[:, 0:4], in0=ss4[:, 0:4], scalar1=NORM_EPS, scalar2=None, op0=ALU.add),
              [('ss4', i) for i in range(4)], [('ss4', i) for i in range(4)])
            A(lambda e: e.activation(out=ss4[:, 0:4], in_=ss4[:, 0:4], func=AF.Sqrt), [('ss4', i) for i in range(4)], [('ss4', i) for i in range(4)])
            V(lambda e: e.reciprocal(out=rn4[:, 0:4], in_=ss4[:, 0:4]), [('ss4', i) for i in range(4)], ['rn4'])
            for i in range(4):
                sc = 128.0 ** -0.5 if i < 2 else 1.0
                V(lambda e, i=i, sc=sc: e.tensor_scalar(out=qk[:, i * 128:(i + 1) * 128], in0=qk[:, i * 128:(i + 1) * 128],
                                                        scalar1=rn4[:, i:i + 1], scalar2=sc, op0=ALU.mult, op1=ALU.mult),
                  ['qk', 'rn4'], ['qk'])
            p1 = newps()
            PE(lambda e, p1=p1: e.matmul(ps[p1][:, 0:6], lhsT=tri, rhs=g6[:], start=True, stop=True), ['cst', 'g6'], [('ps', p1)])
            PE(lambda e, p1=p1: e.matmul(ps[p1][:, 8:14], lhsT=ones, rhs=g6[:], start=True, stop=True), ['cst', 'g6'], [('ps', p1)])
            V(lambda e, p1=p1: e.tensor_copy(out=gc6[:], in_=ps[p1][:, 0:6]), [('ps', p1)], ['gc6'])
            V(lambda e, p1=p1: e.tensor_copy(out=gl6[:], in_=ps[p1][:, 8:14]), [('ps', p1)], ['gl6'])
            A(lambda e: e.activation(out=egc6[:], in_=gc6[:], func=AF.Exp), ['gc6'], ['egc6'])
            A(lambda e: e.activation(out=egl6[:], in_=gl6[:], func=AF.Exp), ['gl6'], ['egl6'])
            V(lambda e: e.tensor_tensor(out=kt6[:], in0=gl6[:], in1=gc6[:], op=ALU.subtract), ['gl6', 'gc6'], ['kt6'])
            A(lambda e: e.activation(out=kt6[:], in_=kt6[:], func=AF.Exp), ['kt6'], ['kt6'])
            for grp, hs in enumerate([(0, 1, 2, 3), (4, 5)]):
                for h in hs:
                    V(lambda e, h=h: e.tensor_scalar(out=triG[:, h, :], in0=tri, scalar1=g6[:, h:h + 1], scalar2=None, op0=ALU.mult),
                      ['cst', 'g6'], [('triG', grp)])
                pd = newps()
                n0, n1 = hs[0], hs[-1] + 1
                PE(lambda e, pd=pd, n0=n0, n1=n1: e.matmul(ps[pd][:, 0:(n1 - n0) * 128], lhsT=ones, rhs=triG[:, n0:n1, :].rearrange("p a b -> p (a b)"),
                                                           start=True, stop=True), ['cst', ('triG', grp)], [('ps', pd)])
                for h in hs:
                    V(lambda e, h=h, pd=pd, n0=n0: e.tensor_scalar(out=decT[:, h, :], in0=ps[pd][:, (h - n0) * 128:(h - n0 + 1) * 128],
                                                                   scalar1=gc6[:, h:h + 1], scalar2=0.0, op0=ALU.subtract, op1=ALU.min),
                      [('ps', pd), 'gc6'], [('decT', h)])
                    A(lambda e, h=h: e.activation(out=decT[:, h, :], in_=decT[:, h, :], func=AF.Exp), [('decT', h)], [('decT', h)])
                    V(lambda e, h=h: e.tensor_tensor(out=decT[:, h, :], in0=decT[:, h, :], in1=tri, op=ALU.mult), [('decT', h), 'cst'], [('decT', h)])
            for h in range(2):
                V(lambda e, h=h: e.tensor_scalar(out=kb[:, h * 128:(h + 1) * 128], in0=qk[:, 256 + h * 128:256 + (h + 1) * 128],
                                                 scalar1=beta[:, h:h + 1], scalar2=None, op0=ALU.mult), ['qk', 'beta'], [('kb', h)])
                V(lambda e, h=h: e.tensor_scalar(out=Rr[h][:, 0:128], in0=vv[:, h * 128:(h + 1) * 128], scalar1=beta[:, h:h + 1], scalar2=None,
                                                 op0=ALU.mult), ['vv', 'beta'], [('R', h)])
                V(lambda e, h=h: e.tensor_scalar(out=Rr[h][:, 128:256], in0=kb[:, h * 128:(h + 1) * 128], scalar1=egc6[:, 4 + h:5 + h], scalar2=None,
                                                 op0=ALU.mult), [('kb', h), 'egc6'], [('R', h)])
            for h in range(4):
                V(lambda e, h=h: e.tensor_scalar(out=xd[:, h * 64:(h + 1) * 64], in0=xbc[:, h * 64:(h + 1) * 64], scalar1=sp6[:, h:h + 1], scalar2=None,
                                                 op0=ALU.mult), ['xbc', 'sp6'], [('xd', h)])
            pa, pb = newps(), newps()
            srcs1 = [(qk, 256, 'qk'), (qk, 384, 'qk'), (qk, 0, 'qk'), (qk, 128, 'qk')]
            srcs2 = [(kb, 0, ('kb', 0)), (kb, 128, ('kb', 1)), (xbc, 256, 'xbc'), (xbc, 384, 'xbc')]
            for pp, srcs, dst, dk in [(pa, srcs1, T1, 'T1'), (pb, srcs2, T2, 'T2')]:
                for i, (src, c0, key) in enumerate(srcs):
                    PE(lambda e, pp=pp, i=i, src=src, c0=c0: e.transpose(out=ps[pp][:, i * 128:(i + 1) * 128], in_=src[:, c0:c0 + 128], identity=ident),
                       [key, 'cst'], [('ps', pp)])
                A(lambda e, pp=pp, dst=dst: e.activation(out=dst[:].rearrange("p a b -> p (a b)"), in_=ps[pp][:], func=AF.Identity), [('ps', pp)], [dk])
            for h in range(2):
                pn = newps()
                PE(lambda e, pn=pn, h=h: e.matmul(ps[pn][:, 0:128], lhsT=T1[:, h, :], rhs=T2[:, h, :], start=True, stop=True), ['T1', 'T2'], [('ps', pn)])
                PE(lambda e, pn=pn, h=h: e.matmul(ps[pn][:, 128:256], lhsT=T1[:, h, :], rhs=T1[:, 2 + h, :], start=True, stop=True), ['T1'], [('ps', pn)])
                V(lambda e, pn=pn, h=h: e.tensor_tensor(out=tmpA[4 + h][:], in0=ps[pn][:, 0:128], in1=decT[:, 4 + h, :], op=ALU.mult),
                  [('ps', pn), ('decT', 4 + h)], [('tmpA', 4 + h)])
                V(lambda e, h=h: e.scalar_tensor_tensor(out=NTm[h][0][:], in0=tmpA[4 + h][:], scalar=-1.0, in1=strictm, op0=ALU.mult, op1=ALU.mult),
                  [('tmpA', 4 + h), 'cst'], [('NT', h, 0)])
                V(lambda e, pn=pn, h=h: e.tensor_tensor(out=attnT[:, 4 + h, :], in0=ps[pn][:, 128:256], in1=decT[:, 4 + h, :], op=ALU.mult),
                  [('ps', pn), ('decT', 4 + h)], [('attnT', 4 + h)])
                pt = newps()
                PE(lambda e, pt=pt, h=h: e.transpose(out=ps[pt][:, 0:128], in_=NTm[h][0][:], identity=ident), [('NT', h, 0), 'cst'], [('ps', pt)])
                A(lambda e, pt=pt, h=h: e.activation(out=Nm[h][0][:], in_=ps[pt][:, 0:128], func=AF.Identity), [('ps', pt)], [('N', h, 0)])
            for lvl in range(7):
                cur = lvl % 2
                nx = 1 - cur
                for h in range(2):
                    pr = newps()
                    PE(lambda e, pr=pr, cur=cur, h=h: e.matmul(ps[pr][:, 0:256], lhsT=NTm[h][cur][:], rhs=Rr[h][:], start=True, stop=True),
                       [('NT', h, cur), ('R', h)], [('ps', pr)])
                    if lvl < 6:
                        PE(lambda e, pr=pr, cur=cur, h=h: e.matmul(ps[pr][:, 256:384], lhsT=NTm[h][cur][:], rhs=Nm[h][cur][:], start=True, stop=True),
                           [('NT', h, cur), ('N', h, cur)], [('ps', pr)])
                        PE(lambda e, pr=pr, cur=cur, h=h: e.matmul(ps[pr][:, 384:512], lhsT=Nm[h][cur][:], rhs=NTm[h][cur][:], start=True, stop=True),
                           [('NT', h, cur), ('N', h, cur)], [('ps', pr)])
                    V(lambda e, pr=pr, h=h: e.tensor_tensor(out=Rr[h][:], in0=Rr[h][:], in1=ps[pr][:, 0:256], op=ALU.add), [('R', h), ('ps', pr)], [('R', h)])
                    if lvl < 6:
                        A(lambda e, pr=pr, nx=nx, h=h: e.activation(out=Nm[h][nx][:], in_=ps[pr][:, 256:384], func=AF.Identity), [('ps', pr)], [('N', h, nx)])
                        V(lambda e, pr=pr, nx=nx, h=h: e.tensor_copy(out=NTm[h][nx][:], in_=ps[pr][:, 384:512]), [('ps', pr)], [('NT', h, nx)])
            pc = newps()
            PE(lambda e, pc=pc: e.matmul(ps[pc][:, 0:128], lhsT=T2[:, 2, :], rhs=T2[:, 3, :], start=True, stop=True), ['T2'], [('ps', pc)])
            A(lambda e, pc=pc: e.activation(out=CBT[:], in_=ps[pc][:, 0:128], func=AF.Identity), [('ps', pc)], ['CBT'])
            for h in range(4):
                V(lambda e, h=h: e.tensor_tensor(out=attnT[:, h, :], in0=CBT[:], in1=decT[:, h, :], op=ALU.mult), ['CBT', ('decT', h)], [('attnT', h)])
                V(lambda e, h=h: e.tensor_scalar(out=ktail[h][:], in0=xbc[:, 256:384], scalar1=kt6[:, h:h + 1], scalar2=None, op0=ALU.mult),
                  ['xbc', 'kt6'], [('ktail', h)])
            for h in range(2):
                V(lambda e, h=h: e.tensor_scalar(out=ktail[4 + h][:], in0=qk[:, 256 + h * 128:256 + (h + 1) * 128], scalar1=kt6[:, 4 + h:5 + h], scalar2=None,
                                                 op0=ALU.mult), ['qk', 'kt6'], [('ktail', 4 + h)])
            pos = {}
            for h in range(4):
                po = newps()
                pos[h] = po
                PE(lambda e, po=po, h=h: e.matmul(ps[po][:, 0:64], lhsT=T2[:, 3, :], rhs=Ss[h][:], start=True, stop=True), ['T2', ('Ss', h)], [('ps', po)])
                PE(lambda e, po=po, h=h: e.matmul(ps[po][:, 64:128], lhsT=attnT[:, h, :], rhs=xd[:, h * 64:(h + 1) * 64], start=True, stop=True),
                   [('attnT', h), ('xd', h)], [('ps', po)])
                PE(lambda e, po=po, h=h: e.matmul(ps[po][:, 128:192], lhsT=ktail[h][:], rhs=xd[:, h * 64:(h + 1) * 64], start=True, stop=True),
                   [('ktail', h), ('xd', h)], [('ps', po)])
            for h in range(4):
                po = pos[h]
                A(lambda e, po=po, h=h: e.activation(out=tmpA[h][:, 0:64], in_=ps[po][:, 64:128], func=AF.Identity), [('ps', po)], [('tmpA', h)])
                V(lambda e, po=po, h=h: e.scalar_tensor_tensor(out=oo[:, h * 64:(h + 1) * 64], in0=ps[po][:, 0:64], scalar=egc6[:, h:h + 1], in1=tmpA[h][:, 0:64],
                                                               op0=ALU.mult, op1=ALU.add), [('ps', po), 'egc6', ('tmpA', h)], [('oo', 0)])
                V(lambda e, po=po, h=h: e.scalar_tensor_tensor(out=Ss[h][:], in0=Ss[h][:], scalar=egl6[:, h:h + 1], in1=ps[po][:, 128:192],
                                                               op0=ALU.mult, op1=ALU.add), [('Ss', h), 'egl6', ('ps', po)], [('Ss', h)])
                V(lambda e, h=h: e.scalar_tensor_tensor(out=oo[:, h * 64:(h + 1) * 64], in0=xbc[:, h * 64:(h + 1) * 64], scalar=pvb[:, 524 + h:525 + h],
                                                        in1=oo[:, h * 64:(h + 1) * 64], op0=ALU.mult, op1=ALU.add), ['xbc', 'pvb', ('oo', 0)], [('oo', 0)])
            for h in range(2):
                pw = newps()
                PE(lambda e, pw=pw, h=h: e.transpose(out=ps[pw][:, 0:128], in_=Rr[h][:, 128:256], identity=ident), [('R', h), 'cst'], [('ps', pw)])
                A(lambda e, pw=pw, h=h: e.activation(out=wT[h][:], in_=ps[pw][:, 0:128], func=AF.Identity), [('ps', pw)], [('wT', h)])
            pvs = {}
            for h in range(2):
                pv_ = newps()
                pvs[h] = pv_
                PE(lambda e, pv_=pv_, h=h: e.matmul(ps[pv_][:, 0:128], lhsT=wT[h][:], rhs=Sg[h][:], start=True, stop=True), [('wT', h), ('Sg', h)], [('ps', pv_)])
                PE(lambda e, pv_=pv_, h=h: e.matmul(ps[pv_][:, 128:256], lhsT=T1[:, 2 + h, :], rhs=Sg[h][:], start=True, stop=True), ['T1', ('Sg', h)], [('ps', pv_)])
                V(lambda e, pv_=pv_, h=h: e.tensor_tensor(out=vnew[h][:], in0=Rr[h][:, 0:128], in1=ps[pv_][:, 0:128], op=ALU.subtract),
                  [('R', h), ('ps', pv_)], [('vnew', h)])
            for h in range(2):
                pv_ = pvs[h]
                po = newps()
                PE(lambda e, po=po, h=h: e.matmul(ps[po][:, 0:128], lhsT=attnT[:, 4 + h, :], rhs=vnew[h][:], start=True, stop=True),
                   [('attnT', 4 + h), ('vnew', h)], [('ps', po)])
                PE(lambda e, po=po, h=h: e.matmul(ps[po][:, 128:256], lhsT=ktail[4 + h][:], rhs=vnew[h][:], start=True, stop=True),
                   [('ktail', 4 + h), ('vnew', h)], [('ps', po)])
                A(lambda e, po=po, h=h: e.activation(out=tmpA[4 + h][:], in_=ps[po][:, 0:128], func=AF.Identity), [('ps', po)], [('tmpA', 4 + h)])
                V(lambda e, pv_=pv_, h=h: e.scalar_tensor_tensor(out=oo[:, 256 + h * 128:256 + (h + 1) * 128], in0=ps[pv_][:, 128:256], scalar=egc6[:, 4 + h:5 + h],
                                                                 in1=tmpA[4 + h][:], op0=ALU.mult, op1=ALU.add), [('ps', pv_), 'egc6', ('tmpA', 4 + h)], [('oo', 2 + h)])
                V(lambda e, po=po, h=h: e.scalar_tensor_tensor(out=Sg[h][:], in0=Sg[h][:], scalar=egl6[:, 4 + h:5 + h], in1=ps[po][:, 128:256],
                                                               op0=ALU.mult, op1=ALU.add), [('Sg', h), 'egl6', ('ps', po)], [('Sg', h)])
            V(lambda e: e.tensor_tensor(out=oo[:, 0:256], in0=oo[:, 0:256], in1=zz[:, 0:256], op=ALU.mult), [('oo', 0), 'zz'], [('oo', 0)])
            groups = [(0, 256, ('oo', 0), 4), (256, 384, ('oo', 2), 5), (384, 512, ('oo', 3), 6)]
            for c0, c1, key, si in groups:
                A(lambda e, c0=c0, c1=c1, si=si: e.activation(out=junk[:, 0:c1 - c0], in_=oo[:, c0:c1], func=AF.Square, scale=(1.0 / (c1 - c0)) ** 0.5,
                                                              accum_out=ss4[:, si:si + 1]),
                  [key], ['junk', ('ss4', si)])
                V(lambda e, si=si: e.tensor_scalar(out=ss4[:, si:si + 1], in0=ss4[:, si:si + 1], scalar1=NORM_EPS, scalar2=None, op0=ALU.add),
                  [('ss4', si)], [('ss4', si)])
                A(lambda e, si=si: e.activation(out=ss4[:, si:si + 1], in_=ss4[:, si:si + 1], func=AF.Sqrt), [('ss4', si)], [('ss4', si)])
                V(lambda e, si=si: e.reciprocal(out=rn4[:, si:si + 1], in_=ss4[:, si:si + 1]), [('ss4', si)], [('rn4b', si)])
                nw0 = 528 if c0 == 0 else 784
                V(lambda e, c0=c0, c1=c1, si=si, nw0=nw0: e.scalar_tensor_tensor(out=outt[:, c0:c1], in0=oo[:, c0:c1], scalar=rn4[:, si:si + 1],
                                                                                 in1=pvb[:, nw0:nw0 + (c1 - c0)], op0=ALU.mult, op1=ALU.mult),
                  [key, ('rn4b', si), 'pvb'], [('outt', c0)])
            V(lambda e: e.tensor_tensor(out=outt[:, 256:512], in0=outt[:, 256:512], in1=zz[:, 256:512], op=ALU.mult),
              [('outt', 256), ('outt', 384), 'zz'], [('outt', 256), ('outt', 384)])
            S.op('sync', lambda e, t0=t0: e.dma_start(out=out[t0:t0 + 128, :], in_=outt[:]), reads=[('outt', 0), ('outt', 256), ('outt', 384)],
                 writes=[('out', t)], dma=True, dkey='outst')
        S.emit()
    return S


def mix0_consts():
    i = np.arange(128)
    ident = np.eye(128, dtype=np.float32)
    tri = (i[:, None] <= i[None, :]).astype(np.float32)
    strict = (i[:, None] < i[None, :]).astype(np.float32)
    ones = np.ones((128, 128), np.float32)
    return np.stack([ident, tri, strict, ones])


def mix0_inputs(xb, g, w_in, ssd_conv_w, ssd_conv_b, ssd_dt_bias, ssd_a_log, ssd_d, ssd_norm_w, gdn_conv_w, gdn_a_log, gdn_dt_bias, gdn_norm_w):
    T = xb.shape[0]
    xTp = np.zeros((D, T + 3), np.float32)
    xTp[:, 3:] = xb.T
    o_zs, o_xbc, o_dt, o_qkv, o_zg, o_b, o_a = np.cumsum([0, 1024, 2048, 16, 3072, 1024, 8])
    r = lambda a, n: np.arange(a, a + n)
    ssd_x = r(g * 256, 256)
    ssd_B = r(1024 + g * 128, 128)
    ssd_C = r(1536 + g * 128, 128)
    gq = r(g * 256, 256)
    gk = r(1024 + g * 256, 256)
    gv = r(2048 + g * 256, 256)
    conv_cols = np.concatenate([o_xbc + ssd_x, o_xbc + ssd_B, o_xbc + ssd_C, o_qkv + gq, o_qkv + gk, o_qkv + gv])
    Wc = w_in[:, conv_cols]
    cw = np.concatenate([ssd_conv_w[:, np.concatenate([ssd_x, ssd_B, ssd_C])], gdn_conv_w[:, np.concatenate([gq, gk, gv])]], axis=1)
    plain_cols = np.concatenate([o_zs + r(g * 256, 256), o_zg + r(g * 256, 256), o_dt + r(g * 4, 4), o_a + r(g * 2, 2), o_b + r(g * 2, 2)])
    Wp = w_in[:, plain_cols]
    pv = np.concatenate([ssd_conv_b[np.concatenate([ssd_x, ssd_B, ssd_C])], ssd_dt_bias[g * 4:g * 4 + 4], gdn_dt_bias[g * 2:g * 2 + 2],
                         ssd_a_log[g * 4:g * 4 + 4], gdn_a_log[g * 2:g * 2 + 2], ssd_d[g * 4:g * 4 + 4], ssd_norm_w[g * 256:(g + 1) * 256], gdn_norm_w])
    assert pv.shape[0] == NPV
    return {"xTp": xTp, "Wc": np.ascontiguousarray(Wc), "cw": np.ascontiguousarray(cw.reshape(-1)), "Wp": np.ascontiguousarray(Wp),
            "pv": np.ascontiguousarray(pv.astype(np.float32)), "consts": mix0_consts()}


D = 1024
NORM_EPS = 1e-6
RW_GN_EPS = 64e-5
NEG = -1.0e30


def phase_mix1(nc, G, T, Wd, x1o, x1g, mxo, strict=True, do_rwkv=True, do_moba=True):
    NTL = T // 128
    Wr, mu, Wm, pv, lw2, la2, lg2, consts, esel_d, cmask_d, kTs, v1s = (Wd[k] for k in
        ['Wr', 'mu', 'Wm', 'pv1', 'lw2', 'la2', 'lg2', 'consts', 'esel', 'cmask', 'kTs', 'v1s'])
    out = mxo

    S = Sched(nc, strict=strict, G=G)
    with ExitStack() as es:
        sb = lambda name, shape, dty=F32: es.enter_context(nc.sbuf_tensor("ph%d_%s" % (G.phase + 1, name), shape, dty))
        wr = sb("wr", [128, 8, 2, 1056], BF16)
        wm = sb("wm", [128, 8, 768], BF16)
        mub = sb("mub", [128, 1056])
        stage = sb("stage", [128, 1056])
        stage2 = sb("stage2", [128, 1056])
        pvb = sb("pvb", [128, 1792])
        cst = sb("cst", [128, 4, 128])
        ident, tri, strictm, ones = (cst[:, i, :] for i in range(4))
        identb = sb("identb", [128, 128], BF16)
        w2sb = sb("w2sb", [64, 256])
        a2sb = sb("a2sb", [64, 256])
        g2sb = sb("g2sb", [128, 256])
        g2sb2 = sb("g2sb2", [32, 256])
        xt = sb("xt", [128, 8, 129], BF16)
        rk = sb("rk", [128, 512])
        vv = sb("vv", [128, 256])
        L1 = sb("L1", [64, 128])
        L1b = sb("L1b", [64, 128])
        sgT = sb("sgT", [128, 128])
        sgT2 = sb("sgT2", [32, 128])
        lw = sb("lw", [128, 256])
        aa = sb("aa", [128, 256])
        gate = sb("gate", [128, 256])
        kk = sb("kk", [128, 256])
        kmod = sb("kmod", [128, 256])
        bq = sb("bq", [128, 256])
        ss = sb("ss", [128, 8])
        rn = sb("rn", [128, 8])
        junk = sb("junk", [128, 256])
        LDs = sb("LDs", [128, 256])
        E = [sb("E%d" % i, [128, 256]) for i in range(4)]
        d3 = sb("d3", [128, 256])
        rt = sb("rt", [128, 256])
        at = sb("at", [128, 256])
        bt = sb("bt", [128, 256])
        kt = sb("kt", [128, 256])
        bh = sb("bh", [128, 256])
        kh = sb("kh", [128, 256])
        XT = [sb("XT%d" % h, [64, 512]) for h in range(4)]
        AT = [sb("AT%d" % h, [128, 256]) for h in range(4)]
        AakT = [sb("AakT%d" % h, [128, 128]) for h in range(4)]
        NTm = [[sb("NT%d_%d" % (h, i), [128, 128]) for i in range(2)] for h in range(4)]
        Nm = [[sb("N%d_%d" % (h, i), [128, 128]) for i in range(2)] for h in range(4)]
        Rr = [sb("R%d" % h, [128, 128]) for h in range(4)]
        Wmm = sb("Wmm", [128, 128])
        WmT = [sb("WmT%d" % h, [64, 128]) for h in range(4)]
        Up = sb("Up", [128, 256])
        Hp = [sb("Hp%d" % h, [64, 64]) for h in range(4)]
        DC = [sb("DC%d" % h, [64, 1]) for h in range(4)]
        yraw = sb("yraw", [128, 256])
        stats = sb("stats", [128, 4, 6])
        mv = sb("mv", [128, 4, 2])
        rkb = sb("rkb", [128, 4])
        outr = sb("outr", [128, 256])
        outrb = sb("outrb", [128, 256], BF16)
        xtok = sb("xtok", [128, 1024])
        esel = sb("eselsb", [64, 64, 128], BF16)
        cmask = sb("cmasksb", [128, 4, 512], BF16)
        qTf = sb("qTf", [128, 256])
        qg = [sb("qg%d" % h, [128, 512], BF16) for h in range(2)]
        kTt = sb("kTt", [128, 256], BF16)
        v1t = sb("v1t", [128, 2, 129], BF16)
        kpart = sb("kpart", [128, 2])
        kacc = sb("kacc", [128, 2])
        kmT = sb("kmT", [128, 2, 64])
        gm = [sb("gm%d" % h, [128, 64]) for h in range(2)]
        m8 = [sb("m8_%d" % h, [128, 8]) for h in range(2)]
        bias = [sb("bias%d" % h, [128, 64]) for h in range(2)]
        biasT = [sb("biasT%d" % h, [64, 512], BF16) for h in range(2)]
        kcb = [sb("kcb%d" % i, [128, 512], BF16) for i in range(2)]
        vcb = [sb("vcb%d" % i, [128, 4, 129], BF16) for i in range(2)]
        PT = [sb("PT%d" % i, [128, 512], BF16) for i in range(2)]
        rec = sb("rec", [128, 4])
        mo = sb("mo", [128, 4, 256], BF16)
        ps = G.ps
        rr = [0]

        def newps():
            rr[0] = (rr[0] + 1) % 2
            return 6 + rr[0]

        rr2 = [0]

        def newps_r():
            rr2[0] = (rr2[0] + 1) % 6
            return 2 + rr2[0]

        V = lambda fn, r, w: S.op('vector', fn, r, w)
        A = lambda fn, r, w: S.op('scalar', fn, r, w)
        PE = lambda fn, r, w: S.op('tensor', fn, r, w, acc=True)

        TQ = x1o.shape[0]
        XR = 256
        NCHX = TQ // XR
        x1g4 = x1g.rearrange("(i r t) c -> i r t c", i=NCHX, r=4)
        for i in range(NCHX):
            S.op('gpsimd', lambda e, i=i: e.collective_compute('AllGather', ALU.bypass, replica_groups=[[0, 1, 2, 3], [4, 5, 6, 7]],
                                                               ins=[x1o[i * XR:(i + 1) * XR, :]], outs=[x1g[i * 4 * XR:(i + 1) * 4 * XR, :]]),
                 writes=[('x1g', i)], cc=True, dkey='ccx')
        S.op('sync', lambda e: e.dma_start(out=cst[:], in_=consts.rearrange("c p f -> p c f")), writes=['cst'], dma=True)
        S.op('sync', lambda e: e.dma_start(out=pvb[:], in_=pv.partition_broadcast(128)), writes=['pvb'], dma=True)
        S.op('sync', lambda e: e.dma_start(out=mub[:], in_=mu.partition_broadcast(128)), writes=['mub'], dma=True)
        S.op('sync', lambda e: e.dma_start(out=w2sb[:], in_=lw2), writes=['w2sb'], dma=True)
        S.op('sync', lambda e: e.dma_start(out=a2sb[:], in_=la2), writes=['a2sb'], dma=True)
        S.op('sync', lambda e: e.dma_start(out=g2sb[:], in_=lg2[0:128, :]), writes=['g2sb'], dma=True)
        S.op('sync', lambda e: e.dma_start(out=g2sb2[:], in_=lg2[128:160, :]), writes=['g2sb2'], dma=True)
        S.op('gpsimd', lambda e: e.dma_start(out=wm[:], in_=Wm.rearrange("(k p) c -> p k c", p=128)), writes=['wm'], dma=True)
        S.op('gpsimd', lambda e: e.dma_start(out=esel[:].rearrange("p a b -> p (a b)"), in_=esel_d), writes=['esel'], dma=True)
        S.op('gpsimd', lambda e: e.dma_start(out=cmask[:].rearrange("p a b -> p (a b)"), in_=cmask_d), writes=['cmask'], dma=True)
        V(lambda e: e.tensor_copy(out=identb[:], in_=ident), ['cst'], ['identb'])
        for k in range(8):
            S.op('sync', lambda e, k=k: e.dma_start(out=stage[:], in_=Wr[k * 128:(k + 1) * 128, :]), writes=['stage'], dma=True)
            V(lambda e: e.tensor_tensor(out=stage2[:], in0=stage[:], in1=mub[:], op=ALU.mult), ['stage', 'mub'], ['stage2'])
            V(lambda e, k=k: e.tensor_copy(out=wr[:, k, 0, :], in_=stage2[:]), ['stage2'], [('wr', k)])
            V(lambda e, k=k: e.tensor_tensor(out=wr[:, k, 1, :], in0=stage[:], in1=stage2[:], op=ALU.subtract), ['stage', 'stage2'], [('wr', k)])
        for p in range(4):
            V(lambda e, p=p: e.memset(Hp[p][:], 0.0), [], [('Hp', p)])
        V(lambda e: e.memset(kmT[:], 0.0), [], ['kmT'])
        V(lambda e: e.memset(v1t[:], 1.0), [], ['v1t'])

        def sumsq_rn(src, skey, n, width, scale2, eps, col0):
            for i in range(n):
                A(lambda e, i=i: e.activation(out=junk[:, 0:width], in_=src[:, i * width:(i + 1) * width], func=AF.Square, scale=scale2 ** 0.5,
                                              accum_out=ss[:, col0 + i:col0 + i + 1]), [skey], ['junk', ('ss', col0 + i)])
            keys = [('ss', col0 + i) for i in range(n)]
            V(lambda e: e.tensor_scalar(out=ss[:, col0:col0 + n], in0=ss[:, col0:col0 + n], scalar1=eps, scalar2=None, op0=ALU.add), keys, keys)
            A(lambda e: e.activation(out=ss[:, col0:col0 + n], in_=ss[:, col0:col0 + n], func=AF.Sqrt), keys, keys)
            V(lambda e: e.reciprocal(out=rn[:, col0:col0 + n], in_=ss[:, col0:col0 + n]), keys, [('rn', col0)])

        for t in range(NTL):
            t0 = t * 128
            S.op('sync', lambda e, t0=t0: e.dma_start(out=xtok[:], in_=x1g4[(t0 % TQ) // XR, t0 // TQ, (t0 % TQ) % XR:(t0 % TQ) % XR + 128, :]),
                 reads=[('x1g', i_) for i_ in range(NCHX)], writes=['xtok'], dma=True)
            if t == 0:
                V(lambda e: e.memset(xt[:, :, 0:1], 0.0), [], ['xt'])
            else:
                V(lambda e: e.tensor_copy(out=xt[:, :, 0:1], in_=xt[:, :, 128:129]), ['xt'], ['xt'])
            for hh in range(2):
                px0 = newps()
                for c4 in range(4):
                    c = hh * 4 + c4
                    PE(lambda e, px0=px0, c4=c4, c=c: e.transpose(out=ps[px0][:, c4 * 128:(c4 + 1) * 128], in_=xtok[:, c * 128:(c + 1) * 128], identity=ident),
                       ['xtok', 'cst'], [('ps', px0)])
                A(lambda e, px0=px0, hh=hh: e.activation(out=xt[:, hh * 4:(hh + 1) * 4, 1:129], in_=ps[px0][:].rearrange("p (c t) -> p c t", c=4), func=AF.Identity),
                  [('ps', px0)], ['xt'])
            if do_rwkv:
                for bi, (c0, c1) in enumerate([(0, 512), (512, 768)]):
                    n = 0
                    for j in range(2):
                        for k in range(8):
                            PE(lambda e, bi=bi, c0=c0, c1=c1, j=j, k=k, n=n: e.matmul(ps[bi][:, 0:c1 - c0], lhsT=xt[:, k, j:j + 128], rhs=wr[:, k, j, c0:c1],
                                                                                      start=(n == 0), stop=(n == 15)), ['xt', ('wr', k)], [('ps', bi)])
                            n += 1
                A(lambda e: e.activation(out=rk[:], in_=ps[0][:], func=AF.Identity), [('ps', 0)], ['rk'])
                V(lambda e: e.tensor_copy(out=vv[:], in_=ps[1][:, 0:256]), [('ps', 1)], ['vv'])
                for bi, (c0, c1) in enumerate([(768, 832), (832, 896), (896, 1024), (1024, 1056)]):
                    b = bi % 2
                    n = 0
                    for j in range(2):
                        for k in range(8):
                            PE(lambda e, b=b, c0=c0, c1=c1, j=j, k=k, n=n: e.matmul(ps[b][0:c1 - c0, 0:128], lhsT=wr[:, k, j, c0:c1], rhs=xt[:, k, j:j + 128],
                                                                                    start=(n == 0), stop=(n == 15)), ['xt', ('wr', k)], [('ps', b)])
                            n += 1
                    if bi == 0:
                        A(lambda e, b=b: e.activation(out=L1[:], in_=ps[b][0:64, 0:128], func=AF.Tanh), [('ps', b)], [('L1', 0)])
                    elif bi == 1:
                        A(lambda e, b=b: e.activation(out=L1b[:], in_=ps[b][0:64, 0:128], func=AF.Identity), [('ps', b)], [('L1', 1)])
                    elif bi == 2:
                        A(lambda e, b=b: e.activation(out=sgT[:], in_=ps[b][:, 0:128], func=AF.Sigmoid), [('ps', b)], ['sgT'])
                    else:
                        A(lambda e, b=b: e.activation(out=sgT2[:], in_=ps[b][0:32, 0:128], func=AF.Sigmoid), [('ps', b)], ['sgT2'])
                pl = newps_r()
                PE(lambda e, pl=pl: e.matmul(ps[pl][:, 0:256], lhsT=L1[:], rhs=w2sb[:], start=True, stop=True), [('L1', 0), 'w2sb'], [('ps', pl)])
                PE(lambda e, pl=pl: e.matmul(ps[pl][:, 256:512], lhsT=L1b[:], rhs=a2sb[:], start=True, stop=True), [('L1', 1), 'a2sb'], [('ps', pl)])
                pg_ = newps_r()
                PE(lambda e, pg_=pg_: e.matmul(ps[pg_][:, 0:256], lhsT=sgT[:], rhs=g2sb[:], start=True, stop=False), ['sgT', 'g2sb'], [('ps', pg_)])
                PE(lambda e, pg_=pg_: e.matmul(ps[pg_][:, 0:256], lhsT=sgT2[:], rhs=g2sb2[:], start=False, stop=True), ['sgT2', 'g2sb2'], [('ps', pg_)])
                V(lambda e, pl=pl: e.tensor_tensor(out=lw[:], in0=ps[pl][:, 0:256], in1=pvb[:, 0:256], op=ALU.add), [('ps', pl), 'pvb'], ['lw'])
                A(lambda e: e.activation(out=lw[:], in_=lw[:], func=AF.Sigmoid), ['lw'], ['lw'])
                V(lambda e: e.tensor_scalar(out=lw[:], in0=lw[:], scalar1=-float(np.exp(-0.5)), scalar2=None, op0=ALU.mult), ['lw'], ['lw'])
                V(lambda e, pl=pl: e.tensor_tensor(out=aa[:], in0=ps[pl][:, 256:512], in1=pvb[:, 256:512], op=ALU.add), [('ps', pl), 'pvb'], ['aa'])
                A(lambda e: e.activation(out=aa[:], in_=aa[:], func=AF.Sigmoid), ['aa'], ['aa'])
                A(lambda e, pg_=pg_: e.activation(out=gate[:], in_=ps[pg_][:, 0:256], func=AF.Identity), [('ps', pg_)], ['gate'])
                V(lambda e: e.tensor_tensor(out=kk[:], in0=rk[:, 256:512], in1=pvb[:, 512:768], op=ALU.mult), ['rk', 'pvb'], ['kk'])
                sumsq_rn(kk, 'kk', 4, 64, 1.0, NORM_EPS, 0)
                for h in range(4):
                    V(lambda e, h=h: e.tensor_scalar(out=kk[:, h * 64:(h + 1) * 64], in0=kk[:, h * 64:(h + 1) * 64], scalar1=rn[:, h:h + 1], scalar2=None,
                                                     op0=ALU.mult), ['kk', ('rn', 0)], ['kk'])
                V(lambda e: e.scalar_tensor_tensor(out=kmod[:], in0=aa[:], scalar=-1.0, in1=pvb[:, 768:1024], op0=ALU.add, op1=ALU.mult), ['aa', 'pvb'], ['kmod'])
                V(lambda e: e.scalar_tensor_tensor(out=kmod[:], in0=kmod[:], scalar=1.0, in1=rk[:, 256:512], op0=ALU.add, op1=ALU.mult), ['kmod', 'rk'], ['kmod'])
                V(lambda e: e.tensor_tensor(out=bq[:], in0=kk[:], in1=aa[:], op=ALU.mult), ['kk', 'aa'], ['bq'])
                pd = newps_r()
                PE(lambda e, pd=pd: e.matmul(ps[pd][:, 0:256], lhsT=tri, rhs=lw[:], start=True, stop=True), ['cst', 'lw'], [('ps', pd)])
                PE(lambda e, pd=pd: e.matmul(ps[pd][:, 256:512], lhsT=ones, rhs=lw[:], start=True, stop=True), ['cst', 'lw'], [('ps', pd)])
                V(lambda e, pd=pd: e.tensor_copy(out=LDs[:], in_=ps[pd][:, 0:256]), [('ps', pd)], ['LDs'])
                V(lambda e, pd=pd: e.tensor_tensor(out=d3[:], in0=ps[pd][:, 256:512], in1=LDs[:], op=ALU.subtract), [('ps', pd), 'LDs'], ['d3'])
                A(lambda e: e.activation(out=E[0][:], in_=LDs[:], func=AF.Exp), ['LDs'], [('E', 0)])
                A(lambda e: e.activation(out=E[1][:], in_=LDs[:], func=AF.Exp, scale=-1.0), ['LDs'], [('E', 1)])
                A(lambda e: e.activation(out=E[2][:], in_=d3[:], func=AF.Exp), ['d3'], [('E', 2)])
                V(lambda e: e.tensor_tensor(out=d3[:], in0=LDs[:], in1=lw[:], op=ALU.subtract), ['LDs', 'lw', ('E', 2)], ['d3'])
                A(lambda e: e.activation(out=E[3][:], in_=d3[:], func=AF.Exp), ['d3'], [('E', 3)])
                V(lambda e: e.tensor_tensor(out=rt[:], in0=rk[:, 0:256], in1=E[0][:], op=ALU.mult), ['rk', ('E', 0)], ['rt'])
                V(lambda e: e.scalar_tensor_tensor(out=at[:], in0=kk[:], scalar=-1.0, in1=E[3][:], op0=ALU.mult, op1=ALU.mult), ['kk', ('E', 3)], ['at'])
                V(lambda e: e.tensor_tensor(out=bt[:], in0=bq[:], in1=E[1][:], op=ALU.mult), ['bq', ('E', 1)], ['bt'])
                V(lambda e: e.tensor_tensor(out=kt[:], in0=kmod[:], in1=E[1][:], op=ALU.mult), ['kmod', ('E', 1)], ['kt'])
                V(lambda e: e.tensor_tensor(out=bh[:], in0=bq[:], in1=E[2][:], op=ALU.mult), ['bq', ('E', 2)], ['bh'])
                V(lambda e: e.tensor_tensor(out=kh[:], in0=kmod[:], in1=E[2][:], op=ALU.mult), ['kmod', ('E', 2)], ['kh'])
                for h in range(4):
                    px = newps_r()
                    for i, (src, key) in enumerate([(at, 'at'), (rt, 'rt'), (bt, 'bt'), (kt, 'kt')]):
                        PE(lambda e, px=px, i=i, src=src, h=h: e.transpose(out=ps[px][0:64, i * 128:(i + 1) * 128], in_=src[:, h * 64:(h + 1) * 64], identity=ident),
                           [key, 'cst'], [('ps', px)])
                    A(lambda e, px=px, h=h: e.activation(out=XT[h][:], in_=ps[px][0:64, :], func=AF.Identity), [('ps', px)], [('XT', h)])
                for h in range(4):
                    pa_ = newps_r()
                    PE(lambda e, pa_=pa_, h=h: e.matmul(ps[pa_][:, 0:256], lhsT=XT[h][:, 256:384], rhs=XT[h][:, 0:256],
                                                        start=True, stop=True), [('XT', h)], [('ps', pa_)])
                    PE(lambda e, pa_=pa_, h=h: e.matmul(ps[pa_][:, 256:512], lhsT=XT[h][:, 384:512], rhs=XT[h][:, 0:256],
                                                        start=True, stop=True), [('XT', h)], [('ps', pa_)])
                    V(lambda e, pa_=pa_, h=h: e.tensor_tensor(out=NTm[h][0][:], in0=ps[pa_][:, 0:128], in1=strictm, op=ALU.mult), [('ps', pa_), 'cst'], [('NT', h, 0)])
                    V(lambda e, pa_=pa_, h=h: e.tensor_tensor(out=AT[h][:, 0:128], in0=ps[pa_][:, 128:256], in1=tri, op=ALU.mult), [('ps', pa_), 'cst'], [('AT', h)])
                    V(lambda e, pa_=pa_, h=h: e.tensor_tensor(out=AakT[h][:], in0=ps[pa_][:, 256:384], in1=strictm, op=ALU.mult), [('ps', pa_), 'cst'], [('AakT', h)])
                    V(lambda e, pa_=pa_, h=h: e.tensor_tensor(out=AT[h][:, 128:256], in0=ps[pa_][:, 384:512], in1=tri, op=ALU.mult), [('ps', pa_), 'cst'], [('AT', h)])
                for h in range(4):
                    pt = newps_r()
                    PE(lambda e, pt=pt, h=h: e.transpose(out=ps[pt][:, 0:128], in_=NTm[h][0][:], identity=ident), [('NT', h, 0), 'cst'], [('ps', pt)])
                    PE(lambda e, pt=pt, h=h: e.matmul(ps[pt][:, 128:192], lhsT=AakT[h][:], rhs=vv[:, h * 64:(h + 1) * 64], start=True, stop=True),
                       [('AakT', h), 'vv'], [('ps', pt)])
                    A(lambda e, pt=pt, h=h: e.activation(out=Nm[h][0][:], in_=ps[pt][:, 0:128], func=AF.Identity), [('ps', pt)], [('N', h, 0)])
                    A(lambda e, pt=pt, h=h: e.activation(out=Rr[h][:, 0:64], in_=ps[pt][:, 128:192], func=AF.Identity), [('ps', pt)], [('R', h)])
                    V(lambda e, h=h: e.tensor_copy(out=Rr[h][:, 64:128], in_=at[:, h * 64:(h + 1) * 64]), ['at'], [('R', h)])
                for lvl in range(7):
                    cur = lvl % 2
                    nx = 1 - cur
                    for h in range(4):
                        pr = newps_r()
                        PE(lambda e, pr=pr, cur=cur, h=h: e.matmul(ps[pr][:, 0:128], lhsT=NTm[h][cur][:], rhs=Rr[h][:], start=True, stop=True),
                           [('NT', h, cur), ('R', h)], [('ps', pr)])
                        if lvl < 6:
                            PE(lambda e, pr=pr, cur=cur, h=h: e.matmul(ps[pr][:, 128:256], lhsT=NTm[h][cur][:], rhs=Nm[h][cur][:], start=True, stop=True),
                               [('NT', h, cur), ('N', h, cur)], [('ps', pr)])
                            PE(lambda e, pr=pr, cur=cur, h=h: e.matmul(ps[pr][:, 256:384], lhsT=Nm[h][cur][:], rhs=NTm[h][cur][:], start=True, stop=True),
                               [('NT', h, cur), ('N', h, cur)], [('ps', pr)])
                        V(lambda e, pr=pr, h=h: e.tensor_tensor(out=Rr[h][:], in0=Rr[h][:], in1=ps[pr][:, 0:128], op=ALU.add), [('R', h), ('ps', pr)], [('R', h)])
                        if lvl < 6:
                            A(lambda e, pr=pr, nx=nx, h=h: e.activation(out=Nm[h][nx][:], in_=ps[pr][:, 128:256], func=AF.Identity), [('ps', pr)], [('N', h, nx)])
                            V(lambda e, pr=pr, nx=nx, h=h: e.tensor_copy(out=NTm[h][nx][:], in_=ps[pr][:, 256:384]), [('ps', pr)], [('NT', h, nx)])
                pws, pus, pys, phs = {}, {}, {}, {}
                for h in range(4):
                    pws[h] = newps_r()
                    PE(lambda e, pw_=pws[h], h=h: e.transpose(out=ps[pw_][0:64, 0:128], in_=Rr[h][:, 64:128], identity=ident), [('R', h), 'cst'], [('ps', pws[h])])
                    A(lambda e, pw_=pws[h], h=h: e.activation(out=WmT[h][:], in_=ps[pw_][0:64, 0:128], func=AF.Identity), [('ps', pws[h])], [('WmT', h)])
                for h in range(4):
                    pus[h] = newps_r()
                    PE(lambda e, pu=pus[h], h=h: e.matmul(ps[pu][:, 0:64], lhsT=WmT[h][:], rhs=Hp[h][:], start=True, stop=True), [('WmT', h), ('Hp', h)], [('ps', pus[h])])
                    V(lambda e, pu=pus[h], h=h: e.tensor_tensor(out=Up[:, h * 64:(h + 1) * 64], in0=Rr[h][:, 0:64], in1=ps[pu][:, 0:64], op=ALU.add),
                      [('R', h), ('ps', pus[h])], [('Up', h)])
                for h in range(4):
                    py = newps_r()
                    PE(lambda e, py=py, h=h: e.matmul(ps[py][:, 0:64], lhsT=XT[h][:, 128:256], rhs=Hp[h][:], start=True, stop=False), [('XT', h), ('Hp', h)], [('ps', py)])
                    PE(lambda e, py=py, h=h: e.matmul(ps[py][:, 0:64], lhsT=AT[h][:, 0:128], rhs=Up[:, h * 64:(h + 1) * 64], start=False, stop=False),
                       [('AT', h), ('Up', h)], [('ps', py)])
                    PE(lambda e, py=py, h=h: e.matmul(ps[py][:, 0:64], lhsT=AT[h][:, 128:256], rhs=vv[:, h * 64:(h + 1) * 64], start=False, stop=True),
                       [('AT', h), 'vv'], [('ps', py)])
                    A(lambda e, py=py, h=h: e.activation(out=yraw[:, h * 64:(h + 1) * 64], in_=ps[py][:, 0:64], func=AF.Identity), [('ps', py)], [('yraw', h // 2)])
                    ph = newps_r()
                    PE(lambda e, ph=ph, h=h: e.matmul(ps[ph][0:64, 0:64], lhsT=bh[:, h * 64:(h + 1) * 64], rhs=Up[:, h * 64:(h + 1) * 64], start=True, stop=False),
                       ['bh', ('Up', h)], [('ps', ph)])
                    PE(lambda e, ph=ph, h=h: e.matmul(ps[ph][0:64, 0:64], lhsT=kh[:, h * 64:(h + 1) * 64], rhs=vv[:, h * 64:(h + 1) * 64], start=False, stop=True),
                       ['kh', 'vv'], [('ps', ph)])
                    PE(lambda e, ph=ph, h=h: e.matmul(ps[ph][0:64, 256:257], lhsT=lw[:, h * 64:(h + 1) * 64], rhs=cst[:, 3, 0:1], start=True, stop=True),
                       ['lw', 'cst'], [('ps', ph)])
                    A(lambda e, ph=ph, h=h: e.activation(out=DC[h][:], in_=ps[ph][0:64, 256:257], func=AF.Exp), [('ps', ph)], [('DC', h)])
                    V(lambda e, ph=ph, h=h: e.scalar_tensor_tensor(out=Hp[h][:], in0=Hp[h][:], scalar=DC[h][:, 0:1], in1=ps[ph][0:64, 0:64], op0=ALU.mult, op1=ALU.add),
                      [('Hp', h), ('DC', h), ('ps', ph)], [('Hp', h)])
                for h in range(4):
                    V(lambda e, h=h: e.bn_stats(out=stats[:, h, :], in_=yraw[:, h * 64:(h + 1) * 64]), [('yraw', h // 2)], [('stats', h)])
                    V(lambda e, h=h: e.bn_aggr(out=mv[:, h, :], in_=stats[:, h, :]), [('stats', h)], [('mv', h)])
                    V(lambda e, h=h: e.tensor_scalar(out=ss[:, 4 + h:5 + h], in0=mv[:, h, 1:2], scalar1=RW_GN_EPS, scalar2=None, op0=ALU.add), [('mv', h)], [('ss', 4 + h)])
                    A(lambda e, h=h: e.activation(out=ss[:, 4 + h:5 + h], in_=ss[:, 4 + h:5 + h], func=AF.Sqrt), [('ss', 4 + h)], [('ss', 4 + h)])
                    V(lambda e, h=h: e.reciprocal(out=rn[:, 4 + h:5 + h], in_=ss[:, 4 + h:5 + h]), [('ss', 4 + h)], [('rn', 4 + h)])
                    V(lambda e, h=h: e.tensor_scalar(out=outr[:, h * 64:(h + 1) * 64], in0=yraw[:, h * 64:(h + 1) * 64], scalar1=mv[:, h, 0:1], scalar2=rn[:, 4 + h:5 + h],
                                                     op0=ALU.subtract, op1=ALU.mult), [('yraw', h // 2), ('mv', h), ('rn', 4 + h)], [('outr', h)])
                ok4 = [('outr', h) for h in range(4)]
                V(lambda e: e.tensor_tensor(out=outr[:], in0=outr[:], in1=pvb[:, 1280:1536], op=ALU.mult), ok4 + ['pvb'], ok4)
                V(lambda e: e.tensor_tensor(out=outr[:], in0=outr[:], in1=pvb[:, 1536:1792], op=ALU.add), ok4 + ['pvb'], ok4)
                V(lambda e: e.tensor_tensor(out=junk[:], in0=rk[:, 0:256], in1=kmod[:], op=ALU.mult), ['rk', 'kmod'], ['junk'])
                V(lambda e: e.tensor_tensor(out=junk[:], in0=junk[:], in1=pvb[:, 1024:1280], op=ALU.mult), ['junk', 'pvb'], ['junk'])
                V(lambda e: e.reduce_sum(out=rkb[:], in_=junk[:].rearrange("p (h c) -> p h c", h=4), axis=AX.X), ['junk'], ['rkb'])
                for h in range(4):
                    V(lambda e, h=h: e.scalar_tensor_tensor(out=outr[:, h * 64:(h + 1) * 64], in0=vv[:, h * 64:(h + 1) * 64], scalar=rkb[:, h:h + 1],
                                                            in1=outr[:, h * 64:(h + 1) * 64], op0=ALU.mult, op1=ALU.add), ['vv', 'rkb'] + ok4, [('outr', h)])
                V(lambda e: e.tensor_tensor(out=outrb[:], in0=outr[:], in1=gate[:], op=ALU.mult), ok4 + ['gate'], ['outrb'])
                S.op('sync', lambda e, t0=t0: e.dma_start(out=out[t0:t0 + 128, 0:256], in_=outrb[:]), reads=['outrb'], writes=[('outA', t)], dma=True, dkey='outAst')
            if do_moba:
                qb = t // 2
                ti = t % 4
                for which, bank in [(0, 0), (1, 1)]:
                    for h in range(2):
                        for k in range(8):
                            PE(lambda e, which=which, bank=bank, h=h, k=k: e.matmul(ps[bank][:, h * 128:(h + 1) * 128],
                                                                                    lhsT=wm[:, k, which * 256 + h * 128: which * 256 + (h + 1) * 128],
                                                                                    rhs=xt[:, k, 1:129], start=(k == 0), stop=(k == 7)), ['xt', 'wm'], [('ps', bank)])
                A(lambda e: e.activation(out=qTf[:], in_=ps[0][:, 0:256], func=AF.Identity), [('ps', 0)], ['qTf'])
                for h in range(2):
                    V(lambda e, h=h, ti=ti: e.tensor_scalar(out=qg[h][:, ti * 128:(ti + 1) * 128], in0=qTf[:, h * 128:(h + 1) * 128], scalar1=128.0 ** -0.5,
                                                            scalar2=None, op0=ALU.mult), ['qTf'], [('qg', h)])
                A(lambda e: e.activation(out=kTt[:], in_=ps[1][:, 0:256], func=AF.Identity), [('ps', 1)], ['kTt'])
                V(lambda e: e.reduce_sum(out=kpart[:], in_=ps[1][:, 0:256].rearrange("p (h c) -> p h c", h=2), axis=AX.X), [('ps', 1)], ['kpart'])
                for k in range(8):
                    PE(lambda e, k=k: e.matmul(ps[0][:, 256:512], lhsT=xt[:, k, 1:129], rhs=wm[:, k, 512:768], start=(k == 0), stop=(k == 7)),
                       ['xt', 'wm'], [('ps', 0)])
                V(lambda e: e.tensor_copy(out=v1t[:, :, 0:128], in_=ps[0][:, 256:512].rearrange("p (h c) -> p h c", h=2)), [('ps', 0)], ['v1t'])
                for h in range(2):
                    S.op('sync', lambda e, h=h, t0=t0: e.dma_start(out=kTs[h, :, t0:t0 + 128], in_=kTt[:, h * 128:(h + 1) * 128]), reads=['kTt'],
                         writes=[('kTs', h, t)], dma=True, dkey=('kst', h))
                    S.op('sync', lambda e, h=h, t0=t0: e.dma_start(out=v1s[h, t0:t0 + 128, :], in_=v1t[:, h, :]), reads=['v1t'],
                         writes=[('v1s', h, t)], dma=True, dkey=('vst', h))
                if t % 2 == 0:
                    V(lambda e: e.tensor_copy(out=kacc[:], in_=kpart[:]), ['kpart'], ['kacc'])
                pgs = {}
                for h in range(2):
                    pgs[h] = newps()
                    PE(lambda e, pg2=pgs[h], h=h: e.matmul(ps[pg2][:, 0:64], lhsT=qTf[:, h * 128:(h + 1) * 128], rhs=kmT[:, h, :], start=True, stop=True),
                       ['qTf', 'kmT'], [('ps', pgs[h])])
                for h in range(2):
                    V(lambda e, pg2=pgs[h], h=h: e.tensor_copy(out=gm[h][:], in_=ps[pg2][:, 0:64]), [('ps', pgs[h])], [('gm', h)])
                    V(lambda e, qb=qb, h=h: e.memset(gm[h][:, qb:64], NEG), [('gm', h)], [('gm', h)])
                for h in range(2):
                    V(lambda e, h=h: e.max(out=m8[h][:], in_=gm[h][:]), [('gm', h)], [('m8', h)])
                for h in range(2):
                    V(lambda e, h=h: e.tensor_scalar(out=bias[h][:], in0=gm[h][:], scalar1=m8[h][:, 2:3], scalar2=None, op0=ALU.is_ge), [('gm', h), ('m8', h)], [('bias', h)])
                for h in range(2):
                    V(lambda e, h=h: e.tensor_scalar(out=bias[h][:], in0=bias[h][:], scalar1=-NEG, scalar2=NEG, op0=ALU.mult, op1=ALU.add), [('bias', h)], [('bias', h)])
                for h in range(2):
                    if qb + 1 < 64:
                        V(lambda e, qb=qb, h=h: e.memset(bias[h][:, qb + 1:64], NEG), [('bias', h)], [('bias', h)])
                    V(lambda e, qb=qb, h=h: e.memset(bias[h][:, qb:qb + 1], 0.0), [('bias', h)], [('bias', h)])
                pbs = {}
                for h in range(2):
                    pbs[h] = newps()
                    PE(lambda e, pb2=pbs[h], h=h: e.transpose(out=ps[pb2][0:64, 0:128], in_=bias[h][:], identity=ident), [('bias', h), 'cst'], [('ps', pbs[h])])
                for h in range(2):
                    A(lambda e, pb2=pbs[h], h=h, ti=ti: e.activation(out=biasT[h][:, ti * 128:(ti + 1) * 128], in_=ps[pb2][0:64, 0:128], func=AF.Identity),
                      [('ps', pbs[h])], [('biasT', h)])
                if t % 2 == 1:
                    V(lambda e: e.tensor_tensor(out=kacc[:], in0=kacc[:], in1=kpart[:], op=ALU.add), ['kacc', 'kpart'], ['kacc'])
                    V(lambda e, qb=qb: e.tensor_scalar(out=kmT[:, :, qb], in0=kacc[:], scalar1=1.0 / 256.0, scalar2=None, op0=ALU.mult), ['kacc'], ['kmT'])
                if ti == 3:
                    G = t // 4
                    for h in range(2):
                        its = [(kc, j) for kc in range(G + 1) for j in range(4)]
                        pend = None

                        def emit_pv(kc, j, pi, sl, h=h):
                            first = (kc == 0 and j == 0)
                            last = (kc == G and j == 3)
                            for qs in range(4):
                                PE(lambda e, qs=qs, pi=pi, sl=sl, j=j, first=first, last=last: e.matmul(
                                    ps[2 + qs][:, 0:129], lhsT=PT[pi][:, qs * 128:(qs + 1) * 128], rhs=vcb[sl][:, j, :],
                                    start=first, stop=last), [('PT', pi), ('vcb', sl)], [('ps', 2 + qs)])
                        for cnt, (kc, j) in enumerate(its):
                            sl = (kc + h) % 2
                            if j == 0:
                                S.op('sync', lambda e, sl=sl, h=h, kc=kc: e.dma_start(out=kcb[sl][:], in_=kTs[h, :, kc * 512:(kc + 1) * 512]),
                                     reads=[('kTs', h, 4 * kc + jj) for jj in range(4)], writes=[('kcb', sl)], dma=True)
                                S.op('sync', lambda e, sl=sl, h=h, kc=kc: e.dma_start(out=vcb[sl][:], in_=v1s[h, kc * 512:(kc + 1) * 512, :].rearrange("(c p) f -> p c f", p=128)),
                                     reads=[('v1s', h, 4 * kc + jj) for jj in range(4)], writes=[('vcb', sl)], dma=True)
                            n = 2 * kc + j // 2
                            pS = newps()
                            diag = (kc == G)
                            PE(lambda e, pS=pS, sl=sl, j=j, h=h: e.matmul(ps[pS][:], lhsT=kcb[sl][:, j * 128:(j + 1) * 128], rhs=qg[h][:], start=True, stop=False),
                               [('kcb', sl), ('qg', h)], [('ps', pS)])
                            PE(lambda e, pS=pS, n=n, h=h, diag=diag: e.matmul(ps[pS][:], lhsT=esel[:, n, :], rhs=biasT[h][:], start=False, stop=(not diag)),
                               ['esel', ('biasT', h)], [('ps', pS)])
                            if diag:
                                PE(lambda e, pS=pS, j=j: e.matmul(ps[pS][:], lhsT=identb[:], rhs=cmask[:, j, :], start=False, stop=True),
                                   ['identb', 'cmask'], [('ps', pS)])
                            pi = cnt % 2
                            if pend is not None:
                                emit_pv(*pend)
                            A(lambda e, pS=pS, pi=pi: e.activation(out=PT[pi][:], in_=ps[pS][:], func=AF.Exp), [('ps', pS)], [('PT', pi)])
                            pend = (kc, j, pi, sl)
                        emit_pv(*pend)
                        for qs in range(4):
                            V(lambda e, qs=qs: e.reciprocal(out=rec[:, qs:qs + 1], in_=ps[2 + qs][:, 128:129]), [('ps', 2 + qs)], [('rec', qs)])
                            V(lambda e, qs=qs, h=h: e.tensor_scalar(out=mo[:, qs, h * 128:(h + 1) * 128], in0=ps[2 + qs][:, 0:128], scalar1=rec[:, qs:qs + 1],
                                                                    scalar2=None, op0=ALU.mult), [('ps', 2 + qs), ('rec', qs)], [('mo', qs)])
                    for qs in range(4):
                        tq = (4 * G + qs) * 128
                        S.op('sync', lambda e, qs=qs, tq=tq: e.dma_start(out=out[tq:tq + 128, 256:512], in_=mo[:, qs, :]), reads=[('mo', qs)],
                             writes=[('outB', 4 * G + qs)], dma=True, dkey=('outBst', qs))
        S.emit()
    return S


def mix1_consts():
    i = np.arange(128)
    ident = np.eye(128, dtype=np.float32)
    tri = (i[:, None] <= i[None, :]).astype(np.float32)
    strict = (i[:, None] < i[None, :]).astype(np.float32)
    ones = np.ones((128, 128), np.float32)
    esel = np.zeros((64, 64, 128), np.float32)
    esel[np.arange(64), np.arange(64), :] = 1.0
    q = np.arange(512)
    cm = np.zeros((128, 4, 512), np.float32)
    for j in range(4):
        cm[:, j, :] = np.where((j * 128 + i)[:, None] <= q[None, :], 0.0, NEG)
    return np.stack([ident, tri, strict, ones]), esel.reshape(64, -1), cm.reshape(128, -1)


def mix1_inputs(xb, g, w_in, rw_mu, rw_w0, rw_w2, rw_a0, rw_a2, rw_g2, rw_k_k, rw_k_a, rw_r_k, rw_gn_g, rw_gn_b):
    T = xb.shape[0]
    xTp = np.zeros((D, T + 1), np.float32)
    xTp[:, 1:] = xb.T
    r = lambda a, n: np.arange(a, a + n)
    hc = r(g * 256, 256)
    rcols = np.concatenate([hc, 1024 + hc, 2048 + hc, r(3072, 288)])
    Wr = w_in[:, rcols]
    mu = rw_mu[rcols]
    mcols = np.concatenate([3360 + hc, 3360 + 1024 + hc, 3360 + 2048 + hc])
    Wm = w_in[:, mcols]
    pv = np.concatenate([rw_w0[hc], rw_a0[hc], rw_k_k[hc], rw_k_a[hc], rw_r_k.reshape(-1)[hc], rw_gn_g[hc], rw_gn_b[hc]])
    c, es_, cm = mix1_consts()
    return {"xTp": xTp, "Wr": np.ascontiguousarray(Wr), "mu": np.ascontiguousarray(mu), "Wm": np.ascontiguousarray(Wm),
            "pv": np.ascontiguousarray(pv.astype(np.float32)), "lw2": np.ascontiguousarray(rw_w2[:, hc]), "la2": np.ascontiguousarray(rw_a2[:, hc]),
            "lg2": np.ascontiguousarray(rw_g2[:, hc]), "consts": c, "esel": es_, "cmask": cm}


class _G:
    pass


def build_fused(T=16384, NEXP=NE):
    nc = bass.Bass("TRN2", target_bir_lowering=False)
    TQ = T // 4
    PT = min(1024, TQ)

    def dt(name, shape, kind="ExternalInput", dty=F32, **kw):
        return nc.dram_tensor(name, shape, dty, kind=kind, **kw).ap()
    consts = dt("consts", [4, 128, 128])
    W0 = dict(xTp=dt("xTp", [D, T + 3]), Wc=dt("Wc", [D, 1280]), cw=dt("cw", [4 * 1280]), Wp=dt("Wp", [D, 520]), pv0=dt("pv0", [NPV]), consts=consts)
    W1 = dict(Wr=dt("Wr", [D, 1056]), mu=dt("mu", [1056]), Wm=dt("Wm", [D, 768]), pv1=dt("pv1", [1792]), lw2=dt("lw2", [64, 256]),
              la2=dt("la2", [64, 256]), lg2=dt("lg2", [160, 256]), consts=consts, esel=dt("esel", [64, 64 * 128]), cmask=dt("cmask", [128, 4 * 512]),
              kTs=dt("kTs", [2, 128, T], kind="Internal", dty=BF16), v1s=dt("v1s", [2, T, 129], kind="Internal", dty=BF16))
    ident = dt("ident", [128, 128])
    x1s = dt("x1s", [TQ, D], kind="Internal")
    posts = []
    for L in range(2):
        posts.append(dict(w_out=dt("w_out_%d" % L, [2048, D]), lnp=dt("lnp_%d" % L, [4 * D]), rw=dt("rw_%d" % L, [D, NE]), rb=dt("rb_%d" % L, [NE]),
                          w_up=dt("w_up_%d" % L, [NEXP, D, 2 * D]), b_upT=dt("b_upT_%d" % L, [128, NE * 16]), w_down=dt("w_down_%d" % L, [NEXP, D, D]),
                          b_down=dt("b_down_%d" % L, [NE, D]), ident=ident, x1s=x1s))
    xres0 = dt("xres0", [TQ, D])
    y = dt("y", [TQ, D], kind="ExternalOutput")
    mxo = dt("mxo", [T, 512], kind="Internal", dty=BF16)
    mxg = dt("mxg", [4 * T, 512], kind="Internal", dty=BF16, addr_space="Local")
    x1o = dt("x1o", [TQ, D], kind="Internal")
    x1g = dt("x1g", [T, D], kind="Internal", addr_space="Local")
    G = _G()
    G.phase = 0
    with ExitStack() as gs:
        G.sem_stack = gs
        G.ps = [gs.enter_context(nc.psum_tensor("ps%d" % i, [128, 512], F32)) for i in range(8)]
        G.bar = gs.enter_context(nc.semaphore("bar"))
        import os
        nph = int(os.environ.get("FUSED_PHASES", "4"))
        phase_mix0(nc, G, T, W0, mxo)
        if nph >= 2:
            phase_post(nc, G, 0, TQ, PT, posts[0], xres0, x1o if nph > 2 else y, mxo, mxg, NEXP=NEXP)
        if nph >= 3:
            phase_mix1(nc, G, T, W1, x1o, x1g, mxo)
        if nph >= 4:
            phase_post(nc, G, 1, TQ, PT, posts[1], x1o, y, mxo, mxg, NEXP=NEXP)
    return nc


_N0 = ['ev_w_in', 'ev_ssd_conv_w', 'ev_ssd_conv_b', 'ev_ssd_dt_bias', 'ev_ssd_a_log', 'ev_ssd_d', 'ev_ssd_norm_w', 'ev_gdn_conv_w',
       'ev_gdn_a_log', 'ev_gdn_dt_bias', 'ev_gdn_norm_w']
_N1 = ['od_w_in', 'od_rw_mu', 'od_rw_w0', 'od_rw_w2', 'od_rw_a0', 'od_rw_a2', 'od_rw_g2', 'od_rw_k_k', 'od_rw_k_a', 'od_rw_r_k',
       'od_rw_gn_g', 'od_rw_gn_b']


def fused_inputs(inp, T=None):
    f32 = lambda a: np.ascontiguousarray(np.asarray(a, dtype=np.float32))
    x = np.asarray(inp['x'], dtype=np.float32)
    if T is not None:
        x = x[:, :T]
    B, T, Dm = x.shape
    TQ = T // 4
    P0 = [f32(inp[k][0]) for k in _N0]
    P1 = [f32(inp[k][0]) for k in _N1]
    rows = np.concatenate([np.concatenate([np.arange(r * 256, (r + 1) * 256), 1024 + np.arange(r * 256, (r + 1) * 256)]) for r in range(4)])
    shared = {"ident": np.eye(128, dtype=np.float32)}
    for L, wkey in [(0, 'ev_w_out'), (1, 'od_w_out')]:
        pi = post_inputs(np.zeros((1, 2048), np.float32), np.zeros((1, Dm), np.float32), f32(inp[wkey][0])[rows], f32(inp['mix_ln_g'][L]),
                         f32(inp['mix_ln_b'][L]), f32(inp['moe_router_w'][L]), f32(inp['moe_router_b'][L]), np.asarray(inp['moe_w_up'][L]),
                         f32(inp['moe_b_up'][L]), f32(inp['moe_w_down'][L]), f32(inp['moe_b_down'][L]), f32(inp['ffn_ln_g'][L]), f32(inp['ffn_ln_b'][L]))
        for k in ['w_out', 'lnp', 'rw', 'rb', 'w_up', 'b_upT', 'w_down', 'b_down']:
            shared["%s_%d" % (k, L)] = pi[k]
    xf = x.reshape(B * T, Dm)
    ims = []
    for c in range(8):
        b, g = c // 4, c % 4
        im = dict(shared)
        m0 = mix0_inputs(x[b], g, *P0)
        m0['pv0'] = m0.pop('pv')
        im.update(m0)
        m1 = mix1_inputs(np.zeros((1, Dm), np.float32), g, *P1)
        m1.pop('xTp')
        m1.pop('consts')
        m1['pv1'] = m1.pop('pv')
        im.update(m1)
        im['xres0'] = np.ascontiguousarray(xf[c * TQ:(c + 1) * TQ])
        ims.append(im)
    return ims, (B, T, Dm)


_NC_CACHE = {}


def kernel(**inp):
    ims, (B, T, Dm) = fused_inputs(inp)
    if T not in _NC_CACHE:
        _NC_CACHE[T] = build_fused(T=T)
    res = run_bass_kernel_spmd(_NC_CACHE[T], ims, core_ids=list(range(8)))
    y = np.concatenate([res.results[c]['y'] for c in range(8)], axis=0)
    return y.reshape(B, T, Dm).astype(np.float32)
```

```python
from contextlib import ExitStack
import numpy as np
import concourse.bass as bass
import concourse.mybir as mybir
from concourse.bass_utils import run_bass_kernel_spmd

F32, BF16 = mybir.dt.float32, mybir.dt.bfloat16
AF = mybir.ActivationFunctionType
ALU = mybir.AluOpType
AX = mybir.AxisListType


class Sched:
    EPOCH = 20000

    def __init__(self, nc, strict=True, G=None):
        self.nc = nc
        self.G = G
        self._q = {}
        self.ops = []
        self.strict = strict
        self.last_writer = {}
        self.readers = {}

    def qv(self, e):
        k = id(e)
        if k not in self._q:
            self._q[k] = e.partition_id() % 4
        return self._q[k]

    def op(self, eng, fn, reads=(), writes=(), dma=False, acc=False, dkey=None, cc=False, alts=None):
        i = len(self.ops)
        deps = set()
        for k in reads:
            w = self.last_writer.get(k)
            if w is not None:
                deps.add(w)
            if isinstance(k, tuple) and k[0] == 'ps':
                for r in self.readers.get(k, ()):
                    if self.ops[r]['eng'] != eng:
                        deps.add(r)
        for k in writes:
            w = self.last_writer.get(k)
            if w is not None:
                pw = self.ops[w]
                if not (pw['eng'] == eng and not pw['dma'] and not dma):
                    deps.add(w)
            for r in self.readers.get(k, ()):
                pr_ = self.ops[r]
                if not (pr_['eng'] == eng and not pr_['dma'] and not dma):
                    deps.add(r)
        for k in reads:
            self.readers.setdefault(k, []).append(i)
        for k in writes:
            self.last_writer[k] = i
            self.readers[k] = []
        if cc:
            dma = True
        if dma and dkey is None:
            dkey = (tuple(writes) + tuple(reads))[0]
        self.ops.append(dict(eng=eng, fn=fn, deps=deps, dma=dma, sig=False, dkey=dkey, tok=None, inc=(1 if cc else 16), alts=alts))
        return i

    def emit(self, final_eng='sync'):
        nc = self.nc
        ops = self.ops
        for op in ops:
            keep = set()
            for d in op['deps']:
                p = ops[d]
                if p['dma'] or op['dma'] or p['eng'] != op['eng'] or self.strict:
                    p['sig'] = True
                    keep.add(d)
            op['deps'] = keep
        last_of = {}
        for i, op in enumerate(ops):
            if not op['dma'] and op['fn'] is not None:
                last_of[op['eng']] = i
        for i in last_of.values():
            ops[i]['sig'] = True
        eng_cnt = {}
        dma_cnt = {}
        sem_names = []
        for op in ops:
            if op['dma']:
                op['sig'] = True
                n = dma_cnt.get(op['dkey'], 0) + 1
                dma_cnt[op['dkey']] = n
                op['tok'] = (('d', op['dkey']), op['inc'] * n)
            elif op['sig']:
                n = eng_cnt.get(op['eng'], 0)
                eng_cnt[op['eng']] = n + 1
                op['tok'] = (('e', op['eng'], n // self.EPOCH), n % self.EPOCH + 1)
        names = []
        for op in ops:
            if op['tok'] is not None and op['tok'][0] not in names:
                names.append(op['tok'][0])
        self.n_sems = len(names)
        G = self.G
        G.phase += 1
        phase = G.phase
        with ExitStack() as st:
            sems = {}
            for i, nm in enumerate(names):
                sems[nm] = G.sem_stack.enter_context(nc.semaphore("p%d_s%d" % (phase, i)))
            block = st.enter_context(nc.Block())
            engs = ['tensor', 'vector', 'scalar', 'gpsimd', 'sync']
            final = {}
            for op in ops:
                if op['dma']:
                    final[op['tok'][0]] = max(final.get(op['tok'][0], 0), op['tok'][1])

            def make_body(en):
                def body(e):
                    waited = {}
                    for op in ops:
                        if op['eng'] != en:
                            continue
                        for d in sorted(op['deps']):
                            nm, val = ops[d]['tok']
                            if waited.get(nm, 0) < val:
                                e.wait_ge(sems[nm], val)
                                waited[nm] = val
                        if op['fn'] is None:
                            continue
                        if op['alts'] is not None:
                            q = self.qv(e)
                            nm, val = op['tok']
                            for j, f in enumerate(op['alts']):
                                with e.If(q == j):
                                    f(e).then_inc(sems[nm], op['inc'])
                            continue
                        ins = op['fn'](e)
                        if op['sig']:
                            nm, val = op['tok']
                            ins.then_inc(sems[nm], op['inc'] if op['dma'] else 1)
                    if en == final_eng:
                        for nm, val in final.items():
                            if waited.get(nm, 0) < val:
                                e.wait_ge(sems[nm], val)
                    if en in last_of:
                        nm, val = ops[last_of[en]]['tok']
                        if waited.get(nm, 0) < val:
                            e.wait_ge(sems[nm], val)
                    e.nop().then_inc(G.bar, 1)
                    e.wait_ge(G.bar, 5 * phase)
                return body

            for en in engs:
                getattr(block, en)(make_body(en))


D = 1024
NE = 32
DN_ALPHA = 4.0 ** 0.25
LN_EPS = 1e-5


class Stop(Exception):
    pass


def phase_post(nc, G, L, T, PT, Wd, xres, y, mxo, mxg, NEXP=NE, strict=True):
    stage = 99
    NP = T // PT
    NT = PT // 128
    NH = PT // 512
    w_out, lnp, rw, rb, w_up, b_upT, w_down, b_down, ident_d, x1s = (Wd[k] for k in
        ['w_out', 'lnp', 'rw', 'rb', 'w_up', 'b_upT', 'w_down', 'b_down', 'ident', 'x1s'])
    TB = mxo.shape[0]

    S = Sched(nc, strict=strict, G=G)
    with ExitStack() as es:
        sb = lambda name, shape, dty=F32: es.enter_context(nc.sbuf_tensor("ph%d_%s" % (G.phase + 1, name), shape, dty))
        wbu = [sb("wbu%d" % i, [128, 8 * 2048], BF16) for i in range(2)]
        wbd = sb("wbd", [128, 8, D], BF16)
        xT = sb("xT", [128, 8, PT], BF16)
        yacc = sb("yacc", [128, NT, D])
        mt = sb("mt", [128, 16, 128], BF16)
        mtok = sb("mtok", [128, 4, 512], BF16)
        identb = sb("identb", [128, 128], BF16)
        xr = sb("xr", [128, D])
        z = sb("z", [128, D])
        x1 = [sb("x1_%d" % i, [128, D]) for i in range(2)]
        xT32 = sb("xT32", [128, 8, 128])
        actT = sb("actT", [128, 8, 512], BF16)
        gt = [sb("g%d" % i, [128, 512]) for i in range(2)]
        st_ = [sb("s%d" % i, [128, 512]) for i in range(2)]
        lt = [sb("l%d" % i, [128, 512]) for i in range(2)]
        gates = sb("gates", [128, T // 128, NE])
        gT = sb("gT", [NE, 128])
        bu = sb("bu", [128, NE * 16])
        bu1 = sb("bu1", [128, NE * 16])
        rw32 = sb("rw32", [128, 8, NE])
        rbb = sb("rbb", [128, NE])
        bd32 = sb("bd32", [NE, D])
        ident = sb("identsb", [128, 128])
        lnpb = sb("lnpb", [128, 4 * D])
        stats = sb("stats", [128, 2, 6])
        mv = sb("mv", [128, 2])
        sm = sb("sm", [128, 8])
        lg = sb("lg", [128, NE])
        m8 = sb("m8", [128, 8])
        ex = sb("ex", [128, NE])
        sel = sb("sel", [128, NE])
        ps = G.ps
        psb = [p_[:].bitcast(BF16) for p_ in ps]

        RCH = 1024
        NCH = TB // RCH
        mxg4 = mxg.rearrange("(i r t) c -> i r t c", i=NCH, r=4)
        for i in range(NCH):
            S.op('gpsimd', lambda e, i=i: e.collective_compute('AllGather', ALU.bypass, replica_groups=[[0, 1, 2, 3], [4, 5, 6, 7]],
                                                               ins=[mxo[i * RCH:(i + 1) * RCH, :]], outs=[mxg[i * 4 * RCH:(i + 1) * 4 * RCH, :]]),
                 writes=[('mxg', i)], cc=True, dkey='ccg')
        S.op('sync', lambda e: e.dma_start(out=lnpb[:], in_=lnp.partition_broadcast(128)), writes=['lnpb'], dma=True)
        S.op('sync', lambda e: e.dma_start(out=rbb[:], in_=rb.partition_broadcast(128)), writes=['rbb'], dma=True)
        S.op('sync', lambda e: e.dma_start(out=rw32[:], in_=rw.rearrange("(k p) e -> p k e", p=128)), writes=['rw32'], dma=True)
        S.op('sync', lambda e: e.dma_start(out=bu[:], in_=b_upT), writes=['bu'], dma=True)
        S.op('sync', lambda e: e.dma_start(out=bd32[:], in_=b_down), writes=['bd32'], dma=True)
        S.op('sync', lambda e: e.dma_start(out=ident[:], in_=ident_d), writes=['ident'], dma=True)
        S.op('vector', lambda e: e.tensor_copy(out=identb[:], in_=ident[:]), reads=['ident'], writes=['identb'])
        S.op('vector', lambda e: e.tensor_scalar(out=bu1[:], in0=bu[:], scalar1=1.0, scalar2=None, op0=ALU.add),
             reads=['bu'], writes=['bu1'])

        def layer_norm(zkey, zt, okey, ot, pidx):
            for h in range(2):
                S.op('vector', lambda e, h=h: e.bn_stats(out=stats[:, h, :], in_=zt[:, h * 512:(h + 1) * 512]),
                     reads=[zkey], writes=[('stats', h)])
            S.op('vector', lambda e: e.bn_aggr(out=mv[:], in_=stats[:].rearrange("p a b -> p (a b)")),
                 reads=[('stats', 0), ('stats', 1)], writes=['mv'])
            S.op('vector', lambda e: e.tensor_scalar(out=sm[:, 0:1], in0=mv[:, 1:2], scalar1=LN_EPS, scalar2=None, op0=ALU.add),
                 reads=['mv'], writes=['sm0'])
            S.op('scalar', lambda e: e.activation(out=sm[:, 1:2], in_=sm[:, 0:1], func=AF.Sqrt), reads=['sm0'], writes=['sm1'])
            S.op('vector', lambda e: e.reciprocal(out=sm[:, 2:3], in_=sm[:, 1:2]), reads=['sm1'], writes=['sm2'])
            S.op('vector', lambda e: e.tensor_scalar(out=zt[:], in0=zt[:], scalar1=mv[:, 0:1], scalar2=sm[:, 2:3],
                                                     op0=ALU.subtract, op1=ALU.mult),
                 reads=[zkey, 'mv', 'sm2'], writes=[zkey])
            S.op('vector', lambda e: e.tensor_tensor(out=zt[:], in0=zt[:], in1=lnpb[:, pidx * D:(pidx + 1) * D], op=ALU.mult),
                 reads=[zkey, 'lnpb'], writes=[zkey])
            S.op('vector', lambda e: e.tensor_tensor(out=ot[:], in0=zt[:], in1=lnpb[:, (pidx + 1) * D:(pidx + 2) * D], op=ALU.add),
                 reads=[zkey, 'lnpb'], writes=[okey])

        w_out_v = w_out.rearrange("(k p) d -> p k d", p=128)
        wo = wbu[1][:, :].rearrange("p (k d) -> p k d", k=16)
        wu = [wbu[i][:, :].rearrange("p (k f) -> p k f", k=8) for i in range(2)]

        try:
          if stage == 0: raise Stop
          for p in range(NP):
              for hh in range(2):
                  S.op('gpsimd', lambda e, hh=hh: e.dma_start(out=wo[:, hh * 8:(hh + 1) * 8, :], in_=w_out_v[:, hh * 8:(hh + 1) * 8, :]),
                       writes=[('wbu', 1)], dma=True, dkey=('wbu', 1))
              for i in range(NT):
                  gi = p * NT + i
                  t0 = gi * 128
                  S.op('sync', True, reads=[('mxg', i_) for i_ in range(NCH)], writes=['mtok'], dma=True, dkey='mtokld',
                       alts=[(lambda e, tt=jq * T + t0: e.dma_start(out=mtok[:], in_=mxg4[tt // RCH, :, tt % RCH:tt % RCH + 128, :].rearrange("r t c -> t r c")))
                             for jq in range(4)])
                  for kk in range(16):
                      S.op('tensor', lambda e, kk=kk: e.transpose(out=psb[2 + kk // 8][:, (kk % 8) * 128:(kk % 8 + 1) * 128],
                                                                    in_=mtok[:, kk // 4, (kk % 4) * 128:(kk % 4 + 1) * 128], identity=identb[:]),
                           reads=['mtok', 'identb'], writes=[('ps', 2 + kk // 8)], acc=True)
                  for hh in range(2):
                      S.op('vector' if hh == 0 else 'scalar', (lambda e, hh=hh: e.tensor_copy(out=mt[:, hh * 8:(hh + 1) * 8, :], in_=psb[2 + hh][:].rearrange("p (c t) -> p c t", c=8))) if hh == 0 else
                           (lambda e, hh=hh: e.activation(out=mt[:, hh * 8:(hh + 1) * 8, :], in_=psb[2 + hh][:].rearrange("p (c t) -> p c t", c=8), func=AF.Identity)),
                           reads=[('ps', 2 + hh)], writes=['mt'])
                  S.op('sync', lambda e, t0=t0: e.dma_start(out=xr[:], in_=xres[t0:t0 + 128, :]), writes=['xr'], dma=True)
                  if stage == 1: raise Stop
                  for h in range(2):
                      for k in range(16):
                          S.op('tensor', lambda e, h=h, k=k: e.matmul(ps[h][:], lhsT=mt[:, k, :], rhs=wo[:, k, h * 512:(h + 1) * 512],
                                                                      start=(k == 0), stop=(k == 15)),
                               reads=['mt', ('wbu', 1)], writes=[('ps', h)], acc=True)
                      S.op('vector', lambda e, h=h: e.scalar_tensor_tensor(out=z[:, h * 512:(h + 1) * 512], in0=xr[:, h * 512:(h + 1) * 512],
                                                                           scalar=DN_ALPHA, in1=ps[h][:], op0=ALU.mult, op1=ALU.add),
                           reads=['xr', ('ps', h)], writes=['z'])
                  if stage == 2: raise Stop
                  xs = x1[gi % 2]
                  xk = ('x1', gi % 2)
                  layer_norm('z', z, xk, xs, 0)
                  if stage == 3: raise Stop
                  S.op('sync', lambda e, t0=t0, xs=xs: e.dma_start(out=x1s[t0:t0 + 128, :], in_=xs[:]), reads=[xk], writes=[('x1s', gi)],
                       dma=True, dkey=('x1st', gi % 2))
                  if stage == 31: raise Stop
                  for c in range(8):
                      S.op('tensor', lambda e, c=c, xs=xs: e.transpose(out=ps[2 + c // 4][:, (c % 4) * 128:(c % 4 + 1) * 128],
                                                                       in_=xs[:, c * 128:(c + 1) * 128], identity=ident[:]),
                           reads=[xk, 'ident'], writes=[('ps', 2 + c // 4)], acc=True)
                  if stage == 32: raise Stop
                  for hh in range(2):
                      S.op('scalar', lambda e, hh=hh: e.activation(out=xT32[:, hh * 4:(hh + 1) * 4, :],
                                                                   in_=ps[2 + hh][:].rearrange("p (c t) -> p c t", c=4), func=AF.Identity),
                           reads=[('ps', 2 + hh)], writes=[('xT32', hh)])
                      if stage == 33: continue
                      S.op('vector', lambda e, hh=hh, i=i: e.tensor_copy(out=xT[:, hh * 4:(hh + 1) * 4, i * 128:(i + 1) * 128],
                                                                         in_=xT32[:, hh * 4:(hh + 1) * 4, :]),
                           reads=[('xT32', hh)], writes=[('xT', i)])
                  if stage == 4: raise Stop
                  for c in range(8):
                      S.op('tensor', lambda e, c=c: e.matmul(ps[6][:, 0:NE], lhsT=xT32[:, c, :], rhs=rw32[:, c, :], start=(c == 0), stop=(c == 7)),
                           reads=[('xT32', c // 4), 'rw32'], writes=[('ps', 6)], acc=True)
                  S.op('vector', lambda e: e.tensor_tensor(out=lg[:], in0=ps[6][:, 0:NE], in1=rbb[:], op=ALU.add),
                       reads=[('ps', 6), 'rbb'], writes=['lg'])
                  S.op('vector', lambda e: e.max(out=m8[:], in_=lg[:]), reads=['lg'], writes=['m8'])
                  S.op('vector', lambda e: e.tensor_scalar(out=sel[:], in0=lg[:], scalar1=m8[:, 3:4], scalar2=None, op0=ALU.is_ge),
                       reads=['lg', 'm8'], writes=['sel'])
                  S.op('vector', lambda e: e.tensor_scalar(out=sm[:, 3:4], in0=m8[:, 0:1], scalar1=-1.0, scalar2=None, op0=ALU.mult),
                       reads=['m8'], writes=['sm3'])
                  S.op('scalar', lambda e: e.activation(out=ex[:], in_=lg[:], func=AF.Exp, bias=sm[:, 3:4], scale=1.0),
                       reads=['lg', 'sm3'], writes=['ex'])
                  S.op('vector', lambda e: e.tensor_tensor(out=ex[:], in0=ex[:], in1=sel[:], op=ALU.mult), reads=['ex', 'sel'], writes=['ex'])
                  S.op('vector', lambda e: e.reduce_sum(out=sm[:, 4:5], in_=ex[:], axis=AX.X), reads=['ex'], writes=['sm4'])
                  S.op('vector', lambda e: e.reciprocal(out=sm[:, 5:6], in_=sm[:, 4:5]), reads=['sm4'], writes=['sm5'])
                  S.op('vector', lambda e, gi=gi: e.tensor_scalar(out=gates[:, gi, :], in0=ex[:], scalar1=sm[:, 5:6], scalar2=None, op0=ALU.mult),
                       reads=['ex', 'sm5'], writes=[('gates', gi)])
                  if stage == 5: raise Stop
                  S.op('tensor', lambda e, gi=gi: e.transpose(out=ps[7][0:NE, 0:128], in_=gates[:, gi, :], identity=ident[:]),
                       reads=[('gates', gi), 'ident'], writes=[('ps', 7)])
                  S.op('scalar', lambda e: e.activation(out=gT[:], in_=ps[7][0:NE, 0:128], func=AF.Identity), reads=[('ps', 7)], writes=['gT'])
                  for h in range(2):
                      S.op('tensor', lambda e, h=h: e.matmul(ps[4 + h][:], lhsT=gT[:], rhs=bd32[:, h * 512:(h + 1) * 512], start=True, stop=True),
                           reads=['gT', 'bd32'], writes=[('ps', 4 + h)])
                      S.op('scalar', lambda e, h=h, i=i: e.activation(out=yacc[:, i, h * 512:(h + 1) * 512], in_=ps[4 + h][:], func=AF.Identity),
                           reads=[('ps', 4 + h)], writes=[('yacc', i)])
              if stage == 6: raise Stop
              for ex_i in range(NEXP):
                  sl = ex_i % 2
                  for hh in range(2):
                      S.op('gpsimd', lambda e, ex_i=ex_i, sl=sl, hh=hh: e.dma_start(
                          out=wu[sl][:, hh * 4:(hh + 1) * 4, :],
                          in_=w_up[ex_i].rearrange("(k p) f -> p k f", p=128)[:, hh * 4:(hh + 1) * 4, :]),
                          writes=[('wbu', sl)], dma=True, dkey=('wbu', sl))
                  S.op('gpsimd', lambda e, ex_i=ex_i: e.dma_start(out=wbd[:], in_=w_down[ex_i].rearrange("(k p) d -> p k d", p=128)),
                       writes=['wbd'], dma=True)
                  for th in range(NH):
                      for fc in range(8):
                          q = fc % 2
                          for half in range(2):
                              pst = ps[q * 2 + half]
                              for k in range(8):
                                  S.op('tensor', lambda e, pst=pst, sl=sl, k=k, fc=fc, half=half, th=th: e.matmul(
                                      pst[:], lhsT=wu[sl][:, k, half * 1024 + fc * 128: half * 1024 + (fc + 1) * 128],
                                      rhs=xT[:, k, th * 512:(th + 1) * 512], start=(k == 0), stop=(k == 7)),
                                      reads=[('wbu', sl)] + [('xT', th * 4 + j) for j in range(4)], writes=[('ps', q * 2 + half)], acc=True)
                          col = ex_i * 16 + fc
                          S.op('vector', lambda e, q=q, col=col: e.tensor_scalar(out=gt[q][:], in0=ps[q * 2][:], scalar1=bu[:, col:col + 1], scalar2=7.0,
                                                                                 op0=ALU.add, op1=ALU.min),
                               reads=[('ps', q * 2), 'bu'], writes=[('g', q)])
                          S.op('vector', lambda e, q=q, col=col: e.tensor_scalar(out=lt[q][:], in0=ps[q * 2 + 1][:], scalar1=bu1[:, col + 8:col + 9], scalar2=8.0,
                                                                                 op0=ALU.add, op1=ALU.min),
                               reads=[('ps', q * 2 + 1), 'bu1'], writes=[('l', q)])
                          S.op('scalar', lambda e, q=q: e.activation(out=st_[q][:], in_=gt[q][:], func=AF.Sigmoid, scale=1.702),
                               reads=[('g', q)], writes=[('s', q)])
                          S.op('vector', lambda e, q=q: e.tensor_tensor(out=gt[q][:], in0=gt[q][:], in1=st_[q][:], op=ALU.mult),
                               reads=[('g', q), ('s', q)], writes=[('g', q)])
                          S.op('vector', lambda e, q=q, fc=fc: e.scalar_tensor_tensor(out=actT[:, fc, :], in0=lt[q][:], scalar=-6.0, in1=gt[q][:],
                                                                                      op0=ALU.max, op1=ALU.mult),
                               reads=[('l', q), ('g', q)], writes=[('actT', fc)])
                      for tt in range(4):
                          ti = th * 4 + tt
                          gi = p * NT + ti
                          for dh in range(2):
                              for fc in range(8):
                                  S.op('tensor', lambda e, dh=dh, fc=fc, tt=tt: e.matmul(
                                      ps[4 + dh][:], lhsT=actT[:, fc, tt * 128:(tt + 1) * 128], rhs=wbd[:, fc, dh * 512:(dh + 1) * 512],
                                      start=(fc == 0), stop=(fc == 7)),
                                      reads=[('actT', fc), 'wbd'], writes=[('ps', 4 + dh)], acc=True)
                              S.op('vector', lambda e, dh=dh, ti=ti, gi=gi, ex_i=ex_i: e.scalar_tensor_tensor(
                                  out=yacc[:, ti, dh * 512:(dh + 1) * 512], in0=ps[4 + dh][:], scalar=gates[:, gi, ex_i:ex_i + 1],
                                  in1=yacc[:, ti, dh * 512:(dh + 1) * 512], op0=ALU.mult, op1=ALU.add),
                                  reads=[('ps', 4 + dh), ('gates', gi), ('yacc', ti)], writes=[('yacc', ti)])
              for i in range(NT):
                  gi = p * NT + i
                  t0 = gi * 128
                  S.op('sync', lambda e, t0=t0: e.dma_start(out=xr[:], in_=x1s[t0:t0 + 128, :]), reads=[('x1s', gi)], writes=['xr'], dma=True)
                  S.op('vector', lambda e, i=i: e.scalar_tensor_tensor(out=z[:], in0=xr[:], scalar=DN_ALPHA, in1=yacc[:, i, :], op0=ALU.mult, op1=ALU.add),
                       reads=['xr', ('yacc', i)], writes=['z'])
                  os_ = x1[gi % 2]
                  ok = ('x1', gi % 2)
                  layer_norm('z', z, ok, os_, 2)
                  S.op('sync', lambda e, t0=t0, os_=os_: e.dma_start(out=y[t0:t0 + 128, :], in_=os_[:]), reads=[ok], writes=[('y', gi)],
                       dma=True, dkey=('x1st', gi % 2))
        except Stop:
            pass
        S.emit()
    return S


def post_inputs(mixed, xres, w_out, lng1, lnb1, rw, rb, w_up, b_up, w_down, b_down, lng2, lnb2):
    perm = np.concatenate([np.arange(0, 2 * D, 2), np.arange(1, 2 * D, 2)])
    b_up_p = b_up[:, perm]
    b_upT = np.ascontiguousarray(b_up_p.reshape(NE, 16, 128).transpose(2, 0, 1).reshape(128, NE * 16))
    return {
        "mixT": np.ascontiguousarray(mixed.T), "xres": np.ascontiguousarray(xres), "w_out": np.ascontiguousarray(w_out),
        "lnp": np.ascontiguousarray(np.concatenate([lng1, lnb1, lng2, lnb2])), "rw": np.ascontiguousarray(rw), "rb": np.ascontiguousarray(rb),
        "w_up": np.ascontiguousarray(w_up[:, :, perm]), "b_upT": b_upT, "w_down": np.ascontiguousarray(w_down),
        "b_down": np.ascontiguousarray(b_down), "ident": np.eye(128, dtype=np.float32),
    }


D = 1024
NORM_EPS = 1e-6
NPV = 912


def phase_mix0(nc, G, T, Wd, mxo, strict=True):
    NTL = T // 128
    xTp, Wc, cw, Wp, pv, consts = (Wd[k] for k in ['xTp', 'Wc', 'cw', 'Wp', 'pv0', 'consts'])
    out = mxo

    S = Sched(nc, strict=strict, G=G)
    with ExitStack() as es:
        sb = lambda name, shape, dty=F32: es.enter_context(nc.sbuf_tensor("ph%d_%s" % (G.phase + 1, name), shape, dty))
        wc = sb("wc", [128, 8, 4, 1280], BF16)
        cwb = sb("cwb", [128, 4, 1280])
        stage = sb("stage", [128, 1280])
        wp = sb("wp", [128, 8, 520], BF16)
        pvb = sb("pvb", [128, NPV])
        cst = sb("cst", [128, 4, 128])
        ident, tri, strictm, ones = (cst[:, i, :] for i in range(4))
        xt = sb("xt", [128, 8, 131], BF16)
        xbc = sb("xbc", [128, 512])
        qk = sb("qk", [128, 512])
        vv = sb("vv", [128, 256])
        zz = sb("zz", [128, 512])
        small = sb("small", [128, 8])
        ah6 = sb("ah6", [128, 6])
        t6 = sb("t6", [128, 6])
        g6 = sb("g6", [128, 6])
        sp6 = sb("sp6", [128, 6])
        beta = sb("beta", [128, 2])
        ss4 = sb("ss4", [128, 8])
        rn4 = sb("rn4", [128, 8])
        junk = sb("junk", [128, 256])
        kb = sb("kb", [128, 256])
        Rr = [sb("R%d" % h, [128, 256]) for h in range(2)]
        xd = sb("xd", [128, 256])
        gc6 = sb("gc6", [128, 6])
        gl6 = sb("gl6", [128, 6])
        egc6 = sb("egc6", [128, 6])
        egl6 = sb("egl6", [128, 6])
        kt6 = sb("kt6", [128, 6])
        triG = sb("triG", [128, 6, 128])
        decT = sb("decT", [128, 6, 128])
        T1 = sb("T1", [128, 4, 128])
        T2 = sb("T2", [128, 4, 128])
        NTm = [[sb("NT%d_%d" % (h, i), [128, 128]) for i in range(2)] for h in range(2)]
        Nm = [[sb("N%d_%d" % (h, i), [128, 128]) for i in range(2)] for h in range(2)]
        attnT = sb("attnT", [128, 6, 128])
        CBT = sb("CBT", [128, 128])
        tmpA = [sb("tmpA%d" % h, [128, 128]) for h in range(6)]
        wT = [sb("wT%d" % h, [128, 128]) for h in range(2)]
        vnew = [sb("vnew%d" % h, [128, 128]) for h in range(2)]
        ktail = [sb("ktail%d" % h, [128, 128]) for h in range(6)]
        Sg = [sb("Sg%d" % h, [128, 128]) for h in range(2)]
        Ss = [sb("Ss%d" % h, [128, 64]) for h in range(4)]
        oo = sb("oo", [128, 512])
        outt = sb("outt", [128, 512], BF16)
        ps = G.ps
        rr = [0]

        def newps():
            rr[0] = (rr[0] + 1) % 6
            return 2 + rr[0]

        V = lambda fn, r, w: S.op('vector', fn, r, w)
        A = lambda fn, r, w: S.op('scalar', fn, r, w)
        PE = lambda fn, r, w: S.op('tensor', fn, r, w, acc=True)

        S.op('sync', lambda e: e.dma_start(out=cst[:], in_=consts.rearrange("c p f -> p c f")), writes=['cst'], dma=True)
        S.op('sync', lambda e: e.dma_start(out=pvb[:], in_=pv.partition_broadcast(128)), writes=['pvb'], dma=True)
        S.op('sync', lambda e: e.dma_start(out=cwb[:].rearrange("p a c -> p (a c)"), in_=cw.partition_broadcast(128)), writes=['cwb'], dma=True)
        S.op('gpsimd', lambda e: e.dma_start(out=wp[:], in_=Wp.rearrange("(k p) c -> p k c", p=128)), writes=['wp'], dma=True)
        for k in range(8):
            S.op('sync', lambda e, k=k: e.dma_start(out=stage[:], in_=Wc[k * 128:(k + 1) * 128, :]), writes=['stage'], dma=True)
            for j in range(4):
                V(lambda e, k=k, j=j: e.tensor_tensor(out=wc[:, k, j, :], in0=stage[:], in1=cwb[:, j, :], op=ALU.mult),
                  ['stage', 'cwb'], [('wc', k, j)])
        wc_keys = [('wc', k, j) for k in range(8) for j in range(4)]
        A(lambda e: e.activation(out=ah6[:], in_=pvb[:, 518:524], func=AF.Exp), ['pvb'], ['ah6'])
        V(lambda e: e.tensor_scalar(out=ah6[:], in0=ah6[:], scalar1=-1.0, scalar2=None, op0=ALU.mult), ['ah6'], ['ah6'])
        for h in range(2):
            V(lambda e, h=h: e.memset(Sg[h][:], 0.0), [], [('Sg', h)])
        for h in range(4):
            V(lambda e, h=h: e.memset(Ss[h][:], 0.0), [], [('Ss', h)])

        for t in range(NTL):
            t0 = t * 128
            S.op('gpsimd', lambda e, t0=t0: e.dma_start(out=xt[:], in_=xTp.rearrange("(k p) t -> p k t", p=128)[:, :, t0:t0 + 131]),
                 writes=['xt'], dma=True)
            blocks = [(0, 512), (512, 1024), (1024, 1280)]
            for bi, (c0, c1) in enumerate(blocks):
                b = bi % 2
                n = 0
                for j in range(4):
                    for k in range(8):
                        PE(lambda e, b=b, c0=c0, c1=c1, j=j, k=k, n=n: e.matmul(ps[b][:, 0:c1 - c0], lhsT=xt[:, k, j:j + 128], rhs=wc[:, k, j, c0:c1],
                                                                                start=(n == 0), stop=(n == 31)),
                           ['xt', ('wc', k, j)], [('ps', b)])
                        n += 1
                if bi == 0:
                    V(lambda e, b=b: e.tensor_tensor(out=xbc[:], in0=ps[b][:], in1=pvb[:, 0:512], op=ALU.add), [('ps', b), 'pvb'], ['xbc'])
                    A(lambda e: e.activation(out=xbc[:], in_=xbc[:], func=AF.Silu), ['xbc'], ['xbc'])
                elif bi == 1:
                    A(lambda e, b=b: e.activation(out=qk[:], in_=ps[b][:], func=AF.Silu), [('ps', b)], ['qk'])
                else:
                    A(lambda e, b=b: e.activation(out=vv[:], in_=ps[b][:, 0:256], func=AF.Silu), [('ps', b)], ['vv'])
            for bi, (c0, c1) in enumerate([(0, 512), (512, 520)]):
                b = (bi + 1) % 2
                for k in range(8):
                    PE(lambda e, b=b, c0=c0, c1=c1, k=k: e.matmul(ps[b][:, 0:c1 - c0], lhsT=xt[:, k, 3:131], rhs=wp[:, k, c0:c1],
                                                                  start=(k == 0), stop=(k == 7)),
                       ['xt', 'wp'], [('ps', b)])
                if bi == 0:
                    A(lambda e, b=b: e.activation(out=zz[:], in_=ps[b][:], func=AF.Silu), [('ps', b)], ['zz'])
                else:
                    V(lambda e, b=b: e.tensor_copy(out=small[:], in_=ps[b][:, 0:8]), [('ps', b)], ['small'])
            V(lambda e: e.tensor_tensor(out=t6[:], in0=small[:, 0:6], in1=pvb[:, 512:518], op=ALU.add), ['small', 'pvb'], ['t6'])
            A(lambda e: e.activation(out=t6[:], in_=t6[:], func=AF.Exp), ['t6'], ['t6'])
            V(lambda e: e.tensor_scalar(out=t6[:], in0=t6[:], scalar1=1.0, scalar2=None, op0=ALU.add), ['t6'], ['t6'])
            A(lambda e: e.activation(out=sp6[:], in_=t6[:], func=AF.Ln), ['t6'], ['sp6'])
            V(lambda e: e.tensor_tensor(out=g6[:], in0=sp6[:], in1=ah6[:], op=ALU.mult), ['sp6', 'ah6'], ['g6'])
            A(lambda e: e.activation(out=beta[:], in_=small[:, 6:8], func=AF.Sigmoid), ['small'], ['beta'])
            for i in range(4):
                A(lambda e, i=i: e.activation(out=junk[:, 0:128], in_=qk[:, i * 128:(i + 1) * 128], func=AF.Square, accum_out=ss4[:, i:i + 1]),
                  ['qk'], ['junk', ('ss4', i)])
            V(lambda e: e.tensor_scalar(out=ss4[:, 0:4], in0=ss4[:, 0:4], scalar1=NORM_EPS, scalar2=None, op0=ALU.add),
              [('ss4', i) for i in range(4)], [('ss4', i) for i in range(4)])
            A(lambda e: e.activation(out=ss4[:, 0:4], in_=ss4[:, 0:4], func=AF.Sqrt), [('ss4', i) for i in range(4)], [('ss4', i) for i in range(4)])
            V(lambda e: e.reciprocal(out=rn4[:, 0:4], in_=ss4[:, 0:4]), [('ss4', i) for i in range(4)], ['rn4'])
            for i in range(4):
                sc = 128.0 ** -0.5 if i < 2 else 1.0
                V(lambda e, i=i, sc=sc: e.tensor_scalar(out=qk[:, i * 128:(i + 1) * 128], in0=qk[:, i * 128:(i + 1) * 128],
                                                        scalar1=rn4[:, i:i + 1], scalar2=sc, op0=ALU.mult, op1=ALU.mult),
                  ['qk', 'rn4'], ['qk'])
            p1 = newps()
            PE(lambda e, p1=p1: e.matmul(ps[p1][:, 0:6], lhsT=tri, rhs=g6[:], start=True, stop=True), ['cst', 'g6'], [('ps', p1)])
            PE(lambda e, p1=p1: e.matmul(ps[p1][:, 8:14], lhsT=ones, rhs=g6[:], start=True, stop=True), ['cst', 'g6'], [('ps', p1)])
            V(lambda e, p1=p1: e.tensor_copy(out=gc6[:], in_=ps[p1][:, 0:6]), [('ps', p1)], ['gc6'])
            V(lambda e, p1=p1: e.tensor_copy(out=gl6[:], in_=ps[p1][:, 8:14]), [('ps', p1)], ['gl6'])
            A(lambda e: e.activation(out=egc6[:], in_=gc6[:], func=AF.Exp), ['gc6'], ['egc6'])
            A(lambda e: e.activation(out=egl6[:], in_=gl6[:], func=AF.Exp), ['gl6'], ['egl6'])
            V(lambda e: e.tensor_tensor(out=kt6[:], in0=gl6[:], in1=gc6[:], op=ALU.subtract), ['gl6', 'gc6'], ['kt6'])
            A(lambda e: e.activation(out=kt6[:], in_=kt6[:], func=AF.Exp), ['kt6'], ['kt6'])
            for grp, hs in enumerate([(0, 1, 2, 3), (4, 5)]):
                for h in hs:
                    V(lambda e, h=h: e.tensor_scalar(out=triG[:, h, :], in0=tri, scalar1=g6[:, h:h + 1], scalar2=None, op0=ALU.mult),
                      ['cst', 'g6'], [('triG', grp)])
                pd = newps()
                n0, n1 = hs[0], hs[-1] + 1
                PE(lambda e, pd=pd, n0=n0, n1=n1: e.matmul(ps[pd][:, 0:(n1 - n0) * 128], lhsT=ones, rhs=triG[:, n0:n1, :].rearrange("p a b -> p (a b)"),
                                                           start=True, stop=True), ['cst', ('triG', grp)], [('ps', pd)])
                for h in hs:
                    V(lambda e, h=h, pd=pd, n0=n0: e.tensor_scalar(out=decT[:, h, :], in0=ps[pd][:, (h - n0) * 128:(h - n0 + 1) * 128],
                                                                   scalar1=gc6[:, h:h + 1], scalar2=0.0, op0=ALU.subtract, op1=ALU.min),
                      [('ps', pd), 'gc6'], [('decT', h)])
                    A(lambda e, h=h: e.activation(out=decT[:, h, :], in_=decT[:, h, :], func=AF.Exp), [('decT', h)], [('decT', h)])
                    V(lambda e, h=h: e.tensor_tensor(out=decT[:, h, :], in0=decT[:, h, :], in1=tri, op=ALU.mult), [('decT', h), 'cst'], [('decT', h)])
            for h in range(2):
                V(lambda e, h=h: e.tensor_scalar(out=kb[:, h * 128:(h + 1) * 128], in0=qk[:, 256 + h * 128:256 + (h + 1) * 128],
                                                 scalar1=beta[:, h:h + 1], scalar2=None, op0=ALU.mult), ['qk', 'beta'], [('kb', h)])
                V(lambda e, h=h: e.tensor_scalar(out=Rr[h][:, 0:128], in0=vv[:, h * 128:(h + 1) * 128], scalar1=beta[:, h:h + 1], scalar2=None,
                                                 op0=ALU.mult), ['vv', 'beta'], [('R', h)])
                V(lambda e, h=h: e.tensor_scalar(out=Rr[h][:, 128:256], in0=kb[:, h * 128:(h + 1) * 128], scalar1=egc6[:, 4 + h:5 + h], scalar2=None,
                                                 op0=ALU.mult), [('kb', h), 'egc6'], [('R', h)])
            for h in range(4):
                V(lambda e, h=h: e.tensor_scalar(out=xd[:, h * 64:(h + 1) * 64], in0=xbc[:, h * 64:(h + 1) * 64], scalar1=sp6[:, h:h + 1], scalar2=None,
                                                 op0=ALU.mult), ['xbc', 'sp6'], [('xd', h)])
            pa, pb = newps(), newps()
            srcs1 = [(qk, 256, 'qk'), (qk, 384, 'qk'), (qk, 0, 'qk'), (qk, 128, 'qk')]
            srcs2 = [(kb, 0, ('kb', 0)), (kb, 128, ('kb', 1)), (xbc, 256, 'xbc'), (xbc, 384, 'xbc')]
            for pp, srcs, dst, dk in [(pa, srcs1, T1, 'T1'), (pb, srcs2, T2, 'T2')]:
                for i, (src, c0, key) in enumerate(srcs):
                    PE(lambda e, pp=pp, i=i, src=src, c0=c0: e.transpose(out=ps[pp][:, i * 128:(i + 1) * 128], in_=src[:, c0:c0 + 128], identity=ident),
                       [key, 'cst'], [('ps', pp)])
                A(lambda e, pp=pp, dst=dst: e.activation(out=dst[:].rearrange("p a b -> p (a b)"), in_=ps[pp][:], func=AF.Identity), [('ps', pp)], [dk])
            for h in range(2):
                pn = newps()
                PE(lambda e, pn=pn, h=h: e.matmul(ps[pn][:, 0:128], lhsT=T1[:, h, :], rhs=T2[:, h, :], start=True, stop=True), ['T1', 'T2'], [('ps', pn)])
                PE(lambda e, pn=pn, h=h: e.matmul(ps[pn][:, 128:256], lhsT=T1[:, h, :], rhs=T1[:, 2 + h, :], start=True, stop=True), ['T1'], [('ps', pn)])
                V(lambda e, pn=pn, h=h: e.tensor_tensor(out=tmpA[4 + h][:], in0=ps[pn][:, 0:128], in1=decT[:, 4 + h, :], op=ALU.mult),
                  [('ps', pn), ('decT', 4 + h)], [('tmpA', 4 + h)])
                V(lambda e, h=h: e.scalar_tensor_tensor(out=NTm[h][0][:], in0=tmpA[4 + h][:], scalar=-1.0, in1=strictm, op0=ALU.mult, op1=ALU.mult),
                  [('tmpA', 4 + h), 'cst'], [('NT', h, 0)])
                V(lambda e, pn=pn, h=h: e.tensor_tensor(out=attnT[:, 4 + h, :], in0=ps[pn][:, 128:256], in1=decT[:, 4 + h, :], op=ALU.mult),
                  [('ps', pn), ('decT', 4 + h)], [('attnT', 4 + h)])
                pt = newps()
                PE(lambda e, pt=pt, h=h: e.transpose(out=ps[pt][:, 0:128], in_=NTm[h][0][:], identity=ident), [('NT', h, 0), 'cst'], [('ps', pt)])
                A(lambda e, pt=pt, h=h: e.activation(out=Nm[h][0][:], in_=ps[pt][:, 0:128], func=AF.Identity), [('ps', pt)], [('N', h, 0)])
            for lvl in range(7):
                cur = lvl % 2
                nx = 1 - cur
                for h in range(2):
                    pr = newps()
                    PE(lambda e, pr=pr, cur=cur, h=h: e.matmul(ps[pr][:, 0:256], lhsT=NTm[h][cur][:], rhs=Rr[h][:], start=True, stop=True),
                       [('NT', h, cur), ('R', h)], [('ps', pr)])
                    if lvl < 6:
                        PE(lambda e, pr=pr, cur=cur, h=h: e.matmul(ps[pr][:, 256:384], lhsT=NTm[h][cur][:], rhs=Nm[h][cur][:], start=True, stop=True),
                           [('NT', h, cur), ('N', h, cur)], [('ps', pr)])
                        PE(lambda e, pr=pr, cur=cur, h=h: e.matmul(ps[pr][:, 384:512], lhsT=Nm[h][cur][:], rhs=NTm[h][cur][:], start=True, stop=True),
                           [('NT', h, cur), ('N', h, cur)], [('ps', pr)])
                    V(lambda e, pr=pr, h=h: e.tensor_tensor(out=Rr[h][:], in0=Rr[h][:], in1=ps[pr][:, 0:256], op=ALU.add), [('R', h), ('ps', pr)], [('R', h)])
                    if lvl < 6:
                        A(lambda e, pr=pr, nx=nx, h=h: e.activation(out=Nm[h][nx][:], in_=ps[pr][:, 256:384], func=AF.Identity), [('ps', pr)], [('N', h, nx)])
                        V(lambda e, pr=pr, nx=nx, h=h: e.tensor_copy(out=NTm[h][nx][:], in_=ps[pr][:, 384:512]), [('ps', pr)], [('NT', h, nx)])
            pc = newps()
            PE(lambda e, pc=pc: e.matmul(ps[pc][:, 0:128], lhsT=T2[:, 2, :], rhs=T2[:, 3, :], start=True, stop=True), ['T2'], [('ps', pc)])
            A(lambda e, pc=pc: e.activation(out=CBT[:], in_=ps[pc][:, 0:128], func=AF.Identity), [('ps', pc)], ['CBT'])
            for h in range(4):
                V(lambda e, h=h: e.tensor_tensor(out=attnT[:, h, :], in0=CBT[:], in1=decT[:, h, :], op=ALU.mult), ['CBT', ('decT', h)], [('attnT', h)])
                V(lambda e, h=h: e.tensor_scalar(out=ktail[h][:], in0=xbc[:, 256:384], scalar1=kt6[:, h:h + 1], scalar2=None, op0=ALU.mult),
                  ['xbc', 'kt6'], [('ktail', h)])
            for h in range(2):
                V(lambda e, h=h: e.tensor_scalar(out=ktail[4 + h][:], in0=qk[:, 256 + h * 128:256 + (h + 1) * 128], scalar1=kt6[:, 4 + h:5 + h], scalar2=None,
                                                 op0=ALU.mult), ['qk', 'kt6'], [('ktail', 4 + h)])
            pos = {}
            for h in range(4):
                po = newps()
                pos[h] = po
                PE(lambda e, po=po, h=h: e.matmul(ps[po][:, 0:64], lhsT=T2[:, 3, :], rhs=Ss[h][:], start=True, stop=True), ['T2', ('Ss', h)], [('ps', po)])
                PE(lambda e, po=po, h=h: e.matmul(ps[po][:, 64:128], lhsT=attnT[:, h, :], rhs=xd[:, h * 64:(h + 1) * 64], start=True, stop=True),
                   [('attnT', h), ('xd', h)], [('ps', po)])
                PE(lambda e, po=po, h=h: e.matmul(ps[po][:, 128:192], lhsT=ktail[h][:], rhs=xd[:, h * 64:(h + 1) * 64], start=True, stop=True),
                   [('ktail', h), ('xd', h)], [('ps', po)])
            for h in range(4):
                po = pos[h]
                A(lambda e, po=po, h=h: e.activation(out=tmpA[h][:, 0:64], in_=ps[po][:, 64:128], func=AF.Identity), [('ps', po)], [('tmpA', h)])
                V(lambda e, po=po, h=h: e.scalar_tensor_tensor(out=oo[:, h * 64:(h + 1) * 64], in0=ps[po][:, 0:64], scalar=egc6[:, h:h + 1], in1=tmpA[h][:, 0:64],
                                                               op0=ALU.mult, op1=ALU.add), [('ps', po), 'egc6', ('tmpA', h)], [('oo', 0)])
                V(lambda e, po=po, h=h: e.scalar_tensor_tensor(out=Ss[h][:], in0=Ss[h][:], scalar=egl6[:, h:h + 1], in1=ps[po][:, 128:192],
                                                               op0=ALU.mult, op1=ALU.add), [('Ss', h), 'egl6', ('ps', po)], [('Ss', h)])
                V(lambda e, h=h: e.scalar_tensor_tensor(out=oo[:, h * 64:(h + 1) * 64], in0=xbc[:, h * 64:(h + 1) * 64], scalar=pvb[:, 524 + h:525 + h],
                                                        in1=oo[:, h * 64:(h + 1) * 64], op0=ALU.mult, op1=ALU.add), ['xbc', 'pvb', ('oo', 0)], [('oo', 0)])
            for h in range(2):
                pw = newps()
                PE(lambda e, pw=pw, h=h: e.transpose(out=ps[pw][:, 0:128], in_=Rr[h][:, 128:256], identity=ident), [('R', h), 'cst'], [('ps', pw)])
                A(lambda e, pw=pw, h=h: e.activation(out=wT[h][:], in_=ps[pw][:, 0:128], func=AF.Identity), [('ps', pw)], [('wT', h)])
            pvs = {}
            for h in range(2):
                pv_ = newps()
                pvs[h] = pv_
                PE(lambda e, pv_=pv_, h=h: e.matmul(ps[pv_][:, 0:128], lhsT=wT[h][:], rhs=Sg[h][:], start=True, stop=True), [('wT', h), ('Sg', h)], [('ps', pv_)])
                PE(lambda e, pv_=pv_, h=h: e.matmul(ps[pv_][:, 128:256], lhsT=T1[:, 2 + h, :], rhs=Sg[h][:], start=True, stop=True), ['T1', ('Sg', h)], [('ps', pv_)])
                V(lambda e, pv_=pv_, h=h: e.tensor_tensor(out=vnew[h][:], in0=Rr[h][:, 0:128], in1=ps[pv_][:, 0:128], op=ALU.subtract),
                  [('R', h), ('ps', pv_)], [('vnew', h)])
            for h in range(2):
                pv_ = pvs[h]
                po = newps()
                PE(lambda e, po=po, h=h: e.matmul(ps[po][:, 0:128], lhsT=attnT[:, 4 + h, :], rhs=vnew[h][:], start=True, stop=True),
                   [('attnT', 4 + h), ('vnew', h)], [('ps', po)])
                PE(lambda e, po=po, h=h: e.matmul(ps[po][:, 128:256], lhsT=ktail[4 + h][:], rhs=vnew[h][:], start=True, stop=True),
                   [('ktail', 4 + h), ('vnew', h)], [('ps', po)])
                A(lambda e, po=po, h=h: e.activation(out=tmpA[4 + h][:], in_=ps[po][:, 0:128], func=AF.Identity), [('ps', po)], [('tmpA', 4 + h)])
                V(lambda e, pv_=pv_, h=h: e.scalar_tensor_tensor(out=oo[:, 256 + h * 128:256 + (h + 1) * 128], in0=ps[pv_][:, 128:256], scalar=egc6[:, 4 + h:5 + h],
                                                                 in1=tmpA[4 + h][:], op0=ALU.mult, op1=ALU.add), [('ps', pv_), 'egc6', ('tmpA', 4 + h)], [('oo', 2 + h)])
                V(lambda e, po=po, h=h: e.scalar_tensor_tensor(out=Sg[h][:], in0=Sg[h][:], scalar=egl6[:, 4 + h:5 + h], in1=ps[po][:, 128:256],
                                                               op0=ALU.mult, op1=ALU.add), [('Sg', h), 'egl6', ('ps', po)], [('Sg', h)])
            V(lambda e: e.tensor_tensor(out=oo[:, 0:256], in0=oo[:, 0:256], in1=zz[:, 0:256], op=ALU.mult), [('oo', 0), 'zz'], [('oo', 0)])
            groups = [(0, 256, ('oo', 0), 4), (256, 384, ('oo', 2), 5), (384, 512, ('oo', 3), 6)]
            for c0, c1, key, si in groups:
                A(lambda e, c0=c0, c1=c1, si=si: e.activation(out=junk[:, 0:c1 - c0], in_=oo[:, c0:c1], func=AF.Square, scale=(1.0 / (c1 - c0)) ** 0.5,
                                                              accum_out=ss4[:, si:si + 1]),
                  [key], ['junk', ('ss4', si)])
                V(lambda e, si=si: e.tensor_scalar(out=ss4[:, si:si + 1], in0=ss4[:, si:si + 1], scalar1=NORM_EPS, scalar2=None, op0=ALU.add),
                  [('ss4', si)], [('ss4', si)])
                A(lambda e, si=si: e.activation(out=ss4[:, si:si + 1], in_=ss4[:, si:si + 1], func=AF.Sqrt), [('ss4', si)], [('ss4', si)])
                V(lambda e, si=si: e.reciprocal(out=rn4[:, si:si + 1], in_=ss4[:, si:si + 1]), [('ss4', si)], [('rn4b', si)])
                nw0 = 528 if c0 == 0 else 784
                V(lambda e, c0=c0, c1=c1, si=si, nw0=nw0: e.scalar_tensor_tensor(out=outt[:, c0:c1], in0=oo[:, c0:c1], scalar=rn4[:, si:si + 1],
                                                                                 in1=pvb[:, nw0:nw0 + (c1 - c0)], op0=ALU.mult, op1=ALU.mult),
                  [key, ('rn4b', si), 'pvb'], [('outt', c0)])
            V(lambda e: e.tensor_tensor(out=outt[:, 256:512], in0=outt[:, 256:512], in1=zz[:, 256:512], op=ALU.mult),
              [('outt', 256), ('outt', 384), 'zz'], [('outt', 256), ('outt', 384)])
            S.op('sync', lambda e, t0=t0: e.dma_start(out=out[t0:t0 + 128, :], in_=outt[:]), reads=[('outt', 0), ('outt', 256), ('outt', 384)],
                 writes=[('out', t)], dma=True, dkey='outst')
        S.emit()
    return S


def mix0_consts():
    i = np.arange(128)
    ident = np.eye(128, dtype=np.float32)
    tri = (i[:, None] <= i[None, :]).astype(np.float32)
    strict = (i[:, None] < i[None, :]).astype(np.float32)
    ones = np.ones((128, 128), np.float32)
    return np.stack([ident, tri, strict, ones])


def mix0_inputs(xb, g, w_in, ssd_conv_w, ssd_conv_b, ssd_dt_bias, ssd_a_log, ssd_d, ssd_norm_w, gdn_conv_w, gdn_a_log, gdn_dt_bias, gdn_norm_w):
    T = xb.shape[0]
    xTp = np.zeros((D, T + 3), np.float32)
    xTp[:, 3:] = xb.T
    o_zs, o_xbc, o_dt, o_qkv, o_zg, o_b, o_a = np.cumsum([0, 1024, 2048, 16, 3072, 1024, 8])
    r = lambda a, n: np.arange(a, a + n)
    ssd_x = r(g * 256, 256)
    ssd_B = r(1024 + g * 128, 128)
    ssd_C = r(1536 + g * 128, 128)
    gq = r(g * 256, 256)
    gk = r(1024 + g * 256, 256)
    gv = r(2048 + g * 256, 256)
    conv_cols = np.concatenate([o_xbc + ssd_x, o_xbc + ssd_B, o_xbc + ssd_C, o_qkv + gq, o_qkv + gk, o_qkv + gv])
    Wc = w_in[:, conv_cols]
    cw = np.concatenate([ssd_conv_w[:, np.concatenate([ssd_x, ssd_B, ssd_C])], gdn_conv_w[:, np.concatenate([gq, gk, gv])]], axis=1)
    plain_cols = np.concatenate([o_zs + r(g * 256, 256), o_zg + r(g * 256, 256), o_dt + r(g * 4, 4), o_a + r(g * 2, 2), o_b + r(g * 2, 2)])
    Wp = w_in[:, plain_cols]
    pv = np.concatenate([ssd_conv_b[np.concatenate([ssd_x, ssd_B, ssd_C])], ssd_dt_bias[g * 4:g * 4 + 4], gdn_dt_bias[g * 2:g * 2 + 2],
                         ssd_a_log[g * 4:g * 4 + 4], gdn_a_log[g * 2:g * 2 + 2], ssd_d[g * 4:g * 4 + 4], ssd_norm_w[g * 256:(g + 1) * 256], gdn_norm_w])
    assert pv.shape[0] == NPV
    return {"xTp": xTp, "Wc": np.ascontiguousarray(Wc), "cw": np.ascontiguousarray(cw.reshape(-1)), "Wp": np.ascontiguousarray(Wp),
            "pv": np.ascontiguousarray(pv.astype(np.float32)), "consts": mix0_consts()}


D = 1024
NORM_EPS = 1e-6
RW_GN_EPS = 64e-5
NEG = -1.0e30


def phase_mix1(nc, G, T, Wd, x1o, x1g, mxo, strict=True, do_rwkv=True, do_moba=True):
    NTL = T // 128
    Wr, mu, Wm, pv, lw2, la2, lg2, consts, esel_d, cmask_d, kTs, v1s = (Wd[k] for k in
        ['Wr', 'mu', 'Wm', 'pv1', 'lw2', 'la2', 'lg2', 'consts', 'esel', 'cmask', 'kTs', 'v1s'])
    out = mxo

    S = Sched(nc, strict=strict, G=G)
    with ExitStack() as es:
        sb = lambda name, shape, dty=F32: es.enter_context(nc.sbuf_tensor("ph%d_%s" % (G.phase + 1, name), shape, dty))
        wr = sb("wr", [128, 8, 2, 1056], BF16)
        wm = sb("wm", [128, 8, 768], BF16)
        mub = sb("mub", [128, 1056])
        stage = sb("stage", [128, 1056])
        stage2 = sb("stage2", [128, 1056])
        pvb = sb("pvb", [128, 1792])
        cst = sb("cst", [128, 4, 128])
        ident, tri, strictm, ones = (cst[:, i, :] for i in range(4))
        identb = sb("identb", [128, 128], BF16)
        w2sb = sb("w2sb", [64, 256])
        a2sb = sb("a2sb", [64, 256])
        g2sb = sb("g2sb", [128, 256])
        g2sb2 = sb("g2sb2", [32, 256])
        xt = sb("xt", [128, 8, 129], BF16)
        rk = sb("rk", [128, 512])
        vv = sb("vv", [128, 256])
        L1 = sb("L1", [64, 128])
        L1b = sb("L1b", [64, 128])
        sgT = sb("sgT", [128, 128])
        sgT2 = sb("sgT2", [32, 128])
        lw = sb("lw", [128, 256])
        aa = sb("aa", [128, 256])
        gate = sb("gate", [128, 256])
        kk = sb("kk", [128, 256])
        kmod = sb("kmod", [128, 256])
        bq = sb("bq", [128, 256])
        ss = sb("ss", [128, 8])
        rn = sb("rn", [128, 8])
        junk = sb("junk", [128, 256])
        LDs = sb("LDs", [128, 256])
        E = [sb("E%d" % i, [128, 256]) for i in range(4)]
        d3 = sb("d3", [128, 256])
        rt = sb("rt", [128, 256])
        at = sb("at", [128, 256])
        bt = sb("bt", [128, 256])
        kt = sb("kt", [128, 256])
        bh = sb("bh", [128, 256])
        kh = sb("kh", [128, 256])
        XT = [sb("XT%d" % h, [64, 512]) for h in range(4)]
        AT = [sb("AT%d" % h, [128, 256]) for h in range(4)]
        AakT = [sb("AakT%d" % h, [128, 128]) for h in range(4)]
        NTm = [[sb("NT%d_%d" % (h, i), [128, 128]) for i in range(2)] for h in range(4)]
        Nm = [[sb("N%d_%d" % (h, i), [128, 128]) for i in range(2)] for h in range(4)]
        Rr = [sb("R%d" % h, [128, 128]) for h in range(4)]
        Wmm = sb("Wmm", [128, 128])
        WmT = [sb("WmT%d" % h, [64, 128]) for h in range(4)]
        Up = sb("Up", [128, 256])
        Hp = [sb("Hp%d" % h, [64, 64]) for h in range(4)]
        DC = [sb("DC%d" % h, [64, 1]) for h in range(4)]
        yraw = sb("yraw", [128, 256])
        stats = sb("stats", [128, 4, 6])
        mv = sb("mv", [128, 4, 2])
        rkb = sb("rkb", [128, 4])
        outr = sb("outr", [128, 256])
        outrb = sb("outrb", [128, 256], BF16)
        xtok = sb("xtok", [128, 1024])
        esel = sb("eselsb", [64, 64, 128], BF16)
        cmask = sb("cmasksb", [128, 4, 512], BF16)
        qTf = sb("qTf", [128, 256])
        qg = [sb("qg%d" % h, [128, 512], BF16) for h in range(2)]
        kTt = sb("kTt", [128, 256], BF16)
        v1t = sb("v1t", [128, 2, 129], BF16)
        kpart = sb("kpart", [128, 2])
        kacc = sb("kacc", [128, 2])
        kmT = sb("kmT", [128, 2, 64])
        gm = [sb("gm%d" % h, [128, 64]) for h in range(2)]
        m8 = [sb("m8_%d" % h, [128, 8]) for h in range(2)]
        bias = [sb("bias%d" % h, [128, 64]) for h in range(2)]
        biasT = [sb("biasT%d" % h, [64, 512], BF16) for h in range(2)]
        kcb = [sb("kcb%d" % i, [128, 512], BF16) for i in range(2)]
        vcb = [sb("vcb%d" % i, [128, 4, 129], BF16) for i in range(2)]
        PT = [sb("PT%d" % i, [128, 512], BF16) for i in range(2)]
        rec = sb("rec", [128, 4])
        mo = sb("mo", [128, 4, 256], BF16)
        ps = G.ps
        rr = [0]

        def newps():
            rr[0] = (rr[0] + 1) % 2
            return 6 + rr[0]

        rr2 = [0]

        def newps_r():
            rr2[0] = (rr2[0] + 1) % 6
            return 2 + rr2[0]

        V = lambda fn, r, w: S.op('vector', fn, r, w)
        A = lambda fn, r, w: S.op('scalar', fn, r, w)
        PE = lambda fn, r, w: S.op('tensor', fn, r, w, acc=True)

        TQ = x1o.shape[0]
        XR = 256
        NCHX = TQ // XR
        x1g4 = x1g.rearrange("(i r t) c -> i r t c", i=NCHX, r=4)
        for i in range(NCHX):
            S.op('gpsimd', lambda e, i=i: e.collective_compute('AllGather', ALU.bypass, replica_groups=[[0, 1, 2, 3], [4, 5, 6, 7]],
                                                               ins=[x1o[i * XR:(i + 1) * XR, :]], outs=[x1g[i * 4 * XR:(i + 1) * 4 * XR, :]]),
                 writes=[('x1g', i)], cc=True, dkey='ccx')
        S.op('sync', lambda e: e.dma_start(out=cst[:], in_=consts.rearrange("c p f -> p c f")), writes=['cst'], dma=True)
        S.op('sync', lambda e: e.dma_start(out=pvb[:], in_=pv.partition_broadcast(128)), writes=['pvb'], dma=True)
        S.op('sync', lambda e: e.dma_start(out=mub[:], in_=mu.partition_broadcast(128)), writes=['mub'], dma=True)
        S.op('sync', lambda e: e.dma_start(out=w2sb[:], in_=lw2), writes=['w2sb'], dma=True)
        S.op('sync', lambda e: e.dma_start(out=a2sb[:], in_=la2), writes=['a2sb'], dma=True)
        S.op('sync', lambda e: e.dma_start(out=g2sb[:], in_=lg2[0:128, :]), writes=['g2sb'], dma=True)
        S.op('sync', lambda e: e.dma_start(out=g2sb2[:], in_=lg2[128:160, :]), writes=['g2sb2'], dma=True)
        S.op('gpsimd', lambda e: e.dma_start(out=wm[:], in_=Wm.rearrange("(k p) c -> p k c", p=128)), writes=['wm'], dma=True)
        S.op('gpsimd', lambda e: e.dma_start(out=esel[:].rearrange("p a b -> p (a b)"), in_=esel_d), writes=['esel'], dma=True)
        S.op('gpsimd', lambda e: e.dma_start(out=cmask[:].rearrange("p a b -> p (a b)"), in_=cmask_d), writes=['cmask'], dma=True)
        V(lambda e: e.tensor_copy(out=identb[:], in_=ident), ['cst'], ['identb'])
        for k in range(8):
            S.op('sync', lambda e, k=k: e.dma_start(out=stage[:], in_=Wr[k * 128:(k + 1) * 128, :]), writes=['stage'], dma=True)
            V(lambda e: e.tensor_tensor(out=stage2[:], in0=stage[:], in1=mub[:], op=ALU.mult), ['stage', 'mub'], ['stage2'])
            V(lambda e, k=k: e.tensor_copy(out=wr[:, k, 0, :], in_=stage2[:]), ['stage2'], [('wr', k)])
            V(lambda e, k=k: e.tensor_tensor(out=wr[:, k, 1, :], in0=stage[:], in1=stage2[:], op=ALU.subtract), ['stage', 'stage2'], [('wr', k)])
        for p in range(4):
            V(lambda e, p=p: e.memset(Hp[p][:], 0.0), [], [('Hp', p)])
        V(lambda e: e.memset(kmT[:], 0.0), [], ['kmT'])
        V(lambda e: e.memset(v1t[:], 1.0), [], ['v1t'])

        def sumsq_rn(src, skey, n, width, scale2, eps, col0):
            for i in range(n):
                A(lambda e, i=i: e.activation(out=junk[:, 0:width], in_=src[:, i * width:(i + 1) * width], func=AF.Square, scale=scale2 ** 0.5,
                                              accum_out=ss[:, col0 + i:col0 + i + 1]), [skey], ['junk', ('ss', col0 + i)])
            keys = [('ss', col0 + i) for i in range(n)]
            V(lambda e: e.tensor_scalar(out=ss[:, col0:col0 + n], in0=ss[:, col0:col0 + n], scalar1=eps, scalar2=None, op0=ALU.add), keys, keys)
            A(lambda e: e.activation(out=ss[:, col0:col0 + n], in_=ss[:, col0:col0 + n], func=AF.Sqrt), keys, keys)
            V(lambda e: e.reciprocal(out=rn[:, col0:col0 + n], in_=ss[:, col0:col0 + n]), keys, [('rn', col0)])

        for t in range(NTL):
            t0 = t * 128
            S.op('sync', lambda e, t0=t0: e.dma_start(out=xtok[:], in_=x1g4[(t0 % TQ) // XR, t0 // TQ, (t0 % TQ) % XR:(t0 % TQ) % XR + 128, :]),
                 reads=[('x1g', i_) for i_ in range(NCHX)], writes=['xtok'], dma=True)
            if t == 0:
                V(lambda e: e.memset(xt[:, :, 0:1], 0.0), [], ['xt'])
            else:
                V(lambda e: e.tensor_copy(out=xt[:, :, 0:1], in_=xt[:, :, 128:129]), ['xt'], ['xt'])
            for hh in range(2):
                px0 = newps()
                for c4 in range(4):
                    c = hh * 4 + c4
                    PE(lambda e, px0=px0, c4=c4, c=c: e.transpose(out=ps[px0][:, c4 * 128:(c4 + 1) * 128], in_=xtok[:, c * 128:(c + 1) * 128], identity=ident),
                       ['xtok', 'cst'], [('ps', px0)])
                A(lambda e, px0=px0, hh=hh: e.activation(out=xt[:, hh * 4:(hh + 1) * 4, 1:129], in_=ps[px0][:].rearrange("p (c t) -> p c t", c=4), func=AF.Identity),
                  [('ps', px0)], ['xt'])
            if do_rwkv:
                for bi, (c0, c1) in enumerate([(0, 512), (512, 768)]):
                    n = 0
                    for j in range(2):
                        for k in range(8):
                            PE(lambda e, bi=bi, c0=c0, c1=c1, j=j, k=k, n=n: e.matmul(ps[bi][:, 0:c1 - c0], lhsT=xt[:, k, j:j + 128], rhs=wr[:, k, j, c0:c1],
                                                                                      start=(n == 0), stop=(n == 15)), ['xt', ('wr', k)], [('ps', bi)])
                            n += 1
                A(lambda e: e.activation(out=rk[:], in_=ps[0][:], func=AF.Identity), [('ps', 0)], ['rk'])
                V(lambda e: e.tensor_copy(out=vv[:], in_=ps[1][:, 0:256]), [('ps', 1)], ['vv'])
                for bi, (c0, c1) in enumerate([(768, 832), (832, 896), (896, 1024), (1024, 1056)]):
                    b = bi % 2
                    n = 0
                    for j in range(2):
                        for k in range(8):
                            PE(lambda e, b=b, c0=c0, c1=c1, j=j, k=k, n=n: e.matmul(ps[b][0:c1 - c0, 0:128], lhsT=wr[:, k, j, c0:c1], rhs=xt[:, k, j:j + 128],
                                                                                    start=(n == 0), stop=(n == 15)), ['xt', ('wr', k)], [('ps', b)])
                            n += 1
                    if bi == 0:
                        A(lambda e, b=b: e.activation(out=L1[:], in_=ps[b][0:64, 0:128], func=AF.Tanh), [('ps', b)], [('L1', 0)])
                    elif bi == 1:
                        A(lambda e, b=b: e.activation(out=L1b[:], in_=ps[b][0:64, 0:128], func=AF.Identity), [('ps', b)], [('L1', 1)])
                    elif bi == 2:
                        A(lambda e, b=b: e.activation(out=sgT[:], in_=ps[b][:, 0:128], func=AF.Sigmoid), [('ps', b)], ['sgT'])
                    else:
                        A(lambda e, b=b: e.activation(out=sgT2[:], in_=ps[b][0:32, 0:128], func=AF.Sigmoid), [('ps', b)], ['sgT2'])
                pl = newps_r()
                PE(lambda e, pl=pl: e.matmul(ps[pl][:, 0:256], lhsT=L1[:], rhs=w2sb[:], start=True, stop=True), [('L1', 0), 'w2sb'], [('ps', pl)])
                PE(lambda e, pl=pl: e.matmul(ps[pl][:, 256:512], lhsT=L1b[:], rhs=a2sb[:], start=True, stop=True), [('L1', 1), 'a2sb'], [('ps', pl)])
                pg_ = newps_r()
                PE(lambda e, pg_=pg_: e.matmul(ps[pg_][:, 0:256], lhsT=sgT[:], rhs=g2sb[:], start=True, stop=False), ['sgT', 'g2sb'], [('ps', pg_)])
                PE(lambda e, pg_=pg_: e.matmul(ps[pg_][:, 0:256], lhsT=sgT2[:], rhs=g2sb2[:], start=False, stop=True), ['sgT2', 'g2sb2'], [('ps', pg_)])
                V(lambda e, pl=pl: e.tensor_tensor(out=lw[:], in0=ps[pl][:, 0:256], in1=pvb[:, 0:256], op=ALU.add), [('ps', pl), 'pvb'], ['lw'])
                A(lambda e: e.activation(out=lw[:], in_=lw[:], func=AF.Sigmoid), ['lw'], ['lw'])
                V(lambda e: e.tensor_scalar(out=lw[:], in0=lw[:], scalar1=-float(np.exp(-0.5)), scalar2=None, op0=ALU.mult), ['lw'], ['lw'])
                V(lambda e, pl=pl: e.tensor_tensor(out=aa[:], in0=ps[pl][:, 256:512], in1=pvb[:, 256:512], op=ALU.add), [('ps', pl), 'pvb'], ['aa'])
                A(lambda e: e.activation(out=aa[:], in_=aa[:], func=AF.Sigmoid), ['aa'], ['aa'])
                A(lambda e, pg_=pg_: e.activation(out=gate[:], in_=ps[pg_][:, 0:256], func=AF.Identity), [('ps', pg_)], ['gate'])
                V(lambda e: e.tensor_tensor(out=kk[:], in0=rk[:, 256:512], in1=pvb[:, 512:768], op=ALU.mult), ['rk', 'pvb'], ['kk'])
                sumsq_rn(kk, 'kk', 4, 64, 1.0, NORM_EPS, 0)
                for h in range(4):
                    V(lambda e, h=h: e.tensor_scalar(out=kk[:, h * 64:(h + 1) * 64], in0=kk[:, h * 64:(h + 1) * 64], scalar1=rn[:, h:h + 1], scalar2=None,
                                                     op0=ALU.mult), ['kk', ('rn', 0)], ['kk'])
                V(lambda e: e.scalar_tensor_tensor(out=kmod[:], in0=aa[:], scalar=-1.0, in1=pvb[:, 768:1024], op0=ALU.add, op1=ALU.mult), ['aa', 'pvb'], ['kmod'])
                V(lambda e: e.scalar_tensor_tensor(out=kmod[:], in0=kmod[:], scalar=1.0, in1=rk[:, 256:512], op0=ALU.add, op1=ALU.mult), ['kmod', 'rk'], ['kmod'])
                V(lambda e: e.tensor_tensor(out=bq[:], in0=kk[:], in1=aa[:], op=ALU.mult), ['kk', 'aa'], ['bq'])
                pd = newps_r()
                PE(lambda e, pd=pd: e.matmul(ps[pd][:, 0:256], lhsT=tri, rhs=lw[:], start=True, stop=True), ['cst', 'lw'], [('ps', pd)])
                PE(lambda e, pd=pd: e.matmul(ps[pd][:, 256:512], lhsT=ones, rhs=lw[:], start=True, stop=True), ['cst', 'lw'], [('ps', pd)])
                V(lambda e, pd=pd: e.tensor_copy(out=LDs[:], in_=ps[pd][:, 0:256]), [('ps', pd)], ['LDs'])
                V(lambda e, pd=pd: e.tensor_tensor(out=d3[:], in0=ps[pd][:, 256:512], in1=LDs[:], op=ALU.subtract), [('ps', pd), 'LDs'], ['d3'])
                A(lambda e: e.activation(out=E[0][:], in_=LDs[:], func=AF.Exp), ['LDs'], [('E', 0)])
                A(lambda e: e.activation(out=E[1][:], in_=LDs[:], func=AF.Exp, scale=-1.0), ['LDs'], [('E', 1)])
                A(lambda e: e.activation(out=E[2][:], in_=d3[:], func=AF.Exp), ['d3'], [('E', 2)])
                V(lambda e: e.tensor_tensor(out=d3[:], in0=LDs[:], in1=lw[:], op=ALU.subtract), ['LDs', 'lw', ('E', 2)], ['d3'])
                A(lambda e: e.activation(out=E[3][:], in_=d3[:], func=AF.Exp), ['d3'], [('E', 3)])
                V(lambda e: e.tensor_tensor(out=rt[:], in0=rk[:, 0:256], in1=E[0][:], op=ALU.mult), ['rk', ('E', 0)], ['rt'])
                V(lambda e: e.scalar_tensor_tensor(out=at[:], in0=kk[:], scalar=-1.0, in1=E[3][:], op0=ALU.mult, op1=ALU.mult), ['kk', ('E', 3)], ['at'])
                V(lambda e: e.tensor_tensor(out=bt[:], in0=bq[:], in1=E[1][:], op=ALU.mult), ['bq', ('E', 1)], ['bt'])
                V(lambda e: e.tensor_tensor(out=kt[:], in0=kmod[:], in1=E[1][:], op=ALU.mult), ['kmod', ('E', 1)], ['kt'])
                V(lambda e: e.tensor_tensor(out=bh[:], in0=bq[:], in1=E[2][:], op=ALU.mult), ['bq', ('E', 2)], ['bh'])
                V(lambda e: e.tensor_tensor(out=kh[:], in0=kmod[:], in1=E[2][:], op=ALU.mult), ['kmod', ('E', 2)], ['kh'])
                for h in range(4):
                    px = newps_r()
                    for i, (src, key) in enumerate([(at, 'at'), (rt, 'rt'), (bt, 'bt'), (kt, 'kt')]):
                        PE(lambda e, px=px, i=i, src=src, h=h: e.transpose(out=ps[px][0:64, i * 128:(i + 1) * 128], in_=src[:, h * 64:(h + 1) * 64], identity=ident),
                           [key, 'cst'], [('ps', px)])
                    A(lambda e, px=px, h=h: e.activation(out=XT[h][:], in_=ps[px][0:64, :], func=AF.Identity), [('ps', px)], [('XT', h)])
                for h in range(4):
                    pa_ = newps_r()
                    PE(lambda e, pa_=pa_, h=h: e.matmul(ps[pa_][:, 0:256], lhsT=XT[h][:, 256:384], rhs=XT[h][:, 0:256],
                                                        start=True, stop=True), [('XT', h)], [('ps', pa_)])
                    PE(lambda e, pa_=pa_, h=h: e.matmul(ps[pa_][:, 256:512], lhsT=XT[h][:, 384:512], rhs=XT[h][:, 0:256],
                                                        start=True, stop=True), [('XT', h)], [('ps', pa_)])
                    V(lambda e, pa_=pa_, h=h: e.tensor_tensor(out=NTm[h][0][:], in0=ps[pa_][:, 0:128], in1=strictm, op=ALU.mult), [('ps', pa_), 'cst'], [('NT', h, 0)])
                    V(lambda e, pa_=pa_, h=h: e.tensor_tensor(out=AT[h][:, 0:128], in0=ps[pa_][:, 128:256], in1=tri, op=ALU.mult), [('ps', pa_), 'cst'], [('AT', h)])
                    V(lambda e, pa_=pa_, h=h: e.tensor_tensor(out=AakT[h][:], in0=ps[pa_][:, 256:384], in1=strictm, op=ALU.mult), [('ps', pa_), 'cst'], [('AakT', h)])
                    V(lambda e, pa_=pa_, h=h: e.tensor_tensor(out=AT[h][:, 128:256], in0=ps[pa_][:, 384:512], in1=tri, op=ALU.mult), [('ps', pa_), 'cst'], [('AT', h)])
                for h in range(4):
                    pt = newps_r()
                    PE(lambda e, pt=pt, h=h: e.transpose(out=ps[pt][:, 0:128], in_=NTm[h][0][:], identity=ident), [('NT', h, 0), 'cst'], [('ps', pt)])
                    PE(lambda e, pt=pt, h=h: e.matmul(ps[pt][:, 128:192], lhsT=AakT[h][:], rhs=vv[:, h * 64:(h + 1) * 64], start=True, stop=True),
                       [('AakT', h), 'vv'], [('ps', pt)])
                    A(lambda e, pt=pt, h=h: e.activation(out=Nm[h][0][:], in_=ps[pt][:, 0:128], func=AF.Identity), [('ps', pt)], [('N', h, 0)])
                    A(lambda e, pt=pt, h=h: e.activation(out=Rr[h][:, 0:64], in_=ps[pt][:, 128:192], func=AF.Identity), [('ps', pt)], [('R', h)])
                    V(lambda e, h=h: e.tensor_copy(out=Rr[h][:, 64:128], in_=at[:, h * 64:(h + 1) * 64]), ['at'], [('R', h)])
                for lvl in range(7):
                    cur = lvl % 2
                    nx = 1 - cur
                    for h in range(4):
                        pr = newps_r()
                        PE(lambda e, pr=pr, cur=cur, h=h: e.matmul(ps[pr][:, 0:128], lhsT=NTm[h][cur][:], rhs=Rr[h][:], start=True, stop=True),
                           [('NT', h, cur), ('R', h)], [('ps', pr)])
                        if lvl < 6:
                            PE(lambda e, pr=pr, cur=cur, h=h: e.matmul(ps[pr][:, 128:256], lhsT=NTm[h][cur][:], rhs=Nm[h][cur][:], start=True, stop=True),
                               [('NT', h, cur), ('N', h, cur)], [('ps', pr)])
                            PE(lambda e, pr=pr, cur=cur, h=h: e.matmul(ps[pr][:, 256:384], lhsT=Nm[h][cur][:], rhs=NTm[h][cur][:], start=True, stop=True),
                               [('NT', h, cur), ('N', h, cur)], [('ps', pr)])
                        V(lambda e, pr=pr, h=h: e.tensor_tensor(out=Rr[h][:], in0=Rr[h][:], in1=ps[pr][:, 0:128], op=ALU.add), [('R', h), ('ps', pr)], [('R', h)])
                        if lvl < 6:
                            A(lambda e, pr=pr, nx=nx, h=h: e.activation(out=Nm[h][nx][:], in_=ps[pr][:, 128:256], func=AF.Identity), [('ps', pr)], [('N', h, nx)])
                            V(lambda e, pr=pr, nx=nx, h=h: e.tensor_copy(out=NTm[h][nx][:], in_=ps[pr][:, 256:384]), [('ps', pr)], [('NT', h, nx)])
                pws, pus, pys, phs = {}, {}, {}, {}
                for h in range(4):
                    pws[h] = newps_r()
                    PE(lambda e, pw_=pws[h], h=h: e.transpose(out=ps[pw_][0:64, 0:128], in_=Rr[h][:, 64:128], identity=ident), [('R', h), 'cst'], [('ps', pws[h])])
                    A(lambda e, pw_=pws[h], h=h: e.activation(out=WmT[h][:], in_=ps[pw_][0:64, 0:128], func=AF.Identity), [('ps', pws[h])], [('WmT', h)])
                for h in range(4):
                    pus[h] = newps_r()
                    PE(lambda e, pu=pus[h], h=h: e.matmul(ps[pu][:, 0:64], lhsT=WmT[h][:], rhs=Hp[h][:], start=True, stop=True), [('WmT', h), ('Hp', h)], [('ps', pus[h])])
                    V(lambda e, pu=pus[h], h=h: e.tensor_tensor(out=Up[:, h * 64:(h + 1) * 64], in0=Rr[h][:, 0:64], in1=ps[pu][:, 0:64], op=ALU.add),
                      [('R', h), ('ps', pus[h])], [('Up', h)])
                for h in range(4):
                    py = newps_r()
                    PE(lambda e, py=py, h=h: e.matmul(ps[py][:, 0:64], lhsT=XT[h][:, 128:256], rhs=Hp[h][:], start=True, stop=False), [('XT', h), ('Hp', h)], [('ps', py)])
                    PE(lambda e, py=py, h=h: e.matmul(ps[py][:, 0:64], lhsT=AT[h][:, 0:128], rhs=Up[:, h * 64:(h + 1) * 64], start=False, stop=False),
                       [('AT', h), ('Up', h)], [('ps', py)])
                    PE(lambda e, py=py, h=h: e.matmul(ps[py][:, 0:64], lhsT=AT[h][:, 128:256], rhs=vv[:, h * 64:(h + 1) * 64], start=False, stop=True),
                       [('AT', h), 'vv'], [('ps', py)])
                    A(lambda e, py=py, h=h: e.activation(out=yraw[:, h * 64:(h + 1) * 64], in_=ps[py][:, 0:64], func=AF.Identity), [('ps', py)], [('yraw', h // 2)])
                    ph = newps_r()
                    PE(lambda e, ph=ph, h=h: e.matmul(ps[ph][0:64, 0:64], lhsT=bh[:, h * 64:(h + 1) * 64], rhs=Up[:, h * 64:(h + 1) * 64], start=True, stop=False),
                       ['bh', ('Up', h)], [('ps', ph)])
                    PE(lambda e, ph=ph, h=h: e.matmul(ps[ph][0:64, 0:64], lhsT=kh[:, h * 64:(h + 1) * 64], rhs=vv[:, h * 64:(h + 1) * 64], start=False, stop=True),
                       ['kh', 'vv'], [('ps', ph)])
                    PE(lambda e, ph=ph, h=h: e.matmul(ps[ph][0:64, 256:257], lhsT=lw[:, h * 64:(h + 1) * 64], rhs=cst[:, 3, 0:1], start=True, stop=True),
                       ['lw', 'cst'], [('ps', ph)])
                    A(lambda e, ph=ph, h=h: e.activation(out=DC[h][:], in_=ps[ph][0:64, 256:257], func=AF.Exp), [('ps', ph)], [('DC', h)])
                    V(lambda e, ph=ph, h=h: e.scalar_tensor_tensor(out=Hp[h][:], in0=Hp[h][:], scalar=DC[h][:, 0:1], in1=ps[ph][0:64, 0:64], op0=ALU.mult, op1=ALU.add),
                      [('Hp', h), ('DC', h), ('ps', ph)], [('Hp', h)])
                for h in range(4):
                    V(lambda e, h=h: e.bn_stats(out=stats[:, h, :], in_=yraw[:, h * 64:(h + 1) * 64]), [('yraw', h // 2)], [('stats', h)])
                for h in range(4):
                    V(lambda e, h=h: e.bn_aggr(out=mv[:, h, :], in_=stats[:, h, :]), [('stats', h)], [('mv', h)])
                for h in range(4):
                    V(lambda e, h=h: e.tensor_scalar(out=ss[:, 4 + h:5 + h], in0=mv[:, h, 1:2], scalar1=RW_GN_EPS, scalar2=None, op0=ALU.add), [('mv', h)], [('ss', 4 + h)])
                for h in range(4):
                    A(lambda e, h=h: e.activation(out=ss[:, 4 + h:5 + h], in_=ss[:, 4 + h:5 + h], func=AF.Sqrt), [('ss', 4 + h)], [('ss', 4 + h)])
                for h in range(4):
                    V(lambda e, h=h: e.reciprocal(out=rn[:, 4 + h:5 + h], in_=ss[:, 4 + h:5 + h]), [('ss', 4 + h)], [('rn', 4 + h)])
                for h in range(4):
                    V(lambda e, h=h: e.tensor_scalar(out=outr[:, h * 64:(h + 1) * 64], in0=yraw[:, h * 64:(h + 1) * 64], scalar1=mv[:, h, 0:1], scalar2=rn[:, 4 + h:5 + h],
                                                     op0=ALU.subtract, op1=ALU.mult), [('yraw', h // 2), ('mv', h), ('rn', 4 + h)], [('outr', h)])
                ok4 = [('outr', h) for h in range(4)]
                V(lambda e: e.tensor_tensor(out=outr[:], in0=outr[:], in1=pvb[:, 1280:1536], op=ALU.mult), ok4 + ['pvb'], ok4)
                V(lambda e: e.tensor_tensor(out=outr[:], in0=outr[:], in1=pvb[:, 1536:1792], op=ALU.add), ok4 + ['pvb'], ok4)
                V(lambda e: e.tensor_tensor(out=junk[:], in0=rk[:, 0:256], in1=kmod[:], op=ALU.mult), ['rk', 'kmod'], ['junk'])
                V(lambda e: e.tensor_tensor(out=junk[:], in0=junk[:], in1=pvb[:, 1024:1280], op=ALU.mult), ['junk', 'pvb'], ['junk'])
                V(lambda e: e.reduce_sum(out=rkb[:], in_=junk[:].rearrange("p (h c) -> p h c", h=4), axis=AX.X), ['junk'], ['rkb'])
                for h in range(4):
                    V(lambda e, h=h: e.scalar_tensor_tensor(out=outr[:, h * 64:(h + 1) * 64], in0=vv[:, h * 64:(h + 1) * 64], scalar=rkb[:, h:h + 1],
                                                            in1=outr[:, h * 64:(h + 1) * 64], op0=ALU.mult, op1=ALU.add), ['vv', 'rkb'] + ok4, [('outr', h)])
                V(lambda e: e.tensor_tensor(out=outrb[:], in0=outr[:], in1=gate[:], op=ALU.mult), ok4 + ['gate'], ['outrb'])
                S.op('sync', lambda e, t0=t0: e.dma_start(out=out[t0:t0 + 128, 0:256], in_=outrb[:]), reads=['outrb'], writes=[('outA', t)], dma=True, dkey='outAst')
            if do_moba:
                qb = t // 2
                ti = t % 4
                for which, bank in [(0, 0), (1, 1)]:
                    for h in range(2):
                        for k in range(8):
                            PE(lambda e, which=which, bank=bank, h=h, k=k: e.matmul(ps[bank][:, h * 128:(h + 1) * 128],
                                                                                    lhsT=wm[:, k, which * 256 + h * 128: which * 256 + (h + 1) * 128],
                                                                                    rhs=xt[:, k, 1:129], start=(k == 0), stop=(k == 7)), ['xt', 'wm'], [('ps', bank)])
                A(lambda e: e.activation(out=qTf[:], in_=ps[0][:, 0:256], func=AF.Identity), [('ps', 0)], ['qTf'])
                for h in range(2):
                    V(lambda e, h=h, ti=ti: e.tensor_scalar(out=qg[h][:, ti * 128:(ti + 1) * 128], in0=qTf[:, h * 128:(h + 1) * 128], scalar1=128.0 ** -0.5,
                                                            scalar2=None, op0=ALU.mult), ['qTf'], [('qg', h)])
                A(lambda e: e.activation(out=kTt[:], in_=ps[1][:, 0:256], func=AF.Identity), [('ps', 1)], ['kTt'])
                V(lambda e: e.reduce_sum(out=kpart[:], in_=ps[1][:, 0:256].rearrange("p (h c) -> p h c", h=2), axis=AX.X), [('ps', 1)], ['kpart'])
                for k in range(8):
                    PE(lambda e, k=k: e.matmul(ps[0][:, 256:512], lhsT=xt[:, k, 1:129], rhs=wm[:, k, 512:768], start=(k == 0), stop=(k == 7)),
                       ['xt', 'wm'], [('ps', 0)])
                V(lambda e: e.tensor_copy(out=v1t[:, :, 0:128], in_=ps[0][:, 256:512].rearrange("p (h c) -> p h c", h=2)), [('ps', 0)], ['v1t'])
                for h in range(2):
                    S.op('sync', lambda e, h=h, t0=t0: e.dma_start(out=kTs[h, :, t0:t0 + 128], in_=kTt[:, h * 128:(h + 1) * 128]), reads=['kTt'],
                         writes=[('kTs', h, t)], dma=True, dkey=('kst', h))
                    S.op('sync', lambda e, h=h, t0=t0: e.dma_start(out=v1s[h, t0:t0 + 128, :], in_=v1t[:, h, :]), reads=['v1t'],
                         writes=[('v1s', h, t)], dma=True, dkey=('vst', h))
                if t % 2 == 0:
                    V(lambda e: e.tensor_copy(out=kacc[:], in_=kpart[:]), ['kpart'], ['kacc'])
                pgs = {}
                for h in range(2):
                    pgs[h] = newps()
                    PE(lambda e, pg2=pgs[h], h=h: e.matmul(ps[pg2][:, 0:64], lhsT=qTf[:, h * 128:(h + 1) * 128], rhs=kmT[:, h, :], start=True, stop=True),
                       ['qTf', 'kmT'], [('ps', pgs[h])])
                for h in range(2):
                    V(lambda e, pg2=pgs[h], h=h: e.tensor_copy(out=gm[h][:], in_=ps[pg2][:, 0:64]), [('ps', pgs[h])], [('gm', h)])
                    V(lambda e, qb=qb, h=h: e.memset(gm[h][:, qb:64], NEG), [('gm', h)], [('gm', h)])
                for h in range(2):
                    V(lambda e, h=h: e.max(out=m8[h][:], in_=gm[h][:]), [('gm', h)], [('m8', h)])
                for h in range(2):
                    V(lambda e, h=h: e.tensor_scalar(out=bias[h][:], in0=gm[h][:], scalar1=m8[h][:, 2:3], scalar2=None, op0=ALU.is_ge), [('gm', h), ('m8', h)], [('bias', h)])
                for h in range(2):
                    V(lambda e, h=h: e.tensor_scalar(out=bias[h][:], in0=bias[h][:], scalar1=-NEG, scalar2=NEG, op0=ALU.mult, op1=ALU.add), [('bias', h)], [('bias', h)])
                for h in range(2):
                    if qb + 1 < 64:
                        V(lambda e, qb=qb, h=h: e.memset(bias[h][:, qb + 1:64], NEG), [('bias', h)], [('bias', h)])
                    V(lambda e, qb=qb, h=h: e.memset(bias[h][:, qb:qb + 1], 0.0), [('bias', h)], [('bias', h)])
                pbs = {}
                for h in range(2):
                    pbs[h] = newps()
                    PE(lambda e, pb2=pbs[h], h=h: e.transpose(out=ps[pb2][0:64, 0:128], in_=bias[h][:], identity=ident), [('bias', h), 'cst'], [('ps', pbs[h])])
                for h in range(2):
                    A(lambda e, pb2=pbs[h], h=h, ti=ti: e.activation(out=biasT[h][:, ti * 128:(ti + 1) * 128], in_=ps[pb2][0:64, 0:128], func=AF.Identity),
                      [('ps', pbs[h])], [('biasT', h)])
                if t % 2 == 1:
                    V(lambda e: e.tensor_tensor(out=kacc[:], in0=kacc[:], in1=kpart[:], op=ALU.add), ['kacc', 'kpart'], ['kacc'])
                    V(lambda e, qb=qb: e.tensor_scalar(out=kmT[:, :, qb], in0=kacc[:], scalar1=1.0 / 256.0, scalar2=None, op0=ALU.mult), ['kacc'], ['kmT'])
                if ti == 3:
                    G = t // 4
                    for h in range(2):
                        its = [(kc, j) for kc in range(G + 1) for j in range(4)]
                        pend = None

                        def emit_pv(kc, j, pi, sl, h=h):
                            first = (kc == 0 and j == 0)
                            last = (kc == G and j == 3)
                            for qs in range(4):
                                PE(lambda e, qs=qs, pi=pi, sl=sl, j=j, first=first, last=last: e.matmul(
                                    ps[2 + qs][:, 0:129], lhsT=PT[pi][:, qs * 128:(qs + 1) * 128], rhs=vcb[sl][:, j, :],
                                    start=first, stop=last), [('PT', pi), ('vcb', sl)], [('ps', 2 + qs)])
                        for cnt, (kc, j) in enumerate(its):
                            sl = (kc + h) % 2
                            if j == 0:
                                S.op('sync', lambda e, sl=sl, h=h, kc=kc: e.dma_start(out=kcb[sl][:], in_=kTs[h, :, kc * 512:(kc + 1) * 512]),
                                     reads=[('kTs', h, 4 * kc + jj) for jj in range(4)], writes=[('kcb', sl)], dma=True)
                                S.op('sync', lambda e, sl=sl, h=h, kc=kc: e.dma_start(out=vcb[sl][:], in_=v1s[h, kc * 512:(kc + 1) * 512, :].rearrange("(c p) f -> p c f", p=128)),
                                     reads=[('v1s', h, 4 * kc + jj) for jj in range(4)], writes=[('vcb', sl)], dma=True)
                            n = 2 * kc + j // 2
                            pS = newps()
                            diag = (kc == G)
                            PE(lambda e, pS=pS, sl=sl, j=j, h=h: e.matmul(ps[pS][:], lhsT=kcb[sl][:, j * 128:(j + 1) * 128], rhs=qg[h][:], start=True, stop=False),
                               [('kcb', sl), ('qg', h)], [('ps', pS)])
                            PE(lambda e, pS=pS, n=n, h=h, diag=diag: e.matmul(ps[pS][:], lhsT=esel[:, n, :], rhs=biasT[h][:], start=False, stop=(not diag)),
                               ['esel', ('biasT', h)], [('ps', pS)])
                            if diag:
                                PE(lambda e, pS=pS, j=j: e.matmul(ps[pS][:], lhsT=identb[:], rhs=cmask[:, j, :], start=False, stop=True),
                                   ['identb', 'cmask'], [('ps', pS)])
                            pi = cnt % 2
                            if pend is not None:
                                emit_pv(*pend)
                            A(lambda e, pS=pS, pi=pi: e.activation(out=PT[pi][:], in_=ps[pS][:], func=AF.Exp), [('ps', pS)], [('PT', pi)])
                            pend = (kc, j, pi, sl)
                        emit_pv(*pend)
                        for qs in range(4):
                            V(lambda e, qs=qs: e.reciprocal(out=rec[:, qs:qs + 1], in_=ps[2 + qs][:, 128:129]), [('ps', 2 + qs)], [('rec', qs)])
                            V(lambda e, qs=qs, h=h: e.tensor_scalar(out=mo[:, qs, h * 128:(h + 1) * 128], in0=ps[2 + qs][:, 0:128], scalar1=rec[:, qs:qs + 1],
                                                                    scalar2=None, op0=ALU.mult), [('ps', 2 + qs), ('rec', qs)], [('mo', qs)])
                    for qs in range(4):
                        tq = (4 * G + qs) * 128
                        S.op('sync', lambda e, qs=qs, tq=tq: e.dma_start(out=out[tq:tq + 128, 256:512], in_=mo[:, qs, :]), reads=[('mo', qs)],
                             writes=[('outB', 4 * G + qs)], dma=True, dkey=('outBst', qs))
        S.emit()
    return S


def mix1_consts():
    i = np.arange(128)
    ident = np.eye(128, dtype=np.float32)
    tri = (i[:, None] <= i[None, :]).astype(np.float32)
    strict = (i[:, None] < i[None, :]).astype(np.float32)
    ones = np.ones((128, 128), np.float32)
    esel = np.zeros((64, 64, 128), np.float32)
    esel[np.arange(64), np.arange(64), :] = 1.0
    q = np.arange(512)
    cm = np.zeros((128, 4, 512), np.float32)
    for j in range(4):
        cm[:, j, :] = np.where((j * 128 + i)[:, None] <= q[None, :], 0.0, NEG)
    return np.stack([ident, tri, strict, ones]), esel.reshape(64, -1), cm.reshape(128, -1)


def mix1_inputs(xb, g, w_in, rw_mu, rw_w0, rw_w2, rw_a0, rw_a2, rw_g2, rw_k_k, rw_k_a, rw_r_k, rw_gn_g, rw_gn_b):
    T = xb.shape[0]
    xTp = np.zeros((D, T + 1), np.float32)
    xTp[:, 1:] = xb.T
    r = lambda a, n: np.arange(a, a + n)
    hc = r(g * 256, 256)
    rcols = np.concatenate([hc, 1024 + hc, 2048 + hc, r(3072, 288)])
    Wr = w_in[:, rcols]
    mu = rw_mu[rcols]
    mcols = np.concatenate([3360 + hc, 3360 + 1024 + hc, 3360 + 2048 + hc])
    Wm = w_in[:, mcols]
    pv = np.concatenate([rw_w0[hc], rw_a0[hc], rw_k_k[hc], rw_k_a[hc], rw_r_k.reshape(-1)[hc], rw_gn_g[hc], rw_gn_b[hc]])
    c, es_, cm = mix1_consts()
    return {"xTp": xTp, "Wr": np.ascontiguousarray(Wr), "mu": np.ascontiguousarray(mu), "Wm": np.ascontiguousarray(Wm),
            "pv": np.ascontiguousarray(pv.astype(np.float32)), "lw2": np.ascontiguousarray(rw_w2[:, hc]), "la2": np.ascontiguousarray(rw_a2[:, hc]),
            "lg2": np.ascontiguousarray(rw_g2[:, hc]), "consts": c, "esel": es_, "cmask": cm}


class _G:
    pass


def build_fused(T=16384, NEXP=NE):
    nc = bass.Bass("TRN2", target_bir_lowering=False)
    TQ = T // 4
    PT = min(1024, TQ)

    def dt(name, shape, kind="ExternalInput", dty=F32, **kw):
        return nc.dram_tensor(name, shape, dty, kind=kind, **kw).ap()
    consts = dt("consts", [4, 128, 128])
    W0 = dict(xTp=dt("xTp", [D, T + 3]), Wc=dt("Wc", [D, 1280]), cw=dt("cw", [4 * 1280]), Wp=dt("Wp", [D, 520]), pv0=dt("pv0", [NPV]), consts=consts)
    W1 = dict(Wr=dt("Wr", [D, 1056]), mu=dt("mu", [1056]), Wm=dt("Wm", [D, 768]), pv1=dt("pv1", [1792]), lw2=dt("lw2", [64, 256]),
              la2=dt("la2", [64, 256]), lg2=dt("lg2", [160, 256]), consts=consts, esel=dt("esel", [64, 64 * 128]), cmask=dt("cmask", [128, 4 * 512]),
              kTs=dt("kTs", [2, 128, T], kind="Internal", dty=BF16), v1s=dt("v1s", [2, T, 129], kind="Internal", dty=BF16))
    ident = dt("ident", [128, 128])
    x1s = dt("x1s", [TQ, D], kind="Internal")
    posts = []
    for L in range(2):
        posts.append(dict(w_out=dt("w_out_%d" % L, [2048, D]), lnp=dt("lnp_%d" % L, [4 * D]), rw=dt("rw_%d" % L, [D, NE]), rb=dt("rb_%d" % L, [NE]),
                          w_up=dt("w_up_%d" % L, [NEXP, D, 2 * D]), b_upT=dt("b_upT_%d" % L, [128, NE * 16]), w_down=dt("w_down_%d" % L, [NEXP, D, D]),
                          b_down=dt("b_down_%d" % L, [NE, D]), ident=ident, x1s=x1s))
    xres0 = dt("xres0", [TQ, D])
    y = dt("y", [TQ, D], kind="ExternalOutput")
    mxo = dt("mxo", [T, 512], kind="Internal", dty=BF16)
    mxg = dt("mxg", [4 * T, 512], kind="Internal", dty=BF16, addr_space="Local")
    x1o = dt("x1o", [TQ, D], kind="Internal")
    x1g = dt("x1g", [T, D], kind="Internal", addr_space="Local")
    G = _G()
    G.phase = 0
    with ExitStack() as gs:
        G.sem_stack = gs
        G.ps = [gs.enter_context(nc.psum_tensor("ps%d" % i, [128, 512], F32)) for i in range(8)]
        G.bar = gs.enter_context(nc.semaphore("bar"))
        import os
        nph = int(os.environ.get("FUSED_PHASES", "4"))
        phase_mix0(nc, G, T, W0, mxo)
        if nph >= 2:
            phase_post(nc, G, 0, TQ, PT, posts[0], xres0, x1o if nph > 2 else y, mxo, mxg, NEXP=NEXP)
        if nph >= 3:
            phase_mix1(nc, G, T, W1, x1o, x1g, mxo)
        if nph >= 4:
            phase_post(nc, G, 1, TQ, PT, posts[1], x1o, y, mxo, mxg, NEXP=NEXP)
    return nc


_N0 = ['ev_w_in', 'ev_ssd_conv_w', 'ev_ssd_conv_b', 'ev_ssd_dt_bias', 'ev_ssd_a_log', 'ev_ssd_d', 'ev_ssd_norm_w', 'ev_gdn_conv_w',
       'ev_gdn_a_log', 'ev_gdn_dt_bias', 'ev_gdn_norm_w']
_N1 = ['od_w_in', 'od_rw_mu', 'od_rw_w0', 'od_rw_w2', 'od_rw_a0', 'od_rw_a2', 'od_rw_g2', 'od_rw_k_k', 'od_rw_k_a', 'od_rw_r_k',
       'od_rw_gn_g', 'od_rw_gn_b']


def fused_inputs(inp, T=None):
    f32 = lambda a: np.ascontiguousarray(np.asarray(a, dtype=np.float32))
    x = np.asarray(inp['x'], dtype=np.float32)
    if T is not None:
        x = x[:, :T]
    B, T, Dm = x.shape
    TQ = T // 4
    P0 = [f32(inp[k][0]) for k in _N0]
    P1 = [f32(inp[k][0]) for k in _N1]
    rows = np.concatenate([np.concatenate([np.arange(r * 256, (r + 1) * 256), 1024 + np.arange(r * 256, (r + 1) * 256)]) for r in range(4)])
    shared = {"ident": np.eye(128, dtype=np.float32)}
    for L, wkey in [(0, 'ev_w_out'), (1, 'od_w_out')]:
        pi = post_inputs(np.zeros((1, 2048), np.float32), np.zeros((1, Dm), np.float32), f32(inp[wkey][0])[rows], f32(inp['mix_ln_g'][L]),
                         f32(inp['mix_ln_b'][L]), f32(inp['moe_router_w'][L]), f32(inp['moe_router_b'][L]), np.asarray(inp['moe_w_up'][L]),
                         f32(inp['moe_b_up'][L]), f32(inp['moe_w_down'][L]), f32(inp['moe_b_down'][L]), f32(inp['ffn_ln_g'][L]), f32(inp['ffn_ln_b'][L]))
        for k in ['w_out', 'lnp', 'rw', 'rb', 'w_up', 'b_upT', 'w_down', 'b_down']:
            shared["%s_%d" % (k, L)] = pi[k]
    xf = x.reshape(B * T, Dm)
    ims = []
    for c in range(8):
        b, g = c // 4, c % 4
        im = dict(shared)
        m0 = mix0_inputs(x[b], g, *P0)
        m0['pv0'] = m0.pop('pv')
        im.update(m0)
        m1 = mix1_inputs(np.zeros((1, Dm), np.float32), g, *P1)
        m1.pop('xTp')
        m1.pop('consts')
        m1['pv1'] = m1.pop('pv')
        im.update(m1)
        im['xres0'] = np.ascontiguousarray(xf[c * TQ:(c + 1) * TQ])
        ims.append(im)
    return ims, (B, T, Dm)


_NC_CACHE = {}


def kernel(**inp):
    ims, (B, T, Dm) = fused_inputs(inp)
    if T not in _NC_CACHE:
        _NC_CACHE[T] = build_fused(T=T)
    res = run_bass_kernel_spmd(_NC_CACHE[T], ims, core_ids=list(range(8)))
    y = np.concatenate([res.results[c]['y'] for c in range(8)], axis=0)
    return y.reshape(B, T, Dm).astype(np.float32)
```
